# Optimizing a Trainium2 kernel written in Bass

```python
import jax, jax.numpy as jnp
from jax import lax
import numpy as np

D_MODEL = 1024
BATCH = 8
SEQ = 8192
DEPTH = 1

CHUNK = 64
D_MIX = D_MODEL
D_RNN = D_MIX // 2
RNN_BLOCKS = 8
RNN_BLOCK = D_RNN // RNN_BLOCKS
CONV_WIDTH = 4
RG_C = 8.0
N_HEADS = 8
HEAD_DIM = 64
D_ATTN = N_HEADS * HEAD_DIM
IDX_HEADS = 8
IDX_DIM = 64
TOPK_KEYS_MAX = 256
Q_BLOCK = 128
N_EXPERTS = 32
TOPK_EXPERTS = 4
D_FF = D_MODEL
SWIGLU_ALPHA = 1.702
SWIGLU_LIMIT = 7.0
MOE_BLOCK = 256
EPS = 1e-6
IN_SPLITS = [D_RNN, D_RNN, D_ATTN, D_ATTN, D_ATTN, IDX_HEADS * IDX_DIM, IDX_DIM, IDX_HEADS]
D_IN = sum(IN_SPLITS)

kernel_name = "hybrid_rglru_dsa_moe_adaln"


def rms_norm(x, g):
    xf = x.astype(jnp.float32)
    y = xf * lax.rsqrt(jnp.mean(xf * xf, axis=-1, keepdims=True) + EPS)
    return (y * g.astype(jnp.float32)).astype(x.dtype)


def rg_lru_branch(xr, xg, conv_w, conv_b, w_a, b_a, w_x, b_x, lam):
    B, S, _ = xr.shape
    xc = lax.conv_general_dilated(
        xr, conv_w[:, None, :].astype(xr.dtype), window_strides=(1,),
        padding=[(CONV_WIDTH - 1, 0)], dimension_numbers=("NWC", "WIO", "NWC"),
        feature_group_count=D_RNN) + conv_b
    xb = xc.reshape(B, S, RNN_BLOCKS, RNN_BLOCK)
    r = jax.nn.sigmoid((jnp.einsum("bshi,hij->bshj", xb, w_a).reshape(B, S, D_RNN) + b_a).astype(jnp.float32))
    i = jax.nn.sigmoid((jnp.einsum("bshi,hij->bshj", xb, w_x).reshape(B, S, D_RNN) + b_x).astype(jnp.float32))
    log_a = -RG_C * r * jax.nn.softplus(-lam.astype(jnp.float32))
    a = jnp.exp(log_a)
    mult = jnp.sqrt(-jnp.expm1(2.0 * log_a))
    u = mult * i * xc.astype(jnp.float32)

    def combine(left, right):
        a1, b1 = left
        a2, b2 = right
        return a1 * a2, a2 * b1 + b2

    _, h = lax.associative_scan(combine, (a, u), axis=1)
    return (jax.nn.gelu(xg.astype(jnp.float32)) * h).astype(xr.dtype)


def dsa_attention(q, k, v, q_idx, k_idx, w_idx):
    B, S = q.shape[0], q.shape[1]
    n_sel = min(TOPK_KEYS_MAX, S // 4)
    n_qb = S // Q_BLOCK
    key_chunk = jnp.arange(S) // CHUNK
    k_idx_f = k_idx.astype(jnp.float32)
    gather = jax.vmap(lambda t, idx: t[idx])

    def to_blocks(t):
        return jnp.swapaxes(t.reshape((B, n_qb, Q_BLOCK) + t.shape[2:]), 0, 1)

    def one_block(args):
        qb, qib, wb, bi = args
        q_chunk = (bi * Q_BLOCK + jnp.arange(Q_BLOCK)) // CHUNK
        s = jnp.einsum("bqhd,bsd->bqhs", qib.astype(jnp.float32), k_idx_f) * (IDX_DIM ** -0.5)
        score = jnp.einsum("bqh,bqhs->bqs", wb.astype(jnp.float32) * (IDX_HEADS ** -0.5), jax.nn.relu(s))
        admissible = key_chunk[None, :] <= q_chunk[:, None]
        score = jnp.where(admissible[None], score, -jnp.inf)
        _, sel = lax.top_k(score, n_sel)
        valid = (sel // CHUNK) <= q_chunk[None, :, None]
        ks = gather(k, sel)
        vs = gather(v, sel)
        logits = jnp.einsum("bqhd,bqkhd->bqhk", qb, ks).astype(jnp.float32) * (HEAD_DIM ** -0.5)
        logits = jnp.where(valid[:, :, None, :], logits, -jnp.inf)
        p = jax.nn.softmax(logits, axis=-1).astype(v.dtype)
        return jnp.einsum("bqhk,bqkhd->bqhd", p, vs)

    out = lax.map(one_block, (to_blocks(q), to_blocks(q_idx), to_blocks(w_idx), jnp.arange(n_qb)))
    return jnp.swapaxes(out, 0, 1).reshape(B, S, D_ATTN)


def moe_ffn(h, w_router, b_router, w1, b1, w2, b2):
    T, D = h.shape
    logits = (h @ w_router + b_router).astype(jnp.float32)
    top_val, top_e = lax.top_k(logits, TOPK_EXPERTS)
    gates = jax.nn.softmax(top_val, axis=-1).astype(h.dtype)
    TK = T * TOPK_EXPERTS
    e_flat = top_e.reshape(TK)
    tok_flat = jnp.arange(TK, dtype=jnp.int32) // TOPK_EXPERTS
    order = jnp.argsort(e_flat, stable=True)
    e_sorted = e_flat[order]
    counts = jnp.bincount(e_flat, length=N_EXPERTS)
    group_start = jnp.cumsum(counts) - counts
    padded = (counts + MOE_BLOCK - 1) // MOE_BLOCK * MOE_BLOCK
    pad_end = jnp.cumsum(padded)
    pad_start = pad_end - padded
    dest = pad_start[e_sorted] + jnp.arange(TK, dtype=jnp.int32) - group_start[e_sorted]
    n_rows = TK + N_EXPERTS * MOE_BLOCK
    n_blocks = n_rows // MOE_BLOCK
    row_tok = jnp.full((n_rows,), T, jnp.int32).at[dest].set(tok_flat[order])
    row_gate = jnp.zeros((n_rows,), h.dtype).at[dest].set(gates.reshape(TK)[order])
    block_e = jnp.minimum(jnp.searchsorted(pad_end, jnp.arange(n_blocks, dtype=jnp.int32) * MOE_BLOCK, side="right"), N_EXPERTS - 1)
    h_pad = jnp.concatenate([h, jnp.zeros((1, D), h.dtype)], axis=0)

    def expert_block(args):
        tok, gate, e = args
        u = h_pad[tok] @ w1[e] + b1[e]
        u_glu = jnp.minimum(u[:, ::2], SWIGLU_LIMIT)
        u_lin = jnp.clip(u[:, 1::2], -SWIGLU_LIMIT, SWIGLU_LIMIT)
        act = u_glu * jax.nn.sigmoid(SWIGLU_ALPHA * u_glu) * (u_lin + 1)
        return (act @ w2[e] + b2[e]) * gate[:, None]

    y = lax.map(expert_block, (row_tok.reshape(n_blocks, MOE_BLOCK), row_gate.reshape(n_blocks, MOE_BLOCK), block_e))
    return jax.ops.segment_sum(y.reshape(n_rows, D), row_tok, num_segments=T + 1)[:T]


def hybrid_layer(x, c, w_ada, b_ada, norm1_g, w_in, conv_w, conv_b, w_rg_a, b_rg_a, w_rg_x, b_rg_x,
                 lru_lambda, q_norm_g, k_norm_g, kidx_norm_g, rg_out_g, attn_out_g, w_out, norm2_g,
                 w_router, b_router, w1, b1, w2, b2):
    B, S, D = x.shape
    mod = jax.nn.silu(c) @ w_ada + b_ada
    sh1, sc1, g1, sh2, sc2, g2 = jnp.split(mod[:, None, :], 6, axis=-1)
    h = rms_norm(x, norm1_g) * (1 + sc1) + sh1
    z = h @ w_in
    xr, xg, q, k, v, qi, ki, wi = jnp.split(z, [int(s) for s in np.cumsum(IN_SPLITS)[:-1]], axis=-1)
    y_rnn = rg_lru_branch(xr, xg, conv_w, conv_b, w_rg_a, b_rg_a, w_rg_x, b_rg_x, lru_lambda)
    q = rms_norm(q.reshape(B, S, N_HEADS, HEAD_DIM), q_norm_g)
    k = rms_norm(k.reshape(B, S, N_HEADS, HEAD_DIM), k_norm_g)
    v = v.reshape(B, S, N_HEADS, HEAD_DIM)
    qi = qi.reshape(B, S, IDX_HEADS, IDX_DIM)
    ki = rms_norm(ki, kidx_norm_g)
    y_attn = dsa_attention(q, k, v, qi, ki, wi)
    mix = jnp.concatenate([rms_norm(y_rnn, rg_out_g), rms_norm(y_attn, attn_out_g)], axis=-1) @ w_out
    x = x + g1 * mix
    h2 = rms_norm(x, norm2_g) * (1 + sc2) + sh2
    ff = moe_ffn(h2.reshape(B * S, D), w_router, b_router, w1, b1, w2, b2).reshape(B, S, D)
    return x + g2 * ff


def setup_inputs(seed: int = 0) -> dict:
    key = jax.random.key(seed)
    ks = jax.random.split(key, 26)
    f32 = jnp.float32
    L = DEPTH

    def nrm(k, shape, scale):
        return jax.random.normal(k, shape, f32) * scale

    a_c = jax.random.uniform(ks[12], (L, D_RNN), f32, 0.81, 0.998)
    sig = a_c ** (1.0 / RG_C)
    return {
        "x": nrm(ks[0], (BATCH, SEQ, D_MODEL), 1.0),
        "c": nrm(ks[1], (BATCH, D_MODEL), 1.0),
        "w_ada": nrm(ks[2], (L, D_MODEL, 6 * D_MODEL), 0.5 * D_MODEL ** -0.5),
        "b_ada": nrm(ks[3], (L, 6 * D_MODEL), 0.02),
        "norm1_g": 1.0 + nrm(ks[4], (L, D_MODEL), 0.02),
        "w_in": nrm(ks[5], (L, D_MODEL, D_IN), D_MODEL ** -0.5),
        "conv_w": nrm(ks[6], (L, CONV_WIDTH, D_RNN), CONV_WIDTH ** -0.5),
        "conv_b": nrm(ks[7], (L, D_RNN), 0.02),
        "w_rg_a": nrm(ks[8], (L, RNN_BLOCKS, RNN_BLOCK, RNN_BLOCK), RNN_BLOCK ** -0.5),
        "b_rg_a": nrm(ks[9], (L, D_RNN), 0.02),
        "w_rg_x": nrm(ks[10], (L, RNN_BLOCKS, RNN_BLOCK, RNN_BLOCK), RNN_BLOCK ** -0.5),
        "b_rg_x": nrm(ks[11], (L, D_RNN), 0.02),
        "lru_lambda": jnp.log(sig) - jnp.log1p(-sig),
        "q_norm_g": 1.0 + nrm(ks[13], (L, HEAD_DIM), 0.02),
        "k_norm_g": 1.0 + nrm(ks[14], (L, HEAD_DIM), 0.02),
        "kidx_norm_g": 1.0 + nrm(ks[15], (L, IDX_DIM), 0.02),
        "rg_out_g": 1.0 + nrm(ks[16], (L, D_RNN), 0.02),
        "attn_out_g": 1.0 + nrm(ks[17], (L, D_ATTN), 0.02),
        "w_out": nrm(ks[18], (L, D_MIX, D_MODEL), D_MIX ** -0.5),
        "norm2_g": 1.0 + nrm(ks[19], (L, D_MODEL), 0.02),
        "w_router": nrm(ks[20], (L, D_MODEL, N_EXPERTS), D_MODEL ** -0.5),
        "b_router": nrm(ks[21], (L, N_EXPERTS), 0.01),
        "w1": nrm(ks[22], (L, N_EXPERTS, D_MODEL, 2 * D_FF), D_MODEL ** -0.5),
        "b1": nrm(ks[23], (L, N_EXPERTS, 2 * D_FF), 0.02),
        "w2": nrm(ks[24], (L, N_EXPERTS, D_FF, D_MODEL), D_FF ** -0.5),
        "b2": nrm(ks[25], (L, N_EXPERTS, D_MODEL), 0.02),
    }


def reference(x, c, w_ada, b_ada, norm1_g, w_in, conv_w, conv_b, w_rg_a, b_rg_a, w_rg_x, b_rg_x,
              lru_lambda, q_norm_g, k_norm_g, kidx_norm_g, rg_out_g, attn_out_g, w_out, norm2_g,
              w_router, b_router, w1, b1, w2, b2):
    for l in range(DEPTH):
        x = hybrid_layer(x, c, w_ada[l], b_ada[l], norm1_g[l], w_in[l], conv_w[l], conv_b[l],
                         w_rg_a[l], b_rg_a[l], w_rg_x[l], b_rg_x[l], lru_lambda[l], q_norm_g[l],
                         k_norm_g[l], kidx_norm_g[l], rg_out_g[l], attn_out_g[l], w_out[l], norm2_g[l],
                         w_router[l], b_router[l], w1[l], b1[l], w2[l], b2[l])
    return x
```

```python
import numpy as np
import concourse.bass as bass
import concourse.mybir as mybir
from concourse.bass_utils import run_bass_kernel_spmd

F32 = mybir.dt.float32
BF16 = mybir.dt.bfloat16
I32 = mybir.dt.int32
U32 = mybir.dt.uint32
U8 = mybir.dt.uint8
ALU = mybir.AluOpType
AF = mybir.ActivationFunctionType
AX = mybir.AxisListType
DSZ = {F32: 4, BF16: 2, I32: 4, U32: 4, U8: 1}


class Tok:
    __slots__ = ("name", "lw", "rd", "excl")

    def __init__(self, name, excl=False):
        self.name = name
        self.lw = None
        self.rd = []
        self.excl = excl


class _Op:
    __slots__ = ("eng", "fn", "deps", "idx", "dma", "sem", "val", "used", "slot_prev")

    def __init__(self, eng, fn, deps, idx, dma):
        self.eng = eng
        self.fn = fn
        self.deps = deps
        self.idx = idx
        self.dma = dma
        self.sem = None
        self.val = 0
        self.used = False
        self.slot_prev = None


class Prog:
    ENGS = ("pe", "act", "dve", "pool", "sp")
    NSLOT = 12
    SEG = 20000

    def __init__(self, nc):
        self.nc = nc
        self.ops = []
        self.by_eng = {e: [] for e in self.ENGS}
        self.pending_barrier = {e: [] for e in self.ENGS}
        self.recent_dma = {e: [] for e in self.ENGS}

    def add(self, eng, fn, rd=(), wr=(), dma=False):
        ex = [t for t in rd if t.excl]
        if ex:
            rd = [t for t in rd if not t.excl]
            wr = list(wr) + ex
        deps = set()
        for t in rd:
            if t.lw is not None:
                deps.add(t.lw)
        for t in wr:
            if t.lw is not None:
                deps.add(t.lw)
            deps.update(t.rd)
        if self.pending_barrier[eng]:
            deps.update(self.pending_barrier[eng])
            self.pending_barrier[eng] = []
        idx = len(self.ops)
        op = _Op(eng, fn, sorted(deps), idx, dma)
        self.ops.append(op)
        self.by_eng[eng].append(op)
        for t in rd:
            t.rd.append(idx)
        for t in wr:
            t.lw = idx
            t.rd = []
        if dma:
            r = self.recent_dma[eng]
            r.append(idx)
            if len(r) > self.NSLOT:
                r.pop(0)
        return idx

    def barrier(self):
        last = []
        for e in self.ENGS:
            ops = self.by_eng[e]
            for o in reversed(ops):
                if not o.dma:
                    last.append(o.idx)
                    break
            last.extend(self.recent_dma[e])
        for e in self.ENGS:
            self.pending_barrier[e] = list(set(self.pending_barrier[e]) | set(last))

    def emit(self, final_wait_eng="sp"):
        nc = self.nc
        ops = self.ops
        self.barrier()
        fin = self.pending_barrier[final_wait_eng]
        for o in ops:
            for d in o.deps:
                ops[d].used = True
        for d in fin:
            ops[d].used = True
        nsem = 0
        plan = {}
        for e in self.ENGS:
            ncomp = sum(1 for o in self.by_eng[e] if (not o.dma) and o.used)
            nseg = (ncomp + self.SEG - 1) // self.SEG
            ndma = self.NSLOT if any(o.dma for o in self.by_eng[e]) else 0
            plan[e] = (nseg, ndma)
            nsem += nseg + ndma
        import contextlib

        with contextlib.ExitStack() as st:
            sems = [st.enter_context(nc.semaphore(f"s{i}")) for i in range(nsem)]
            si = 0
            for e in self.ENGS:
                nseg, ndma = plan[e]
                seg = sems[si:si + nseg]
                si += nseg
                slots = sems[si:si + ndma]
                si += ndma
                slot_cnt = [0] * ndma
                k = 0
                c = 0
                for o in self.by_eng[e]:
                    if o.dma:
                        s = k % ndma
                        k += 1
                        o.slot_prev = (slots[s], slot_cnt[s]) if slot_cnt[s] else None
                        slot_cnt[s] += 16
                        o.sem, o.val = slots[s], slot_cnt[s]
                    elif o.used:
                        o.sem = seg[c // self.SEG]
                        o.val = (c % self.SEG) + 1
                        c += 1
            block = st.enter_context(nc.Block())

            def run(eng_name):
                def body(e):
                    waited = {}

                    def wait(sem, val):
                        key = id(sem)
                        if waited.get(key, 0) >= val:
                            return
                        waited[key] = val
                        e.wait_ge(sem, val)

                    for o in self.by_eng[eng_name]:
                        for d in o.deps:
                            p = ops[d]
                            if p.eng == "pe" and eng_name == "pe" and not p.dma:
                                continue
                            wait(p.sem, p.val)
                        if o.slot_prev is not None:
                            wait(*o.slot_prev)
                        ins = o.fn(e)
                        if o.dma:
                            ins.then_inc(o.sem, 16)
                        elif o.used:
                            ins.then_inc(o.sem, 1)
                    if eng_name == final_wait_eng:
                        for d in fin:
                            wait(ops[d].sem, ops[d].val)
                return body

            block.tensor(run("pe"))
            block.scalar(run("act"))
            block.vector(run("dve"))
            block.gpsimd(run("pool"))
            block.sync(run("sp"))


class Arena:
    def __init__(self, nc, st, nbytes, name="arena"):
        self.t = st.enter_context(nc.sbuf_tensor(name, [128, nbytes], U8))
        self.nbytes = nbytes
        self.off = 0
        self.marks = []

    def alloc(self, shape, dtype, name=None):
        assert shape[0] <= 128
        free = int(np.prod(shape[1:]))
        nb = free * DSZ[dtype]
        nb_al = (nb + 63) // 64 * 64
        assert self.off + nb_al <= self.nbytes, (self.off, nb_al, self.nbytes, name)
        ap = self.t[0:shape[0], self.off:self.off + nb]
        self.off += nb_al
        if dtype != U8:
            ap = ap.bitcast(dtype)
        if len(shape) > 2:
            names = " ".join(f"d{i}" for i in range(1, len(shape)))
            kw = {f"d{i}": shape[i] for i in range(1, len(shape))}
            ap = ap.rearrange(f"p ({names}) -> p {names}", **kw)
        return ap

    def mark(self):
        self.marks.append(self.off)

    def release(self):
        self.off = self.marks.pop()

import contextlib

D = 1024
KC = 8
NE = 32
EPS = 1e-6
BLK = 512
NIT = 14
BIGC = float(2 ** 20)


class K:
    def __init__(self, P):
        self.P = P

    def mm(self, out, lhsT, rhs, start, stop, rd, wr):
        self.P.add("pe", lambda e: e.matmul(out, lhsT=lhsT, rhs=rhs, start=start, stop=stop,
                                            skip_group_check=True), rd, wr)

    def tr(self, out, in_, ident, rd, wr):
        self.P.add("pe", lambda e: e.transpose(out=out, in_=in_, identity=ident), rd, wr)

    def act(self, out, in_, func, rd, wr, scale=1.0, bias=0.0, accum=None, eng="act"):
        if accum is None:
            self.P.add(eng, lambda e: e.activation(out=out, in_=in_, func=func, scale=scale, bias=bias), rd, wr)
        else:
            self.P.add(eng, lambda e: e.activation(out=out, in_=in_, func=func, scale=scale, bias=bias,
                                                   accum_out=accum), rd, wr)

    def ts(self, eng, out, in0, s1, s2, op0, op1, rd, wr, accum=None):
        if op1 is None:
            self.P.add(eng, lambda e: e.tensor_scalar(out=out, in0=in0, scalar1=s1, scalar2=None, op0=op0), rd, wr)
        elif accum is None:
            self.P.add(eng, lambda e: e.tensor_scalar(out=out, in0=in0, scalar1=s1, scalar2=s2, op0=op0, op1=op1), rd, wr)
        else:
            self.P.add(eng, lambda e: e.tensor_scalar(out=out, in0=in0, scalar1=s1, scalar2=s2, op0=op0, op1=op1,
                                                      accum_out=accum), rd, wr)

    def tt(self, eng, out, in0, in1, op, rd, wr):
        self.P.add(eng, lambda e: e.tensor_tensor(out=out, in0=in0, in1=in1, op=op), rd, wr)

    def stt(self, out, in0, scalar, in1, op0, op1, rd, wr, accum=None):
        if accum is None:
            self.P.add("dve", lambda e: e.scalar_tensor_tensor(out=out, in0=in0, scalar=scalar, in1=in1,
                                                              op0=op0, op1=op1), rd, wr)
        else:
            self.P.add("dve", lambda e: e.scalar_tensor_tensor(out=out, in0=in0, scalar=scalar, in1=in1,
                                                              op0=op0, op1=op1, accum_out=accum), rd, wr)

    def cp(self, eng, out, in_, rd, wr):
        if eng == "act":
            self.P.add("act", lambda e: e.activation(out=out, in_=in_, func=AF.Copy), rd, wr)
        else:
            self.P.add(eng, lambda e: e.tensor_copy(out=out, in_=in_), rd, wr)

    def memset(self, eng, ap, val, wr):
        self.P.add(eng, lambda e: e.memset(ap, val), (), wr)

    def dma(self, q, out, in_, rd, wr):
        self.P.add(q, lambda e: e.dma_start(out=out, in_=in_), rd, wr, dma=True)

    def gather(self, out, in_, idx, rd, wr, bounds=None):
        if bounds is None:
            self.P.add("pool", lambda e: e.indirect_dma_start(
                out=out, out_offset=None, in_=in_,
                in_offset=bass.IndirectOffsetOnAxis(ap=idx, axis=0)), rd, wr, dma=True)
        else:
            self.P.add("pool", lambda e: e.indirect_dma_start(
                out=out, out_offset=None, in_=in_,
                in_offset=bass.IndirectOffsetOnAxis(ap=idx, axis=0),
                bounds_check=bounds, oob_is_err=False), rd, wr, dma=True)

    def scatter(self, out, in_, idx, rd, wr):
        self.P.add("pool", lambda e: e.indirect_dma_start(
            out=out, out_offset=bass.IndirectOffsetOnAxis(ap=idx, axis=0),
            in_=in_, in_offset=None), rd, wr, dma=True)

    def recip(self, out, in_, rd, wr):
        self.P.add("dve", lambda e: e.reciprocal(out=out, in_=in_), rd, wr)

    def iota(self, out, pattern, base, cm, wr):
        self.P.add("pool", lambda e: e.iota(out, pattern=pattern, base=base, channel_multiplier=cm,
                                            allow_small_or_imprecise_dtypes=True), (), wr)


def build_nc(S, stages=5, debug=False):
    NT = S // 128
    NG = S // 512
    NSEL = min(256, S // 4)
    NSLOT = 4 * S + NE * BLK
    NBLK = NSLOT // BLK
    JMAX = S // BLK

    nc = bass.Bass("TRN2", target_bir_lowering=False)

    def din(name, shape, dt=F32):
        return nc.dram_tensor(name, list(shape), dt, kind="ExternalInput").ap()

    def dscr(name, shape, dt):
        kind = "ExternalOutput" if debug else "Internal"
        return nc.dram_tensor(name, list(shape), dt, kind=kind).ap()

    x_d = din("x", [S, D])
    c_d = din("c_fm", [128, KC])
    w_ada_d = din("w_ada", [D, 6 * D])
    b_ada_d = din("b_ada_fm", [128, 48])
    n1g_d = din("n1g_fm", [128, KC])
    n2g_d = din("n2g_fm", [128, KC])
    w_in_d = din("w_in", [D, 3144])
    convw_d = din("convw_fm", [128, 16])
    vec4_d = din("vec4_fm", [128, 28])
    wabd_d = din("wabd", [128, 4 * 128])
    wxbd_d = din("wxbd", [128, 4 * 128])
    w_out_d = din("w_out", [D, D])
    w_rt_d = din("w_rt_fm", [128, KC * NE])
    b_rt_d = din("b_rt", [1, NE])
    if stages >= 4:
        w1_d = din("w1", [NE, D, 2 * D])
        b1_d = din("b1", [NE, 2 * D])
        w2_d = din("w2", [NE, D, D])
        b2_d = din("b2", [NE, D])
    import os as _os
    CUT = int(_os.environ.get("P1CUT", "99"))
    out_d = nc.dram_tensor("out", [S, D], F32, kind="ExternalOutput").ap()

    MODROW = dscr("modrow", [64, 128], F32)
    QT = dscr("qt", [4, 128, S], BF16)
    KV = dscr("kv", [NT, 128, 1032], BF16)
    QIT = dscr("qit", [4, 128, S], BF16)
    KIT = dscr("kit", [128, S], BF16)
    WI = dscr("wi", [NT, 128, 8], F32)
    YR = dscr("yr", [4, 128, S], BF16)
    YA = dscr("ya", [4, 128, S], BF16)
    X1 = dscr("x1", [S, D], F32)
    H2 = dscr("h2", [S, D], BF16)
    HG = dscr("hg", [NSLOT, D], BF16)
    YY = dscr("yy", [NSLOT, D], F32)
    RTD = dscr("rtd", [128, NT * 8], F32)

    P = Prog(nc)
    k = K(P)
    T = Tok
    st = contextlib.ExitStack()
    with st:
        A = Arena(nc, st, 206 * 1024)
        psT = [st.enter_context(nc.psum_tensor(f"ps{i}", [128, 1024], F32)) for i in range(4)]
        PSH = [[psT[i][:, 0:512], psT[i][:, 512:1024]] for i in range(4)]
        t_psh = [[Tok(f"ps{i}a", True), Tok(f"ps{i}b", True)] for i in range(4)]
        t_dram = {n_: T(n_) for n_ in ("QT", "KV", "QIT", "KIT", "WI", "YR", "YA", "X1", "H2", "HG", "YY",
                                      "MODROW", "RTD", "OUT")}

        io = A.alloc([128, 128], F32); t_io = T("io")
        identf = A.alloc([128, 128], F32); t_idf = T("identf")
        ident = A.alloc([128, 128], BF16); t_id = T("ident")
        ones_b = A.alloc([128, 512], BF16); t_ones = T("ones")
        bo64 = A.alloc([128, 128], BF16); t_bo = T("bo64")
        ustr = A.alloc([128, 128], BF16); t_ustr = T("ustr")
        modfm = A.alloc([128, 64], F32); t_mod = T("modfm")
        vec4 = A.alloc([128, 28], F32); t_vec4 = T("vec4")
        convw = A.alloc([128, 16], F32); t_convw = T("convw")
        nsp = A.alloc([128, 4], F32); t_nsp = T("nsp")
        pidx = A.alloc([128, 1], F32); t_pidx = T("pidx")
        dest4 = A.alloc([128, NT, 4], I32); t_dest4 = T("dest4")
        gate4 = A.alloc([128, NT, 4], F32); t_gate4 = T("gate4")
        widx = A.alloc([128, NBLK, 8], I32); t_widx = T("widx")
        bidx = A.alloc([128, NBLK], I32); t_bidx = T("bidx")

        k.iota(io[:], [[1, 128]], 0, -1, [t_io])
        k.ts("dve", identf[:], io[:], 0.0, None, ALU.is_equal, None, [t_io], [t_idf])
        k.cp("dve", ident[:], identf[:], [t_idf], [t_id])
        k.ts("dve", ustr[:], io[:], 0.0, None, ALU.is_gt, None, [t_io], [t_ustr])
        k.iota(pidx[:], [[0, 1]], 0, 1, [t_pidx])
        k.memset("pool", ones_b[:], 1.0, [t_ones])
        k.memset("pool", bo64[:], 0.0, [t_bo])
        k.memset("pool", bo64[0:64, 0:64], 1.0, [t_bo])
        k.memset("pool", bo64[64:128, 64:128], 1.0, [t_bo])
        k.dma("sp", vec4[:], vec4_d[:, :], [], [t_vec4])
        k.dma("sp", convw[:], convw_d[:, :], [], [t_convw])
        CB, BA, BX, LAM, RGG, AOG = 0, 4, 8, 12, 16, 20
        QG, KG, KIG = 24, 25, 26

        A.mark()
        csb = A.alloc([128, KC], F32); t_c = T("c")
        scs = A.alloc([128, KC], F32); t_scs = T("scs")
        bada = A.alloc([128, 48], F32); t_bada = T("bada")
        n1g = A.alloc([128, KC], F32); t_n1g = T("n1g")
        n2g = A.alloc([128, KC], F32); t_n2g = T("n2g")
        wa_buf = [A.alloc([128, KC, 768], F32) for _ in range(2)]
        t_wa = [T("wa0"), T("wa1")]
        modT = A.alloc([64, 128], F32); t_modT = T("modT")
        tmp4 = A.alloc([128, 4], F32); t_tmp4 = T("tmp4")
        k.dma("sp", csb[:], c_d[:, :], [], [t_c])
        k.dma("sp", bada[:], b_ada_d[:, :], [], [t_bada])
        k.dma("sp", n1g[:], n1g_d[:, :], [], [t_n1g])
        k.dma("sp", n2g[:], n2g_d[:, :], [], [t_n2g])
        k.act(scs[:], csb[:], AF.Silu, [t_c], [t_scs])
        pmod = PSH[0][0]
        for jg in range(8):
            wb_ = wa_buf[jg % 2]
            k.dma("sp", wb_[:], w_ada_d[:, jg * 768:(jg + 1) * 768].rearrange("(kc p) n -> p kc n", p=128),
                  [], [t_wa[jg % 2]])
            for jj in range(6):
                j = jg * 6 + jj
                for kc in range(KC):
                    k.mm(pmod[:, j:j + 1], wb_[:, kc, jj * 128:(jj + 1) * 128], scs[:, kc:kc + 1],
                         kc == 0, kc == KC - 1, [t_wa[jg % 2], t_scs], [t_psh[0][0]])
        k.tt("dve", modfm[:, 0:48], pmod[:, 0:48], bada[:], ALU.add, [t_psh[0][0], t_bada], [t_mod])
        k.stt(modfm[:, 48:56], modfm[:, 8:16], 1.0, n1g[:], ALU.add, ALU.mult, [t_mod, t_n1g], [t_mod])
        k.stt(modfm[:, 56:64], modfm[:, 32:40], 1.0, n2g[:], ALU.add, ALU.mult, [t_mod, t_n2g], [t_mod])
        pTm = PSH[0][1]
        k.tr(pTm[0:64, 0:128], modfm[:, 0:64], identf[:], [t_mod, t_idf], [t_psh[0][1]])
        k.cp("dve", modT[:], pTm[0:64, 0:128], [t_psh[0][1]], [t_modT])
        k.dma("sp", MODROW[:, :], modT[:], [t_modT], [t_dram["MODROW"]])
        mr = MODROW.rearrange("j p -> (j p)")

        def bc_row(dst, j0, tok):
            src = mr[j0 * 128:(j0 + 8) * 128].rearrange("(o n) -> o n", o=1).partition_broadcast(128)
            k.dma("sp", dst[:], src, [t_dram["MODROW"]], [tok])

        k.act(tmp4[:], vec4[:, LAM:LAM + 4], AF.Exp, [t_vec4], [t_tmp4], scale=-1.0)
        k.act(nsp[:], tmp4[:], AF.Ln, [t_tmp4], [t_nsp], bias=1.0)
        k.ts("dve", nsp[:], nsp[:], -8.0, None, ALU.mult, None, [t_nsp], [t_nsp])
        P.barrier()
        A.release()

        if stages == 0:
            A.mark()
            zt2 = A.alloc([128, D], F32); t_zt2 = T("zt2")
            k.memset("pool", zt2[:], 0.0, [t_zt2])
            k.dma("sp", out_d[0:128, :], zt2[:], [t_zt2], [t_dram["OUT"]])
            P.emit()
            return nc
        A.mark()
        cv_ld = [A.alloc([128, 2048], F32) for _ in range(3)]
        t_cvld = [T(f"cvld{i}") for i in range(3)]

        A.mark()
        w_in_sb = A.alloc([128, KC, 3208], BF16); t_win = T("w_in")
        wabd = A.alloc([128, 4, 128], BF16); t_wabd = T("wabd")
        wxbd = A.alloc([128, 4, 128], BF16); t_wxbd = T("wxbd")
        n = 0
        for kc in range(KC):
            sg = cv_ld[n % 3]; tg = t_cvld[n % 3]
            k.dma("sp", sg[:, 0:2048], w_in_d[kc * 128:(kc + 1) * 128, 0:2048], [], [tg])
            k.cp(["dve", "act", "pool"][n % 3], w_in_sb[:, kc, 0:2048], sg[:, 0:2048], [tg], [t_win])
            n += 1
            sg = cv_ld[n % 3]; tg = t_cvld[n % 3]
            k.dma("sp", sg[:, 0:1096], w_in_d[kc * 128:(kc + 1) * 128, 2048:3144], [], [tg])
            k.cp(["dve", "act", "pool"][n % 3], w_in_sb[:, kc, 2048:3136], sg[:, 0:1088], [tg], [t_win])
            k.cp("pool", w_in_sb[:, kc, 3136:3200], sg[:, 1024:1088], [tg], [t_win])
            k.cp("pool", w_in_sb[:, kc, 3200:3208], sg[:, 1088:1096], [tg], [t_win])
            n += 1
        for (dst, src, tk) in ((wabd, wabd_d, t_wabd), (wxbd, wxbd_d, t_wxbd)):
            sg = cv_ld[n % 3]; tg = t_cvld[n % 3]
            k.dma("sp", sg[:, 0:512], src[:, :], [], [tg])
            k.cp("dve", dst[:].rearrange("p c m -> p (c m)"), sg[:, 0:512], [tg], [tk])
            n += 1

        xt = [A.alloc([128, D], F32) for _ in range(2)]; t_xt = [T("xt0"), T("xt1")]
        xn = [A.alloc([128, D], BF16) for _ in range(2)]; t_xn = [T("xn0"), T("xn1")]
        junkb = A.alloc([128, D], BF16); t_junkb = T("junkb")
        st1 = A.alloc([128, 8], F32); t_st1 = T("st1")
        hT = [A.alloc([128, KC, 512], BF16) for _ in range(2)]; t_hT = [T("hT0"), T("hT1")]
        vsb = [A.alloc([128, 8, 65], BF16) for _ in range(2)]; t_vsb = [T("vsb0"), T("vsb1")]
        wisb = [A.alloc([128, 8], F32) for _ in range(2)]; t_wisb = [T("wisb0"), T("wisb1")]
        xr_ext = A.alloc([128, 4, 516], F32); t_xre = [T(f"xre{c}") for c in range(4)]
        carry = A.alloc([128, 4], F32); t_carry = T("carry")
        xtail = A.alloc([128, 4, 4], F32); t_xtail = T("xtail")
        NW = 8
        wf = [A.alloc([128, 512], F32) for _ in range(NW)]; t_wf = [T(f"wf{i}") for i in range(NW)]
        NWB = 6
        wb = [A.alloc([128, 512], BF16) for _ in range(NWB)]; t_wb = [T(f"wb{i}") for i in range(NWB)]
        yv = A.alloc([128, 4, 512], F32); t_yv = [T(f"yv{i}") for i in range(4)]
        xc_all = A.alloc([128, 4, 512], F32); t_xc = [T(f"xc{i}") for i in range(4)]
        xcb_all = A.alloc([128, 4, 512], BF16); t_xcb = [T(f"xcb{i}") for i in range(4)]
        ysq = A.alloc([128, 4, 512], BF16); t_ysq = [T(f"ysq{i}") for i in range(4)]
        for b in range(2):
            k.memset("pool", vsb[b][:], 1.0, [t_vsb[b]])
        k.memset("pool", xr_ext[:], 0.0, t_xre)
        k.memset("pool", carry[:], 0.0, [t_carry])
        wfi = [0]; wbi = [0]; zi = [0]

        def new_wf():
            i = wfi[0] % NW; wfi[0] += 1
            return wf[i], t_wf[i]

        def new_wb():
            i = wbi[0] % NWB; wbi[0] += 1
            return wb[i], t_wb[i]

        zbufs = [(PSH[1][0], t_psh[1][0]), (PSH[1][1], t_psh[1][1]), (PSH[2][0], t_psh[2][0])]

        def new_z():
            i = zi[0] % 3; zi[0] += 1
            return zbufs[i]

        ps_wi, t_pswi = PSH[2][1], t_psh[2][1]
        ps_ss, t_psss = PSH[3][0], t_psh[3][0]
        ps_v, t_psv = PSH[3][1], t_psh[3][1]
        gm1 = modfm[:, 48:56]
        sh1 = modfm[:, 0:8]

        for G in range(NG):
            h_T = hT[G % 2]; th = t_hT[G % 2]
            for j in range(4):
                i = 4 * G + j
                x_t = xt[i % 2]; tx = t_xt[i % 2]
                x_n = xn[i % 2]; txn = t_xn[i % 2]
                k.dma("sp", x_t[:], x_d[i * 128:(i + 1) * 128, :], [], [tx])
                k.act(junkb[:], x_t[:], AF.Square, [tx], [t_junkb, t_st1], accum=st1[:, 0:1])
                k.act(st1[:, 1:2], st1[:, 0:1], AF.Sqrt, [t_st1], [t_st1], scale=1.0 / D, bias=EPS)
                k.recip(st1[:, 2:3], st1[:, 1:2], [t_st1], [t_st1])
                k.act(x_n[:], x_t[:], AF.Copy, [tx, t_st1], [txn], scale=st1[:, 2:3])
                pt = psT[0][:, (i % 2) * 512:(i % 2 + 1) * 512].bitcast(BF16)
                tpt = t_psh[0][i % 2]
                for kc in range(KC):
                    k.tr(pt[:, kc * 128:(kc + 1) * 128], x_n[:, kc * 128:(kc + 1) * 128], ident[:],
                         [txn, t_id], [tpt])
                for kc in range(KC):
                    dst = h_T[:, kc, j * 128:(j + 1) * 128]
                    src = pt[:, kc * 128:(kc + 1) * 128]
                    if kc % 2 == 0:
                        k.act(dst, src, AF.Identity, [tpt, t_mod], [th], scale=gm1[:, kc:kc + 1],
                              bias=sh1[:, kc:kc + 1])
                    else:
                        k.ts("dve", dst, src, gm1[:, kc:kc + 1], sh1[:, kc:kc + 1], ALU.mult, ALU.add,
                             [tpt, t_mod], [th])
                if CUT < 2:
                    continue
                for kc in range(KC):
                    k.mm(ps_v[:, 0:512], h_T[:, kc, j * 128:(j + 1) * 128], w_in_sb[:, kc, 2048:2560],
                         kc == 0, kc == KC - 1, [th, t_win], [t_psv])
                for kc in range(KC):
                    k.mm(ps_wi[:, 0:8], h_T[:, kc, j * 128:(j + 1) * 128], w_in_sb[:, kc, 3200:3208],
                         kc == 0, kc == KC - 1, [th, t_win], [t_pswi])
                vb = vsb[i % 2]; tvb = t_vsb[i % 2]
                k.cp("act", vb[:, :, 0:64], ps_v[:, 0:512].rearrange("p (h d) -> p h d", d=64), [t_psv], [tvb])
                k.dma("pool", KV[i, :, 512:1032], vb[:].rearrange("p h d -> p (h d)"), [tvb], [t_dram["KV"]])
                wbt = wisb[i % 2]; twb = t_wisb[i % 2]
                k.cp("dve", wbt[:], ps_wi[:, 0:8], [t_pswi], [twb])
                k.dma("pool", WI[i], wbt[:], [twb], [t_dram["WI"]])

            if CUT < 3:
                continue
            cols = slice(G * 512, (G + 1) * 512)

            def zchunk(col0, M=128):
                pz, tz = new_z()
                for kc in range(KC):
                    k.mm(pz[0:M, :], w_in_sb[:, kc, col0:col0 + M], h_T[:, kc, :], kc == 0, kc == KC - 1,
                         [th, t_win], [tz])
                return pz, tz

            rnn_state = []
            for c in range(4):
                pz, tz = zchunk(c * 128)
                if G > 0:
                    k.cp("pool", xr_ext[:, c, 0:3], xtail[:, c, 0:3], [t_xtail], [t_xre[c]])
                k.cp("act", xr_ext[:, c, 3:515], pz[:, :], [tz], [t_xre[c]])
                t0, tt0 = xc_all[:, c, :], t_xc[c]
                txre = t_xre[c]
                k.ts("dve", t0[:], xr_ext[:, c, 0:512], convw[:, c * 4:c * 4 + 1], vec4[:, CB + c:CB + c + 1],
                     ALU.mult, ALU.add, [txre, t_convw, t_vec4], [tt0])
                for tap in (1, 2, 3):
                    k.stt(t0[:], xr_ext[:, c, tap:tap + 512], convw[:, c * 4 + tap:c * 4 + tap + 1], t0[:],
                          ALU.mult, ALU.add, [txre, t_convw, tt0], [tt0])
                k.cp("pool", xtail[:, c, 0:3], xr_ext[:, c, 512:515], [txre], [t_xtail])
                xcb, txcb = xcb_all[:, c, :], t_xcb[c]
                k.cp("pool", xcb, t0, [tt0], [txcb])
                rnn_state.append((t0, tt0, xcb, txcb))
            if CUT < 4:
                continue
            for c in range(4):
                pz, tz = zchunk(512 + c * 128)
                k.act(yv[:, c, :], pz[:, :], AF.Gelu_apprx_tanh, [tz], [t_yv[c]])
            if CUT < 5:
                continue
            for (base_col, gcol, dst, tkd) in ((1024, QG, QT, t_dram["QT"]), (1536, KG, None, t_dram["KV"])):
                for c in range(4):
                    pz, tz = zchunk(base_col + c * 128)
                    sq, tsq = new_wb()
                    k.act(sq[:], pz[:, :], AF.Square, [tz], [tsq])
                    qf, tqf = new_wf()
                    k.ts("dve", qf[:], pz[:, :], vec4[:, gcol:gcol + 1], None, ALU.mult, None, [tz, t_vec4], [tqf])
                    k.mm(ps_ss[:, :], bo64[:], sq[:], True, True, [t_bo, tsq], [t_psss])
                    rs, trs = new_wf()
                    k.act(rs[:], ps_ss[:, :], AF.Sqrt, [t_psss], [trs], scale=1.0 / 64, bias=EPS)
                    k.recip(rs[:], rs[:], [trs], [trs])
                    qn, tqn = new_wb()
                    k.tt("dve", qn[:], qf[:], rs[:], ALU.mult, [tqf, trs], [tqn])
                    if dst is not None:
                        k.dma("pool", dst[c, :, cols], qn[:], [tqn], [tkd])
                    else:
                        k.dma("pool", KV[4 * G:4 * G + 4, :, c * 128:(c + 1) * 128].rearrange("t p s -> p t s"),
                              qn[:].rearrange("p (t s) -> p t s", t=4), [tqn], [tkd])
            if CUT < 6:
                continue
            for c in range(4 * int(_os.environ.get("DUP", "1"))):
                c = c % 4
                pz, tz = zchunk(2560 + c * 128)
                qn, tqn = new_wb()
                k.cp("act", qn[:], pz[:, :], [tz], [tqn])
                k.dma("pool", QIT[c, :, cols], qn[:], [tqn], [t_dram["QIT"]])
            for _d in range(int(_os.environ.get("DVEDUP", "0"))):
                qf, tqf = new_wf()
                k.ts(_os.environ.get("DUPENG", "dve"), qf[:], xc_all[:, 0, :], 2.0, None, ALU.mult, None, [t_xc[0]], [tqf])
            if CUT < 7:
                continue
            SK = set(_os.environ.get("SKIP", "").split(","))
            pz, tz = zchunk(int(_os.environ.get("KICOL", "3072")))
            sq, tsq = new_wb()
            if "a" not in SK:
                k.act(sq[:], pz[:, :], AF.Square, [tz], [tsq])
            qf, tqf = new_wf()
            if "b" not in SK:
                k.ts("dve", qf[:], pz[:, :], vec4[:, KIG:KIG + 1], None, ALU.mult, None, [tz, t_vec4], [tqf])
            if "c" not in SK:
                k.mm(ps_ss[:, :], bo64[:], sq[:], True, True, [t_bo, tsq], [t_psss])
            rs, trs = new_wf()
            if "d" not in SK:
                k.act(rs[:], ps_ss[:, :], AF.Sqrt, [t_psss], [trs], scale=1.0 / 64, bias=EPS)
            if "e" not in SK:
                k.recip(rs[:], rs[:], [trs], [trs])
            qn, tqn = new_wb()
            if "f" not in SK:
                k.tt("dve", qn[:], qf[:], rs[:], ALU.mult, [tqf, trs], [tqn])
            if "g" not in SK:
                k.dma("sp", KIT[:, cols], qn[:], [tqn], [t_dram["KIT"]])
            if CUT < 8:
                continue
            for c in range(4):
                xc, txc, xcb, txcb = rnn_state[c]
                pa, tpa = new_z()
                k.mm(pa[:, :], wabd[:, c, :], xcb[:], True, True, [t_wabd, txcb], [tpa])
                r, tr_ = new_wf()
                k.act(r[:], pa[:, :], AF.Sigmoid, [tpa, t_vec4], [tr_], bias=vec4[:, BA + c:BA + c + 1])
                px, tpx = new_z()
                k.mm(px[:, :], wxbd[:, c, :], xcb[:], True, True, [t_wxbd, txcb], [tpx])
                ig, tig = new_wf()
                k.act(ig[:], px[:, :], AF.Sigmoid, [tpx, t_vec4], [tig], bias=vec4[:, BX + c:BX + c + 1])
                a, ta = new_wf()
                k.act(a[:], r[:], AF.Exp, [tr_, t_nsp], [ta], scale=nsp[:, c:c + 1])
                k.tt("pool", r[:], a[:], a[:], ALU.mult, [ta], [tr_])
                k.act(r[:], r[:], AF.Sqrt, [tr_], [tr_], scale=-1.0, bias=1.0)
                k.tt("dve", ig[:], ig[:], r[:], ALU.mult, [tig, tr_], [tig])
                k.tt("dve", ig[:], ig[:], xc[:], ALU.mult, [tig, txc], [tig])
                hsc, thsc = new_wf()
                P.add("dve", (lambda o_, a_, u_, i_: (lambda e: e.tensor_tensor_scan(
                    out=o_, data0=a_, data1=u_, initial=i_, op0=ALU.mult, op1=ALU.add)))(
                        hsc[:], a[:], ig[:], carry[:, c:c + 1]), [ta, tig, t_carry], [thsc])
                k.cp("dve", carry[:, c:c + 1], hsc[:, 511:512], [thsc], [t_carry])
                k.tt("dve", yv[:, c, :], yv[:, c, :], hsc[:], ALU.mult, [t_yv[c], thsc], [t_yv[c]])
                k.act(ysq[:, c, :], yv[:, c, :], AF.Square, [t_yv[c]], [t_ysq[c]])
            if CUT < 9:
                continue
            for c in range(4):
                k.mm(ps_ss[:, :], ones_b[:, 0:128], ysq[:, c, :], c == 0, c == 3, [t_ones, t_ysq[c]], [t_psss])
            rs, trs = new_wf()
            k.act(rs[:], ps_ss[:, :], AF.Sqrt, [t_psss], [trs], scale=1.0 / 512, bias=EPS)
            k.recip(rs[:], rs[:], [trs], [trs])
            for c in range(4):
                yn, tyn = new_wb()
                k.stt(yn[:], yv[:, c, :], vec4[:, RGG + c:RGG + c + 1], rs[:], ALU.mult, ALU.mult,
                      [t_yv[c], t_vec4, trs], [tyn])
                k.dma("pool", YR[c, :, cols], yn[:], [tyn], [t_dram["YR"]])
        P.barrier()
        A.release()
        A.release()

        if stages >= 2:
            A.mark()
            kiT = A.alloc([128, S], BF16); t_kiT = T("kiT")
            score = [A.alloc([128, S], F32) for _ in range(2)]; t_score = [T("score0"), T("score1")]
            negm = [A.alloc([128, S], BF16) for _ in range(3)]; t_negm = [T("negm0"), T("negm1"), T("negm2")]
            junk8 = A.alloc([128, S], U8)
            I4 = A.alloc([128, 512], BF16); t_I4 = T("I4")
            zb = A.alloc([128, 260], BF16); t_zb = T("zb")
            pow2 = A.alloc([128, NIT + 1], F32); t_pow2 = T("pow2")
            qT_t = [A.alloc([128, 4, 128], BF16) for _ in range(2)]; t_qT = [T("qT0"), T("qT1")]
            qiT_t = [A.alloc([128, 4, 128], BF16) for _ in range(3)]; t_qiT = [T("qiT0"), T("qiT1"), T("qiT2")]
            wi_t = [A.alloc([128, 8], F32) for _ in range(3)]; t_wi = [T("wi0"), T("wi1"), T("wi2")]
            NTR = 4
            trelu = [A.alloc([128, 512], F32) for _ in range(NTR)]; t_trelu = [T(f"trelu{i}") for i in range(NTR)]
            pm = [A.alloc([128, 1024], BF16) for _ in range(2)]; t_pm = [T("pm0"), T("pm1")]
            NKV = 4
            kv = [A.alloc([128, 1032], BF16) for _ in range(NKV)]; t_kv = [T(f"kv{i}") for i in range(NKV)]
            bis = [A.alloc([128, 8], F32) for _ in range(2)]; t_bis = [T("bis0"), T("bis1")]
            dall = [A.alloc([128, NIT + 1], F32) for _ in range(2)]; t_dall = [T("dall0"), T("dall1")]
            yaf = A.alloc([128, 8, 64], F32); t_yaf = T("yaf")
            yab = A.alloc([128, 512], BF16); t_yab = T("yab")
            yaT = A.alloc([128, 4, 128], BF16); t_yaT = T("yaT")
            fin = A.alloc([128, 16], F32); t_fin = T("fin")

            k.dma("sp", kiT[:, :], KIT[:, :], [t_dram["KIT"]], [t_kiT])
            for r4 in range(4):
                k.cp("pool", I4[:, r4 * 128:(r4 + 1) * 128], ident[:], [t_id], [t_I4])
            k.memset("pool", zb[:], 0.0, [t_zb])
            for it in range(NIT + 1):
                k.memset("pool", pow2[:, it:it + 1], float(2.0 ** (-it)), [t_pow2])

            ps_i = [PSH[3][0], PSH[3][1]]; t_psi = [t_psh[3][0], t_psh[3][1]]
            psl = [psT[0], psT[1]]
            pso = [PSH[2][0], PSH[2][1]]; t_pso = [t_psh[2][0], t_psh[2][1]]
            cnt_i = [0]; cnt_v = [0]

            def A_pieces(qt):
                L = (qt + 1) * 128
                b = qt % 3
                s2 = qt % 2
                sc_ = score[s2]; tsc = t_score[s2]
                bs = bis[s2]; tbs = t_bis[s2]
                dl = dall[s2]; tdl = t_dall[s2]
                pcs = []

                def ld():
                    k.dma("sp", qiT_t[b][:], QIT.rearrange("c p s -> p c s")[:, :, qt * 128:(qt + 1) * 128],
                          [t_dram["QIT"]], [t_qiT[b]])
                    k.dma("sp", wi_t[b][:], WI[qt], [t_dram["WI"]], [t_wi[b]])
                pcs.append(ld)
                items = [(g, min(512, L - g * 512), h) for g in range((L + 511) // 512) for h in range(8)]
                LAG = 2
                used_tr = {}

                def mk(idx):
                    def pc():
                        if idx < len(items):
                            g, n_, h = items[idx]
                            c = h // 2; base = (h % 2) * 64
                            ib = cnt_i[0] % 2; itr = cnt_i[0] % NTR; cnt_i[0] += 1
                            used_tr[idx] = itr
                            k.mm(ps_i[ib][:, 0:n_], qiT_t[b][base:base + 64, c, :],
                                 kiT[base:base + 64, g * 512:g * 512 + n_], True, True,
                                 [t_qiT[b], t_kiT], [t_psi[ib]])
                            k.act(trelu[itr][:, 0:n_], ps_i[ib][:, 0:n_], AF.Relu, [t_psi[ib]], [t_trelu[itr]])
                        j = idx - LAG
                        if j >= 0:
                            g, n_, h = items[j]
                            itr = used_tr[j]
                            sc = sc_[:, g * 512:g * 512 + n_]
                            if h == 0:
                                k.ts("dve", sc, trelu[itr][:, 0:n_], wi_t[b][:, 0:1], None, ALU.mult, None,
                                     [t_trelu[itr], t_wi[b]], [tsc])
                            else:
                                k.stt(sc, trelu[itr][:, 0:n_], wi_t[b][:, h:h + 1], sc, ALU.mult, ALU.add,
                                      [t_trelu[itr], t_wi[b], tsc], [tsc])
                    return pc
                for idx in range(len(items) + LAG):
                    pcs.append(mk(idx))

                def prep():
                    if L > NSEL:
                        P.add("dve", lambda e: e.tensor_reduce(out=bs[:, 0:1], in_=sc_[:, 0:L], axis=AX.X,
                                                               op=ALU.max, apply_absolute_value=True),
                              [tsc], [tbs])
                        k.ts("dve", dl[:], pow2[:], bs[:, 0:1], None, ALU.mult, None, [t_pow2, tbs], [tdl])
                        k.memset("dve", bs[:, 1:2], 0.0, [tbs])
                        k.memset("dve", bs[:, 5:6], 0.0, [tbs])
                    else:
                        k.memset("dve", bs[:, 4:5], -1e29, [tbs])
                    k.memset("dve", sc_[0:64, L - 64:L], -1e30, [tsc])
                pcs.append(prep)
                nsplit = len(pcs)
                use_act = (qt % 2 == 1)
                if L > NSEL:
                    for it in range(NIT):
                        def pc(it=it):
                            if not use_act:
                                k.ts("dve", junk8[:, 0:L], sc_[:, 0:L], bs[:, 1:2], None, ALU.is_gt, ALU.add,
                                     [tsc, tbs], [tbs], accum=bs[:, 2:3])
                                k.ts("dve", bs[:, 3:4], bs[:, 2:3], float(NSEL), -0.5, ALU.is_ge, ALU.add,
                                     [tbs], [tbs])
                            else:
                                k.act(negm[b][:, 0:L], sc_[:, 0:L], AF.Sign, [tsc, tbs], [t_negm[b], tbs],
                                      bias=bs[:, 5:6], accum=bs[:, 2:3])
                                k.ts("dve", bs[:, 3:4], bs[:, 2:3], float(2 * NSEL - L), -0.5, ALU.is_ge, ALU.add,
                                     [tbs], [tbs])
                            k.stt(bs[:, 1:2], bs[:, 3:4], dl[:, it:it + 1], bs[:, 1:2], ALU.mult, ALU.add,
                                  [tbs, tdl], [tbs])
                            if use_act:
                                k.ts("dve", bs[:, 5:6], bs[:, 1:2], -1.0, None, ALU.mult, None, [tbs], [tbs])
                        pcs.append(pc)
                    nsplit += NIT // 4

                def fin_():
                    if L > NSEL:
                        k.tt("dve", bs[:, 4:5], bs[:, 1:2], dl[:, NIT:NIT + 1], ALU.subtract,
                             [tbs, tdl], [tbs])
                    k.ts("dve", negm[b][:, 0:L], sc_[:, 0:L], bs[:, 4:5], -30000.0, ALU.is_le, ALU.mult,
                         [tsc, tbs], [t_negm[b]])
                pcs.append(fin_)
                return pcs[:nsplit], pcs[nsplit:]

            def ld_q(qt):
                b = qt % 2
                k.dma("sp", qT_t[b][:], QT.rearrange("c p s -> p c s")[:, :, qt * 128:(qt + 1) * 128],
                      [t_dram["QT"]], [t_qT[b]])

            def B_pieces(qt):
                b = qt % 2
                nb3 = qt % 3
                pcs = []

                def init():
                    if qt + 1 < NT:
                        ld_q(qt + 1)
                    for hb in range(2):
                        k.mm(pso[hb][:, 0:260], zb[:, 0:128], zb[:, 0:260], True, False, [t_zb], [t_pso[hb]])
                pcs.append(init)
                st_ = {}

                def mkb(kt):
                    def pc():
                        if kt <= qt:
                            iv = cnt_v[0] % NKV; lb = cnt_v[0] % 2; cnt_v[0] += 1
                            st_[kt] = (iv, lb)
                            k.dma("sp", kv[iv][:], KV[kt], [t_dram["KV"]], [t_kv[iv]])
                            tl = t_psh[lb][0]
                            for half in range(2):
                                k.mm(psl[lb][:, half * 512:(half + 1) * 512], negm[nb3][:, kt * 128:(kt + 1) * 128],
                                     I4[:, :], True, False, [t_negm[nb3], t_I4], [tl])
                            for h in range(8):
                                c = h // 2; base = (h % 2) * 64
                                j = (h % 2) * 4 + h // 2
                                k.mm(psl[lb][:, j * 128:(j + 1) * 128], kv[iv][base:base + 64, c * 128:(c + 1) * 128],
                                     qT_t[b][base:base + 64, c, :], False, (h >= 6), [t_kv[iv], t_qT[b]], [tl])
                            k.act(pm[lb][:], psl[lb][:, :], AF.Exp, [tl], [t_pm[lb]], scale=0.125)
                        kp = kt - 1
                        if kp >= 0:
                            iv, lb = st_[kp]
                            for h in range(8):
                                hb = h // 4; o = (h % 4) * 65
                                j = (h % 2) * 4 + h // 2
                                k.mm(pso[hb][:, o:o + 65], pm[lb][:, j * 128:(j + 1) * 128],
                                     kv[iv][:, 512 + h * 65:512 + (h + 1) * 65], False, kp == qt,
                                     [t_pm[lb], t_kv[iv]], [t_pso[hb]])
                    return pc
                for kt in range(qt + 2):
                    pcs.append(mkb(kt))
                return pcs

            def finalize(qt):
                cols = slice(qt * 128, (qt + 1) * 128)
                for hb in range(2):
                    v3 = pso[hb][:, 0:260].rearrange("p (h e) -> p h e", e=65)
                    k.recip(fin[:, hb * 4:(hb + 1) * 4], v3[:, :, 64], [t_pso[hb]], [t_fin])
                    k.tt("dve", yaf[:, hb * 4:(hb + 1) * 4, :], v3[:, :, 0:64],
                         fin[:, hb * 4:(hb + 1) * 4].unsqueeze(2).to_broadcast([128, 4, 64]), ALU.mult,
                         [t_pso[hb], t_fin], [t_yaf])
                yf = yaf[:].rearrange("p h d -> p (h d)")
                k.act(yab[:], yf, AF.Square, [t_yaf], [t_yab, t_fin], accum=fin[:, 8:9])
                k.act(fin[:, 9:10], fin[:, 8:9], AF.Sqrt, [t_fin], [t_fin], scale=1.0 / 512, bias=EPS)
                k.recip(fin[:, 10:11], fin[:, 9:10], [t_fin], [t_fin])
                k.act(yab[:], yf, AF.Copy, [t_yaf, t_fin], [t_yab], scale=fin[:, 10:11])
                ptb = PSH[3][0].bitcast(BF16)
                for c in range(4):
                    k.tr(ptb[:, c * 128:(c + 1) * 128], yab[:, c * 128:(c + 1) * 128], ident[:],
                         [t_yab, t_id], [t_psh[3][0]])
                for c in range(4):
                    k.act(yaT[:, c, :], ptb[:, c * 128:(c + 1) * 128], AF.Copy, [t_psh[3][0], t_vec4], [t_yaT],
                          scale=vec4[:, AOG + c:AOG + c + 1])
                k.dma("pool", YA.rearrange("c p s -> p c s")[:, :, cols], yaT[:], [t_yaT], [t_dram["YA"]])

            def run_pieces(lists):
                tot = max(len(l_) for l_ in lists)
                idx = [0] * len(lists)
                for step in range(tot):
                    for li, l_ in enumerate(lists):
                        tgt = (step + 1) * len(l_) // tot
                        while idx[li] < tgt:
                            l_[idx[li]]()
                            idx[li] += 1

            ld_q(0)
            AP_ = {}

            def get_A(q):
                if q not in AP_:
                    AP_[q] = A_pieces(q)
                return AP_[q]

            run_pieces([get_A(0)[0]])
            lists0 = [get_A(0)[1]]
            if NT > 1:
                lists0.append(get_A(1)[0])
            run_pieces(lists0)
            for qt in range(NT):
                lists = [B_pieces(qt)]
                if qt + 1 < NT:
                    lists.append(get_A(qt + 1)[1])
                if qt + 2 < NT:
                    lists.append(get_A(qt + 2)[0])
                run_pieces(lists)
                finalize(qt)
                AP_.pop(qt, None)
            P.barrier()
            A.release()

        if stages >= 3:
            A.mark()
            maskd = A.alloc([128, NT, NE], F32); t_maskd = T("maskd")
            gated = A.alloc([128, NT, NE], F32); t_gated = T("gated")
            rankd = A.alloc([128, NT, NE], F32); t_rankd = T("rankd")
            basec = A.alloc([128, NE], F32); t_basec = T("basec")
            A.mark()
            g1bc = A.alloc([128, D], F32); t_g1bc = T("g1bc")
            gm2bc = A.alloc([128, D], F32); t_gm2bc = T("gm2bc")
            sh2bc = A.alloc([128, D], F32); t_sh2bc = T("sh2bc")
            bc_row(g1bc, 16, t_g1bc)
            bc_row(gm2bc, 56, t_gm2bc)
            bc_row(sh2bc, 24, t_sh2bc)
            w_out_sb = A.alloc([128, KC, D], BF16); t_wout = T("w_out")
            w_rt = A.alloc([128, KC, NE], F32); t_wrt = T("w_rt")
            brt = A.alloc([128, NE], F32); t_brt = T("brt")
            stg = [A.alloc([128, D], F32) for _ in range(2)]; t_stg = [T("stg0"), T("stg1")]
            for kc in range(KC):
                k.dma("sp", stg[kc % 2][:], w_out_d[kc * 128:(kc + 1) * 128, :], [], [t_stg[kc % 2]])
                k.cp(["dve", "act"][kc % 2], w_out_sb[:, kc, :], stg[kc % 2][:], [t_stg[kc % 2]], [t_wout])
            k.dma("sp", w_rt[:].rearrange("p c e -> p (c e)"), w_rt_d[:, :], [], [t_wrt])
            k.dma("sp", brt[:], b_rt_d[0:1, :].partition_broadcast(128), [], [t_brt])
            k.memset("pool", basec[:], 0.0, [t_basec])
            zt = A.alloc([128, 4, D], BF16); t_zt = T("zt")
            k.memset("pool", zt[:], 0.0, [t_zt])
            for jb in range(NBLK):
                k.dma("pool", HG[jb * BLK:(jb + 1) * BLK, :].rearrange("(a p) d -> p a d", p=128), zt[:],
                      [t_zt], [t_dram["HG"]])
            x_t = [A.alloc([128, D], F32) for _ in range(2)]; t_x2 = [T("x2a"), T("x2b")]
            cat = [A.alloc([128, 8, 128], BF16) for _ in range(2)]; t_cat = [T("cat0"), T("cat1")]
            x1 = [A.alloc([128, D], F32) for _ in range(2)]; t_x1 = [T("x1a"), T("x1b")]
            h2 = [A.alloc([128, D], F32) for _ in range(2)]; t_h2 = [T("h2a"), T("h2b")]
            h2b = [A.alloc([128, D], BF16) for _ in range(2)]; t_h2b = [T("h2ba"), T("h2bb")]
            h2T = [A.alloc([128, KC, 128], F32) for _ in range(2)]; t_h2T = [T("h2Ta"), T("h2Tb")]
            rt = [A.alloc([128, 64], F32) for _ in range(2)]; t_rt = [T("rta"), T("rtb")]
            lg = [A.alloc([128, NE], F32) for _ in range(2)]; t_lg = [T("lga"), T("lgb")]
            ex = [A.alloc([128, NE], F32) for _ in range(2)]; t_ex = [T("exa"), T("exb")]
            mb = [A.alloc([128, NE], BF16) for _ in range(2)]; t_mb = [T("mba"), T("mbb")]
            jk2 = [A.alloc([128, D], BF16) for _ in range(2)]; t_jk2 = [T("jk2a"), T("jk2b")]
            ps_mix = [PSH[0][0], PSH[0][1]]; t_psmix = [t_psh[0][0], t_psh[0][1]]
            ps_trs = [psT[1], psT[3]]; t_pstrs = [t_psh[1][0], t_psh[3][0]]
            ps_rs = [PSH[2][0], PSH[2][1]]; t_psrs = [t_psh[2][0], t_psh[2][1]]

            def tile_pieces(i):
                b = i % 2
                cols = slice(i * 128, (i + 1) * 128)
                ps_tr = ps_trs[b]; t_pstr = t_pstrs[b]
                ps_r = ps_rs[b]; t_psr = t_psrs[b]
                pcs = []

                def p0():
                    k.dma("sp", x_t[b][:], x_d[cols, :], [], [t_x2[b]])
                    k.dma("sp", cat[b][:, 0:4, :], YR.rearrange("c p s -> p c s")[:, :, cols], [t_dram["YR"]], [t_cat[b]])
                    k.dma("sp", cat[b][:, 4:8, :], YA.rearrange("c p s -> p c s")[:, :, cols], [t_dram["YA"]], [t_cat[b]])
                pcs.append(p0)

                def p1():
                    for half in range(2):
                        for c in range(8):
                            k.mm(ps_mix[half][:, :], cat[b][:, c, :], w_out_sb[:, c, half * 512:(half + 1) * 512],
                                 c == 0, c == 7, [t_cat[b], t_wout], [t_psmix[half]])
                    for half in range(2):
                        hs_ = slice(half * 512, (half + 1) * 512)
                        k.tt("dve", x1[b][:, hs_], ps_mix[half][:, :], g1bc[:, hs_], ALU.mult,
                             [t_psmix[half], t_g1bc], [t_x1[b]])
                pcs.append(p1)

                def p2():
                    k.tt("pool", x1[b][:], x1[b][:], x_t[b][:], ALU.add, [t_x1[b], t_x2[b]], [t_x1[b]])
                    k.dma("pool", X1[cols, :], x1[b][:], [t_x1[b]], [t_dram["X1"]])
                    k.act(jk2[b][:], x1[b][:], AF.Square, [t_x1[b]], [t_jk2[b], t_rt[b]], accum=rt[b][:, 0:1])
                pcs.append(p2)
                pcs.append(lambda: k.act(rt[b][:, 1:2], rt[b][:, 0:1], AF.Sqrt, [t_rt[b]], [t_rt[b]], scale=1.0 / D, bias=EPS))
                pcs.append(lambda: k.recip(rt[b][:, 2:3], rt[b][:, 1:2], [t_rt[b]], [t_rt[b]]))
                pcs.append(lambda: k.stt(h2[b][:], x1[b][:], rt[b][:, 2:3], gm2bc[:], ALU.mult, ALU.mult,
                                         [t_x1[b], t_rt[b], t_gm2bc], [t_h2[b]]))
                pcs.append(lambda: k.tt("pool", h2[b][:], h2[b][:], sh2bc[:], ALU.add, [t_h2[b], t_sh2bc], [t_h2[b]]))

                def p3():
                    k.cp("act", h2b[b][:], h2[b][:], [t_h2[b]], [t_h2b[b]])
                    k.dma("pool", H2[cols, :], h2b[b][:], [t_h2b[b]], [t_dram["H2"]])
                    for kc in range(KC):
                        k.tr(ps_tr[:, kc * 128:(kc + 1) * 128], h2[b][:, kc * 128:(kc + 1) * 128], identf[:],
                             [t_h2[b], t_idf], [t_pstr])
                pcs.append(p3)
                pcs.append(lambda: k.cp("act", h2T[b][:].rearrange("p c t -> p (c t)"), ps_tr[:, :], [t_pstr], [t_h2T[b]]))

                def p4():
                    for kc in range(KC):
                        k.mm(ps_r[:, 0:NE], h2T[b][:, kc, :], w_rt[:, kc, :], kc == 0, kc == KC - 1,
                             [t_h2T[b], t_wrt], [t_psr])
                pcs.append(p4)
                pcs.append(lambda: k.tt("dve", lg[b][:], ps_r[:, 0:NE], brt[:], ALU.add, [t_psr, t_brt], [t_lg[b]]))
                pcs.append(lambda: P.add("dve", (lambda o_, i_: (lambda e: e.max(out=o_, in_=i_)))(rt[b][:, 8:16], lg[b][:]),
                                         [t_lg[b]], [t_rt[b]]))
                pcs.append(lambda: k.ts("dve", maskd[:, i, :], lg[b][:], rt[b][:, 11:12], None, ALU.is_ge, None,
                                        [t_lg[b], t_rt[b]], [t_maskd]))

                def p5():
                    k.cp("dve", mb[b][:], maskd[:, i, :], [t_maskd], [t_mb[b]])
                    k.ts("dve", rt[b][:, 3:4], rt[b][:, 8:9], -1.0, None, ALU.mult, None, [t_rt[b]], [t_rt[b]])
                pcs.append(p5)

                def p6():
                    k.act(ex[b][:], lg[b][:], AF.Exp, [t_lg[b], t_rt[b]], [t_ex[b]], bias=rt[b][:, 3:4])
                    k.mm(ps_r[:, 32:64], ustr[:], mb[b][:], True, True, [t_ustr, t_mb[b]], [t_psr])
                    k.mm(ps_r[:, 64:96], ones_b[:, 0:128], mb[b][:], True, True, [t_ones, t_mb[b]], [t_psr])
                pcs.append(p6)
                pcs.append(lambda: k.stt(ex[b][:], ex[b][:], 1.0, maskd[:, i, :], ALU.mult, ALU.mult,
                                         [t_ex[b], t_maskd], [t_ex[b], t_rt[b]], accum=rt[b][:, 4:5]))
                pcs.append(lambda: k.recip(rt[b][:, 5:6], rt[b][:, 4:5], [t_rt[b]], [t_rt[b]]))
                pcs.append(lambda: k.ts("dve", gated[:, i, :], ex[b][:], rt[b][:, 5:6], None, ALU.mult, None,
                                        [t_ex[b], t_rt[b]], [t_gated]))

                def p7():
                    k.tt("dve", rankd[:, i, :], ps_r[:, 32:64], basec[:], ALU.add, [t_psr, t_basec], [t_rankd])
                    k.tt("dve", basec[:], ps_r[:, 64:96], basec[:], ALU.add, [t_psr, t_basec], [t_basec])
                pcs.append(p7)
                return pcs

            for i0_ in range(0, NT, 2):
                run_pieces([tile_pieces(i0_), tile_pieces(i0_ + 1)])
            P.barrier()
            A.release()

        if stages >= 4:
            A.mark()
            jrow = A.alloc([128, JMAX], F32); t_jrow = T("jrow")
            cmp3 = A.alloc([128, NE, JMAX], F32); t_cmp3 = T("cmp3")
            nb = A.alloc([128, NE], F32); t_nb = T("nb")
            incl = A.alloc([128, NE], F32); t_incl = T("incl")
            pst = A.alloc([128, NE], F32); t_pst = T("pst")
            onesf = A.alloc([128, NE], F32); t_onesf = T("onesf")
            jb_ = A.alloc([128, NBLK], F32); t_jb = T("jb")
            cmpb = A.alloc([128, NBLK, NE], F32); t_cmpb = T("cmpb")
            be = A.alloc([128, NBLK], F32); t_be = T("be")
            widxf = A.alloc([128, NBLK, 8], F32); t_widxf = T("widxf")
            pbig = A.alloc([128, 1], F32); t_pbig = T("pbig")
            bef = A.alloc([128, NBLK], F32); t_bef = T("bef")
            key3 = A.alloc([128, NT, NE], F32); t_key3 = T("key3")
            top8 = A.alloc([128, 8], F32); t_top8 = T("top8")
            d4f = A.alloc([128, NT, 4], F32); t_d4f = T("d4f")
            jk32 = A.alloc([128, NE], F32); t_jk32 = T("jk32")
            h2l = [A.alloc([128, D], BF16) for _ in range(3)]; t_h2l = [T(f"h2l{i}") for i in range(3)]
            k.iota(jrow[:], [[BLK, JMAX]], 0, 0, [t_jrow])
            k.iota(jb_[:], [[1, NBLK]], 0, 0, [t_jb])
            k.memset("pool", onesf[:], 1.0, [t_onesf])
            k.tt("dve", cmp3[:], jrow[:].unsqueeze(1).to_broadcast([128, NE, JMAX]),
                 basec[:].unsqueeze(2).to_broadcast([128, NE, JMAX]), ALU.is_lt, [t_jrow, t_basec], [t_cmp3])
            P.add("dve", lambda e: e.tensor_reduce(out=nb[:], in_=cmp3[:], axis=AX.X, op=ALU.add), [t_cmp3], [t_nb])
            P.add("dve", lambda e: e.tensor_tensor_scan(out=incl[:], data0=onesf[:], data1=nb[:], initial=0.0,
                                                        op0=ALU.mult, op1=ALU.add), [t_onesf, t_nb], [t_incl])
            k.tt("dve", pst[:], incl[:], nb[:], ALU.subtract, [t_incl, t_nb], [t_pst])
            k.ts("dve", pst[:], pst[:], float(BLK), None, ALU.mult, None, [t_pst], [t_pst])
            k.tt("dve", cmpb[:], incl[:].unsqueeze(1).to_broadcast([128, NBLK, NE]),
                 jb_[:].unsqueeze(2).to_broadcast([128, NBLK, NE]), ALU.is_le, [t_incl, t_jb], [t_cmpb])
            P.add("dve", lambda e: e.tensor_reduce(out=be[:], in_=cmpb[:], axis=AX.X, op=ALU.add), [t_cmpb], [t_be])
            k.ts("dve", be[:], be[:], float(NE - 1), None, ALU.min, None, [t_be], [t_be])
            k.cp("dve", bidx[:], be[:], [t_be], [t_bidx])
            k.ts("dve", be[:], be[:], 1024.0, pidx[:, 0:1], ALU.mult, ALU.add, [t_be, t_pidx], [t_be])
            for kc in range(KC):
                k.ts("dve", widxf[:, :, kc], be[:], float(kc * 128), None, ALU.add, None, [t_be], [t_widxf])
            k.cp("dve", widx[:].rearrange("p b c -> p (b c)"), widxf[:].rearrange("p b c -> p (b c)"),
                 [t_widxf], [t_widx])
            k.tt("dve", key3[:], rankd[:], pst[:].unsqueeze(1).to_broadcast([128, NT, NE]), ALU.add,
                 [t_rankd, t_pst], [t_key3])
            k.ts("dve", key3[:], key3[:], -1.0, BIGC, ALU.mult, ALU.add, [t_key3], [t_key3])
            k.tt("dve", key3[:], key3[:], maskd[:], ALU.mult, [t_key3, t_maskd], [t_key3])
            for i in range(NT):
                P.add("dve", (lambda o_, i_: (lambda e: e.max(out=o_, in_=i_)))(top8[:], key3[:, i, :]),
                      [t_key3], [t_top8])
                k.ts("dve", d4f[:, i, :], top8[:, 0:4], -1.0, BIGC, ALU.mult, ALU.add, [t_top8], [t_d4f])
                for k4 in range(4):
                    k.stt(jk32[:], key3[:, i, :], top8[:, k4:k4 + 1], gated[:, i, :], ALU.is_equal, ALU.mult,
                          [t_key3, t_top8, t_gated], [t_jk32, t_gate4], accum=gate4[:, i, k4:k4 + 1])
            k.cp("dve", dest4[:].rearrange("p t f -> p (t f)"), d4f[:].rearrange("p t f -> p (t f)"),
                 [t_d4f], [t_dest4])
            if debug:
                k.dma("sp", RTD[:, 0:NT * 4], d4f[:].rearrange("p t f -> p (t f)"), [t_d4f], [t_dram["RTD"]])
                k.dma("sp", RTD[:, NT * 4:NT * 8], gate4[:].rearrange("p t f -> p (t f)"), [t_gate4], [t_dram["RTD"]])
            for i in range(NT):
                hb_ = h2l[i % 3]; thb = t_h2l[i % 3]
                k.dma("sp", hb_[:], H2[i * 128:(i + 1) * 128, :], [t_dram["H2"]], [thb])
                for k4 in range(4):
                    k.scatter(HG[:, :], hb_[:], dest4[:, i, k4:k4 + 1], [thb, t_dest4, t_dram["HG"]], [t_dram["HG"]])
            P.barrier()
            A.release()
            A.release()

            A.mark()
            w1sb = [A.alloc([128, 9 * 2048], BF16) for _ in range(2)]; t_w1sb = [T("w1sb0"), T("w1sb1")]
            w2sb = [A.alloc([128, 9 * 1024], BF16) for _ in range(2)]; t_w2sb = [T("w2sb0"), T("w2sb1")]
            hg = [A.alloc([128, 4, D], BF16) for _ in range(2)]; t_hg = [T("hg0"), T("hg1")]
            hgT = A.alloc([128, KC, 512], BF16); t_hgT = [T(f"hgT{c}") for c in range(KC)]
            actT = A.alloc([128, KC, 512], BF16); t_actT = [T(f"actT{c}") for c in range(KC)]
            NE4 = 6
            ew = [A.alloc([128, 512], F32) for _ in range(NE4)]; t_ew = [T(f"ew{i}") for i in range(NE4)]
            ysb = [A.alloc([128, D], F32) for _ in range(2)]; t_ysb = [T("ysb0"), T("ysb1")]
            ewi = [0]

            def new_ew():
                i = ewi[0] % NE4; ewi[0] += 1
                return ew[i], t_ew[i]

            ps_t4 = [PSH[0][0].bitcast(BF16), PSH[0][1].bitcast(BF16)]; t_pst4 = [t_psh[0][0], t_psh[0][1]]
            ps_gl = [(PSH[1][0], t_psh[1][0], PSH[1][1], t_psh[1][1]), (PSH[2][0], t_psh[2][0], PSH[2][1], t_psh[2][1])]
            ps_y = [PSH[3][0], PSH[3][1]]; t_psy = [t_psh[3][0], t_psh[3][1]]
            ntr = 0; ngl = 0; ny = 0; nwf = 0
            NWS = 4
            wst = [A.alloc([128, 2048], F32) for _ in range(NWS)]; t_wst = [T(f"wst{i}") for i in range(NWS)]
            w1rows = w1_d.rearrange("e k n -> (e k) n")
            w2rows = w2_d.rearrange("e k n -> (e k) n")
            for jb in range(NBLK):
                b = jb % 2
                for kc in range(KC):
                    s_ = nwf % NWS; nwf += 1
                    k.gather(wst[s_][:, :], w1rows[:, :], widx[:, jb, kc:kc + 1], [t_widx], [t_wst[s_]])
                    k.cp("act",
                         w1sb[b][:, kc * 2048:(kc + 1) * 2048].rearrange("p (two f) -> p two f", two=2),
                         wst[s_][:, :].rearrange("p (f two) -> p two f", two=2), [t_wst[s_]], [t_w1sb[b]])
                s_ = nwf % NWS; nwf += 1
                k.gather(wst[s_][:, :], b1_d[:, :], bidx[:, jb:jb + 1], [t_bidx], [t_wst[s_]])
                k.cp("dve", w1sb[b][0:1, 8 * 2048:9 * 2048].rearrange("p (two f) -> p two f", two=2),
                     wst[s_][0:1, :].rearrange("p (f two) -> p two f", two=2), [t_wst[s_]], [t_w1sb[b]])
                for kc in range(KC):
                    s_ = nwf % NWS; nwf += 1
                    k.gather(wst[s_][:, 0:1024], w2rows[:, :], widx[:, jb, kc:kc + 1], [t_widx], [t_wst[s_]])
                    k.cp("dve", w2sb[b][:, kc * 1024:(kc + 1) * 1024], wst[s_][:, 0:1024], [t_wst[s_]], [t_w2sb[b]])
                s_ = nwf % NWS; nwf += 1
                k.gather(wst[s_][:, 0:1024], b2_d[:, :], bidx[:, jb:jb + 1], [t_bidx], [t_wst[s_]])
                k.ts("dve", w2sb[b][0:1, 8 * 1024:9 * 1024], wst[s_][0:1, 0:1024], 1.702, None, ALU.mult, None,
                     [t_wst[s_]], [t_w2sb[b]])
                k.dma("sp", hg[b][:], HG[jb * BLK:(jb + 1) * BLK, :].rearrange("(a p) d -> p a d", p=128),
                      [t_dram["HG"]], [t_hg[b]])
                for kc in range(KC):
                    pt_ = ps_t4[ntr % 2]; tpt_ = t_pst4[ntr % 2]; ntr += 1
                    for a_ in range(4):
                        k.tr(pt_[:, a_ * 128:(a_ + 1) * 128], hg[b][:, a_, kc * 128:(kc + 1) * 128], ident[:],
                             [t_hg[b], t_id], [tpt_])
                    k.cp("act" if kc % 2 == 0 else "dve", hgT[:, kc, :], pt_[:, 0:512], [tpt_], [t_hgT[kc]])
                for fc in range(KC):
                    pg, tpg, pl, tpl = ps_gl[ngl % 2]; ngl += 1
                    for (pz_, tz_, off) in ((pg, tpg, 0), (pl, tpl, 1024)):
                        for kc in range(KC):
                            k.mm(pz_[:, :], w1sb[b][:, kc * 2048 + off + fc * 128:kc * 2048 + off + (fc + 1) * 128],
                                 hgT[:, kc, :], kc == 0, False, [t_w1sb[b], t_hgT[kc]], [tz_])
                        k.mm(pz_[:, :], w1sb[b][0:1, 8 * 2048 + off + fc * 128:8 * 2048 + off + (fc + 1) * 128],
                             ones_b[0:1, 0:512], False, True, [t_w1sb[b], t_ones], [tz_])
                    g_, tg_ = new_ew()
                    k.ts("dve", g_[:], pg[:, :], 7.0, None, ALU.min, None, [tpg], [tg_])
                    sl, tsl = new_ew()
                    k.act(sl[:], g_[:], AF.Silu, [tg_], [tsl], scale=1.702)
                    l_, tl_ = new_ew()
                    k.ts("dve", l_[:], pl[:, :], -7.0, 7.0, ALU.max, ALU.min, [tpl], [tl_])
                    k.stt(actT[:, fc, :], l_[:], 1.0, sl[:], ALU.add, ALU.mult, [tl_, tsl], [t_actT[fc]])
                for a_ in range(4):
                    yb = ysb[ny % 2]; tyb = t_ysb[ny % 2]; ny += 1
                    for dh in range(2):
                        py = ps_y[dh]; tpy = t_psy[dh]
                        for fc in range(KC):
                            k.mm(py[:, :], actT[:, fc, a_ * 128:(a_ + 1) * 128],
                                 w2sb[b][:, fc * 1024 + dh * 512:fc * 1024 + (dh + 1) * 512], fc == 0, False,
                                 [t_actT[fc], t_w2sb[b]], [tpy])
                        k.mm(py[:, :], ones_b[0:1, 0:128], w2sb[b][0:1, 8 * 1024 + dh * 512:8 * 1024 + (dh + 1) * 512],
                             False, True, [t_ones, t_w2sb[b]], [tpy])
                        k.act(yb[:, dh * 512:(dh + 1) * 512], py[:, :], AF.Copy, [tpy], [tyb], scale=1.0 / 1.702)
                    r0 = jb * BLK + a_ * 128
                    k.dma("sp", YY[r0:r0 + 128, :], yb[:], [tyb], [t_dram["YY"]])
            P.barrier()
            A.release()

            A.mark()
            g2bc = A.alloc([128, D], F32); t_g2bc = T("g2bc")
            bc_row(g2bc, 40, t_g2bc)
            x1l = [A.alloc([128, D], F32) for _ in range(2)]; t_x1l = [T("x1l0"), T("x1l1")]
            yg = [[A.alloc([128, D], F32) for _ in range(4)] for _ in range(2)]
            t_yg = [[T(f"yg{b}{q}") for q in range(4)] for b in range(2)]
            acc = [A.alloc([128, D], F32) for _ in range(2)]; t_acc = [T("acc0"), T("acc1")]
            for i in range(NT):
                b = i % 2
                cols = slice(i * 128, (i + 1) * 128)
                k.dma("sp", x1l[b][:], X1[cols, :], [t_dram["X1"]], [t_x1l[b]])
                for k4 in range(4):
                    k.gather(yg[b][k4][:, :], YY[:, :], dest4[:, i, k4:k4 + 1], [t_dest4, t_dram["YY"]], [t_yg[b][k4]])
                k.ts("dve", acc[b][:], yg[b][0][:], gate4[:, i, 0:1], None, ALU.mult, None,
                     [t_yg[b][0], t_gate4], [t_acc[b]])
                for k4 in range(1, 4):
                    k.stt(acc[b][:], yg[b][k4][:], gate4[:, i, k4:k4 + 1], acc[b][:], ALU.mult, ALU.add,
                          [t_yg[b][k4], t_gate4, t_acc[b]], [t_acc[b]])
                k.tt("pool", acc[b][:], acc[b][:], g2bc[:], ALU.mult, [t_acc[b], t_g2bc], [t_acc[b]])
                k.tt("dve", acc[b][:], acc[b][:], x1l[b][:], ALU.add, [t_acc[b], t_x1l[b]], [t_acc[b]])
                k.dma("sp", out_d[cols, :], acc[b][:], [t_acc[b]], [t_dram["OUT"]])
            A.release()
        elif stages >= 1:
            A.mark()
            zt2 = A.alloc([128, D], F32); t_zt2 = T("zt2")
            k.memset("pool", zt2[:], 0.0, [t_zt2])
            k.dma("sp", out_d[0:128, :], zt2[:], [t_zt2], [t_dram["OUT"]])
            A.release()
        P.emit()
    return nc


def _fm(v, nchunk):
    return np.ascontiguousarray(np.asarray(v, np.float32).reshape(nchunk, 128).T)


def prep_shared(inp, small=False):
    L = 0
    f32 = np.float32
    sh = {}
    sh["w_ada"] = np.ascontiguousarray(inp["w_ada"][L], f32)
    sh["b_ada_fm"] = _fm(inp["b_ada"][L], 48)
    sh["n1g_fm"] = _fm(inp["norm1_g"][L], 8)
    sh["n2g_fm"] = _fm(inp["norm2_g"][L], 8)
    sh["w_in"] = np.ascontiguousarray(inp["w_in"][L], f32)
    cw = np.asarray(inp["conv_w"][L], f32)
    sh["convw_fm"] = np.ascontiguousarray(cw.T.reshape(4, 128, 4).transpose(1, 0, 2).reshape(128, 16))
    v4 = np.zeros((128, 28), f32)
    for j, name in enumerate(["conv_b", "b_rg_a", "b_rg_x", "lru_lambda", "rg_out_g", "attn_out_g"]):
        v4[:, j * 4:(j + 1) * 4] = _fm(inp[name][L], 4)
    v4[:, 24] = np.tile(np.asarray(inp["q_norm_g"][L], f32), 2)
    v4[:, 25] = np.tile(np.asarray(inp["k_norm_g"][L], f32), 2)
    v4[:, 26] = np.tile(np.asarray(inp["kidx_norm_g"][L], f32), 2)
    sh["vec4_fm"] = v4
    for nm, key in (("wabd", "w_rg_a"), ("wxbd", "w_rg_x")):
        w = np.asarray(inp[key][L], f32)
        bd = np.zeros((128, 4, 128), f32)
        for c in range(4):
            bd[0:64, c, 0:64] = w[2 * c]
            bd[64:128, c, 64:128] = w[2 * c + 1]
        sh[nm] = bd.reshape(128, 512)
    sh["w_out"] = np.ascontiguousarray(inp["w_out"][L], f32)
    wr = np.asarray(inp["w_router"][L], f32)
    sh["w_rt_fm"] = np.ascontiguousarray(wr.reshape(8, 128, 32).transpose(1, 0, 2).reshape(128, 256))
    sh["b_rt"] = np.asarray(inp["b_router"][L], f32).reshape(1, 32)
    if not small:
        sh["w1"] = np.ascontiguousarray(inp["w1"][L], f32)
        sh["b1"] = np.ascontiguousarray(inp["b1"][L], f32)
        sh["w2"] = np.ascontiguousarray(inp["w2"][L], f32)
        sh["b2"] = np.ascontiguousarray(inp["b2"][L], f32)
    return sh


def kernel(**inputs):
    x = np.asarray(inputs["x"], np.float32)
    c = np.asarray(inputs["c"], np.float32)
    B, S, _ = x.shape
    sh = prep_shared(inputs)
    nc = build_nc(S)
    in_maps = []
    for b in range(B):
        m = dict(sh)
        m["x"] = np.ascontiguousarray(x[b])
        m["c_fm"] = _fm(c[b], 8)
        in_maps.append(m)
    res = run_bass_kernel_spmd(nc, in_maps, core_ids=list(range(B)))
    return np.stack([np.asarray(r["out"], np.float32) for r in res.results], axis=0)
```

```python
import numpy as np
import concourse.bass as bass
import concourse.mybir as mybir
from concourse.bass_utils import run_bass_kernel_spmd

F32 = mybir.dt.float32
BF16 = mybir.dt.bfloat16
I32 = mybir.dt.int32
U32 = mybir.dt.uint32
U8 = mybir.dt.uint8
ALU = mybir.AluOpType
AF = mybir.ActivationFunctionType
AX = mybir.AxisListType
DSZ = {F32: 4, BF16: 2, I32: 4, U32: 4, U8: 1}


class Tok:
    __slots__ = ("name", "lw", "rd", "excl")

    def __init__(self, name, excl=False):
        self.name = name
        self.lw = None
        self.rd = []
        self.excl = excl


class _Op:
    __slots__ = ("eng", "fn", "deps", "idx", "dma", "sem", "val", "used", "slot_prev")

    def __init__(self, eng, fn, deps, idx, dma):
        self.eng = eng
        self.fn = fn
        self.deps = deps
        self.idx = idx
        self.dma = dma
        self.sem = None
        self.val = 0
        self.used = False
        self.slot_prev = None


class Prog:
    ENGS = ("pe", "act", "dve", "pool", "sp")
    NSLOT = 12
    SEG = 20000

    def __init__(self, nc):
        self.nc = nc
        self.ops = []
        self.by_eng = {e: [] for e in self.ENGS}
        self.pending_barrier = {e: [] for e in self.ENGS}
        self.recent_dma = {e: [] for e in self.ENGS}

    def add(self, eng, fn, rd=(), wr=(), dma=False):
        ex = [t for t in rd if t.excl]
        if ex:
            rd = [t for t in rd if not t.excl]
            wr = list(wr) + ex
        deps = set()
        for t in rd:
            if t.lw is not None:
                deps.add(t.lw)
        for t in wr:
            if t.lw is not None:
                deps.add(t.lw)
            deps.update(t.rd)
        if self.pending_barrier[eng]:
            deps.update(self.pending_barrier[eng])
            self.pending_barrier[eng] = []
        idx = len(self.ops)
        op = _Op(eng, fn, sorted(deps), idx, dma)
        self.ops.append(op)
        self.by_eng[eng].append(op)
        for t in rd:
            t.rd.append(idx)
        for t in wr:
            t.lw = idx
            t.rd = []
        if dma:
            r = self.recent_dma[eng]
            r.append(idx)
            if len(r) > self.NSLOT:
                r.pop(0)
        return idx

    def barrier(self):
        last = []
        for e in self.ENGS:
            ops = self.by_eng[e]
            for o in reversed(ops):
                if not o.dma:
                    last.append(o.idx)
                    break
            last.extend(self.recent_dma[e])
        for e in self.ENGS:
            self.pending_barrier[e] = list(set(self.pending_barrier[e]) | set(last))

    def emit(self, final_wait_eng="sp"):
        nc = self.nc
        ops = self.ops
        self.barrier()
        fin = self.pending_barrier[final_wait_eng]
        for o in ops:
            for d in o.deps:
                ops[d].used = True
        for d in fin:
            ops[d].used = True
        nsem = 0
        plan = {}
        for e in self.ENGS:
            ncomp = sum(1 for o in self.by_eng[e] if (not o.dma) and o.used)
            nseg = (ncomp + self.SEG - 1) // self.SEG
            ndma = self.NSLOT if any(o.dma for o in self.by_eng[e]) else 0
            plan[e] = (nseg, ndma)
            nsem += nseg + ndma
        import contextlib

        with contextlib.ExitStack() as st:
            sems = [st.enter_context(nc.semaphore(f"s{i}")) for i in range(nsem)]
            si = 0
            for e in self.ENGS:
                nseg, ndma = plan[e]
                seg = sems[si:si + nseg]
                si += nseg
                slots = sems[si:si + ndma]
                si += ndma
                slot_cnt = [0] * ndma
                k = 0
                c = 0
                for o in self.by_eng[e]:
                    if o.dma:
                        s = k % ndma
                        k += 1
                        o.slot_prev = (slots[s], slot_cnt[s]) if slot_cnt[s] else None
                        slot_cnt[s] += 16
                        o.sem, o.val = slots[s], slot_cnt[s]
                    elif o.used:
                        o.sem = seg[c // self.SEG]
                        o.val = (c % self.SEG) + 1
                        c += 1
            block = st.enter_context(nc.Block())

            def run(eng_name):
                def body(e):
                    waited = {}

                    def wait(sem, val):
                        key = id(sem)
                        if waited.get(key, 0) >= val:
                            return
                        waited[key] = val
                        e.wait_ge(sem, val)

                    for o in self.by_eng[eng_name]:
                        for d in o.deps:
                            p = ops[d]
                            if p.eng == "pe" and eng_name == "pe" and not p.dma:
                                continue
                            wait(p.sem, p.val)
                        if o.slot_prev is not None:
                            wait(*o.slot_prev)
                        ins = o.fn(e)
                        if o.dma:
                            ins.then_inc(o.sem, 16)
                        elif o.used:
                            ins.then_inc(o.sem, 1)
                    if eng_name == final_wait_eng:
                        for d in fin:
                            wait(ops[d].sem, ops[d].val)
                return body

            block.tensor(run("pe"))
            block.scalar(run("act"))
            block.vector(run("dve"))
            block.gpsimd(run("pool"))
            block.sync(run("sp"))


class Arena:
    def __init__(self, nc, st, nbytes, name="arena"):
        self.t = st.enter_context(nc.sbuf_tensor(name, [128, nbytes], U8))
        self.nbytes = nbytes
        self.off = 0
        self.marks = []

    def alloc(self, shape, dtype, name=None):
        assert shape[0] <= 128
        free = int(np.prod(shape[1:]))
        nb = free * DSZ[dtype]
        nb_al = (nb + 63) // 64 * 64
        assert self.off + nb_al <= self.nbytes, (self.off, nb_al, self.nbytes, name)
        ap = self.t[0:shape[0], self.off:self.off + nb]
        self.off += nb_al
        if dtype != U8:
            ap = ap.bitcast(dtype)
        if len(shape) > 2:
            names = " ".join(f"d{i}" for i in range(1, len(shape)))
            kw = {f"d{i}": shape[i] for i in range(1, len(shape))}
            ap = ap.rearrange(f"p ({names}) -> p {names}", **kw)
        return ap

    def mark(self):
        self.marks.append(self.off)

    def release(self):
        self.off = self.marks.pop()

import contextlib

D = 1024
KC = 8
NE = 32
EPS = 1e-6
BLK = 512
NIT = 14
BIGC = float(2 ** 20)


class K:
    def __init__(self, P):
        self.P = P

    def mm(self, out, lhsT, rhs, start, stop, rd, wr):
        self.P.add("pe", lambda e: e.matmul(out, lhsT=lhsT, rhs=rhs, start=start, stop=stop,
                                            skip_group_check=True), rd, wr)

    def tr(self, out, in_, ident, rd, wr):
        self.P.add("pe", lambda e: e.transpose(out=out, in_=in_, identity=ident), rd, wr)

    def act(self, out, in_, func, rd, wr, scale=1.0, bias=0.0, accum=None, eng="act"):
        if accum is None:
            self.P.add(eng, lambda e: e.activation(out=out, in_=in_, func=func, scale=scale, bias=bias), rd, wr)
        else:
            self.P.add(eng, lambda e: e.activation(out=out, in_=in_, func=func, scale=scale, bias=bias,
                                                   accum_out=accum), rd, wr)

    def ts(self, eng, out, in0, s1, s2, op0, op1, rd, wr, accum=None):
        if op1 is None:
            self.P.add(eng, lambda e: e.tensor_scalar(out=out, in0=in0, scalar1=s1, scalar2=None, op0=op0), rd, wr)
        elif accum is None:
            self.P.add(eng, lambda e: e.tensor_scalar(out=out, in0=in0, scalar1=s1, scalar2=s2, op0=op0, op1=op1), rd, wr)
        else:
            self.P.add(eng, lambda e: e.tensor_scalar(out=out, in0=in0, scalar1=s1, scalar2=s2, op0=op0, op1=op1,
                                                      accum_out=accum), rd, wr)

    def tt(self, eng, out, in0, in1, op, rd, wr):
        self.P.add(eng, lambda e: e.tensor_tensor(out=out, in0=in0, in1=in1, op=op), rd, wr)

    def stt(self, out, in0, scalar, in1, op0, op1, rd, wr, accum=None):
        if accum is None:
            self.P.add("dve", lambda e: e.scalar_tensor_tensor(out=out, in0=in0, scalar=scalar, in1=in1,
                                                              op0=op0, op1=op1), rd, wr)
        else:
            self.P.add("dve", lambda e: e.scalar_tensor_tensor(out=out, in0=in0, scalar=scalar, in1=in1,
                                                              op0=op0, op1=op1, accum_out=accum), rd, wr)

    def cp(self, eng, out, in_, rd, wr):
        if eng == "act":
            self.P.add("act", lambda e: e.activation(out=out, in_=in_, func=AF.Copy), rd, wr)
        else:
            self.P.add(eng, lambda e: e.tensor_copy(out=out, in_=in_), rd, wr)

    def memset(self, eng, ap, val, wr):
        self.P.add(eng, lambda e: e.memset(ap, val), (), wr)

    def dma(self, q, out, in_, rd, wr):
        self.P.add(q, lambda e: e.dma_start(out=out, in_=in_), rd, wr, dma=True)

    def gather(self, out, in_, idx, rd, wr, bounds=None):
        if bounds is None:
            self.P.add("pool", lambda e: e.indirect_dma_start(
                out=out, out_offset=None, in_=in_,
                in_offset=bass.IndirectOffsetOnAxis(ap=idx, axis=0)), rd, wr, dma=True)
        else:
            self.P.add("pool", lambda e: e.indirect_dma_start(
                out=out, out_offset=None, in_=in_,
                in_offset=bass.IndirectOffsetOnAxis(ap=idx, axis=0),
                bounds_check=bounds, oob_is_err=False), rd, wr, dma=True)

    def scatter(self, out, in_, idx, rd, wr):
        self.P.add("pool", lambda e: e.indirect_dma_start(
            out=out, out_offset=bass.IndirectOffsetOnAxis(ap=idx, axis=0),
            in_=in_, in_offset=None), rd, wr, dma=True)

    def recip(self, out, in_, rd, wr):
        self.P.add("dve", lambda e: e.reciprocal(out=out, in_=in_), rd, wr)

    def iota(self, out, pattern, base, cm, wr):
        self.P.add("pool", lambda e: e.iota(out, pattern=pattern, base=base, channel_multiplier=cm,
                                            allow_small_or_imprecise_dtypes=True), (), wr)


def build_nc(S, stages=5, debug=False):
    NT = S // 128
    NG = S // 512
    NSEL = min(256, S // 4)
    NSLOT = 4 * S + NE * BLK
    NBLK = NSLOT // BLK
    JMAX = S // BLK

    nc = bass.Bass("TRN2", target_bir_lowering=False)

    def din(name, shape, dt=F32):
        return nc.dram_tensor(name, list(shape), dt, kind="ExternalInput").ap()

    def dscr(name, shape, dt):
        kind = "ExternalOutput" if debug else "Internal"
        return nc.dram_tensor(name, list(shape), dt, kind=kind).ap()

    x_d = din("x", [S, D])
    c_d = din("c_fm", [128, KC])
    w_ada_d = din("w_ada", [D, 6 * D])
    b_ada_d = din("b_ada_fm", [128, 48])
    n1g_d = din("n1g_fm", [128, KC])
    n2g_d = din("n2g_fm", [128, KC])
    w_in_d = din("w_in", [D, 3144])
    convw_d = din("convw_fm", [128, 16])
    vec4_d = din("vec4_fm", [128, 28])
    wabd_d = din("wabd", [128, 4 * 128])
    wxbd_d = din("wxbd", [128, 4 * 128])
    w_out_d = din("w_out", [D, D])
    w_rt_d = din("w_rt_fm", [128, KC * NE])
    b_rt_d = din("b_rt", [1, NE])
    if stages >= 4:
        w1_d = din("w1", [NE, D, 2 * D])
        b1_d = din("b1", [NE, 2 * D])
        w2_d = din("w2", [NE, D, D])
        b2_d = din("b2", [NE, D])
    import os as _os
    CUT = int(_os.environ.get("P1CUT", "99"))
    out_d = nc.dram_tensor("out", [S, D], F32, kind="ExternalOutput").ap()

    MODROW = dscr("modrow", [64, 128], F32)
    QT = dscr("qt", [4, 128, S], BF16)
    KV = dscr("kv", [NT, 128, 1032], BF16)
    QIT = dscr("qit", [4, 128, S], BF16)
    KIT = dscr("kit", [128, S], BF16)
    WI = dscr("wi", [NT, 128, 8], F32)
    YR = dscr("yr", [4, 128, S], BF16)
    YA = dscr("ya", [4, 128, S], BF16)
    X1 = dscr("x1", [S, D], F32)
    H2 = dscr("h2", [S, D], BF16)
    HG = dscr("hg", [NSLOT, D], BF16)
    YY = dscr("yy", [NSLOT, D], F32)
    RTD = dscr("rtd", [128, NT * 8], F32)

    P = Prog(nc)
    k = K(P)
    T = Tok
    st = contextlib.ExitStack()
    with st:
        A = Arena(nc, st, 206 * 1024)
        psT = [st.enter_context(nc.psum_tensor(f"ps{i}", [128, 1024], F32)) for i in range(4)]
        PSH = [[psT[i][:, 0:512], psT[i][:, 512:1024]] for i in range(4)]
        t_psh = [[Tok(f"ps{i}a", True), Tok(f"ps{i}b", True)] for i in range(4)]
        t_dram = {n_: T(n_) for n_ in ("QT", "KV", "QIT", "KIT", "WI", "YR", "YA", "X1", "H2", "HG", "YY",
                                      "MODROW", "RTD", "OUT")}

        io = A.alloc([128, 128], F32); t_io = T("io")
        identf = A.alloc([128, 128], F32); t_idf = T("identf")
        ident = A.alloc([128, 128], BF16); t_id = T("ident")
        ones_b = A.alloc([128, 512], BF16); t_ones = T("ones")
        bo64 = A.alloc([128, 128], BF16); t_bo = T("bo64")
        ustr = A.alloc([128, 128], BF16); t_ustr = T("ustr")
        modfm = A.alloc([128, 64], F32); t_mod = T("modfm")
        vec4 = A.alloc([128, 28], F32); t_vec4 = T("vec4")
        convw = A.alloc([128, 16], F32); t_convw = T("convw")
        nsp = A.alloc([128, 4], F32); t_nsp = T("nsp")
        pidx = A.alloc([128, 1], F32); t_pidx = T("pidx")
        dest4 = A.alloc([128, NT, 4], I32); t_dest4 = T("dest4")
        gate4 = A.alloc([128, NT, 4], F32); t_gate4 = T("gate4")
        widx = A.alloc([128, NBLK, 8], I32); t_widx = T("widx")
        bidx = A.alloc([128, NBLK], I32); t_bidx = T("bidx")

        k.iota(io[:], [[1, 128]], 0, -1, [t_io])
        k.ts("dve", identf[:], io[:], 0.0, None, ALU.is_equal, None, [t_io], [t_idf])
        k.cp("dve", ident[:], identf[:], [t_idf], [t_id])
        k.ts("dve", ustr[:], io[:], 0.0, None, ALU.is_gt, None, [t_io], [t_ustr])
        k.iota(pidx[:], [[0, 1]], 0, 1, [t_pidx])
        k.memset("pool", ones_b[:], 1.0, [t_ones])
        k.memset("pool", bo64[:], 0.0, [t_bo])
        k.memset("pool", bo64[0:64, 0:64], 1.0, [t_bo])
        k.memset("pool", bo64[64:128, 64:128], 1.0, [t_bo])
        k.dma("sp", vec4[:], vec4_d[:, :], [], [t_vec4])
        k.dma("sp", convw[:], convw_d[:, :], [], [t_convw])
        CB, BA, BX, LAM, RGG, AOG = 0, 4, 8, 12, 16, 20
        QG, KG, KIG = 24, 25, 26

        A.mark()
        csb = A.alloc([128, KC], F32); t_c = T("c")
        scs = A.alloc([128, KC], F32); t_scs = T("scs")
        bada = A.alloc([128, 48], F32); t_bada = T("bada")
        n1g = A.alloc([128, KC], F32); t_n1g = T("n1g")
        n2g = A.alloc([128, KC], F32); t_n2g = T("n2g")
        wa_buf = [A.alloc([128, KC, 768], F32) for _ in range(2)]
        t_wa = [T("wa0"), T("wa1")]
        modT = A.alloc([64, 128], F32); t_modT = T("modT")
        tmp4 = A.alloc([128, 4], F32); t_tmp4 = T("tmp4")
        k.dma("sp", csb[:], c_d[:, :], [], [t_c])
        k.dma("sp", bada[:], b_ada_d[:, :], [], [t_bada])
        k.dma("sp", n1g[:], n1g_d[:, :], [], [t_n1g])
        k.dma("sp", n2g[:], n2g_d[:, :], [], [t_n2g])
        k.act(scs[:], csb[:], AF.Silu, [t_c], [t_scs])
        pmod = PSH[0][0]
        for jg in range(8):
            wb_ = wa_buf[jg % 2]
            k.dma("sp", wb_[:], w_ada_d[:, jg * 768:(jg + 1) * 768].rearrange("(kc p) n -> p kc n", p=128),
                  [], [t_wa[jg % 2]])
            for jj in range(6):
                j = jg * 6 + jj
                for kc in range(KC):
                    k.mm(pmod[:, j:j + 1], wb_[:, kc, jj * 128:(jj + 1) * 128], scs[:, kc:kc + 1],
                         kc == 0, kc == KC - 1, [t_wa[jg % 2], t_scs], [t_psh[0][0]])
        k.tt("dve", modfm[:, 0:48], pmod[:, 0:48], bada[:], ALU.add, [t_psh[0][0], t_bada], [t_mod])
        k.stt(modfm[:, 48:56], modfm[:, 8:16], 1.0, n1g[:], ALU.add, ALU.mult, [t_mod, t_n1g], [t_mod])
        k.stt(modfm[:, 56:64], modfm[:, 32:40], 1.0, n2g[:], ALU.add, ALU.mult, [t_mod, t_n2g], [t_mod])
        pTm = PSH[0][1]
        k.tr(pTm[0:64, 0:128], modfm[:, 0:64], identf[:], [t_mod, t_idf], [t_psh[0][1]])
        k.cp("dve", modT[:], pTm[0:64, 0:128], [t_psh[0][1]], [t_modT])
        k.dma("sp", MODROW[:, :], modT[:], [t_modT], [t_dram["MODROW"]])
        mr = MODROW.rearrange("j p -> (j p)")

        def bc_row(dst, j0, tok):
            src = mr[j0 * 128:(j0 + 8) * 128].rearrange("(o n) -> o n", o=1).partition_broadcast(128)
            k.dma("sp", dst[:], src, [t_dram["MODROW"]], [tok])

        k.act(tmp4[:], vec4[:, LAM:LAM + 4], AF.Exp, [t_vec4], [t_tmp4], scale=-1.0)
        k.act(nsp[:], tmp4[:], AF.Ln, [t_tmp4], [t_nsp], bias=1.0)
        k.ts("dve", nsp[:], nsp[:], -8.0, None, ALU.mult, None, [t_nsp], [t_nsp])
        P.barrier()
        A.release()

        if stages == 0:
            A.mark()
            zt2 = A.alloc([128, D], F32); t_zt2 = T("zt2")
            k.memset("pool", zt2[:], 0.0, [t_zt2])
            k.dma("sp", out_d[0:128, :], zt2[:], [t_zt2], [t_dram["OUT"]])
            P.emit()
            return nc
        A.mark()
        cv_ld = [A.alloc([128, 2048], F32) for _ in range(3)]
        t_cvld = [T(f"cvld{i}") for i in range(3)]

        A.mark()
        w_in_sb = A.alloc([128, KC, 3208], BF16); t_win = T("w_in")
        wabd = A.alloc([128, 4, 128], BF16); t_wabd = T("wabd")
        wxbd = A.alloc([128, 4, 128], BF16); t_wxbd = T("wxbd")
        n = 0
        for kc in range(KC):
            sg = cv_ld[n % 3]; tg = t_cvld[n % 3]
            k.dma("sp", sg[:, 0:2048], w_in_d[kc * 128:(kc + 1) * 128, 0:2048], [], [tg])
            k.cp(["dve", "act", "pool"][n % 3], w_in_sb[:, kc, 0:2048], sg[:, 0:2048], [tg], [t_win])
            n += 1
            sg = cv_ld[n % 3]; tg = t_cvld[n % 3]
            k.dma("sp", sg[:, 0:1096], w_in_d[kc * 128:(kc + 1) * 128, 2048:3144], [], [tg])
            k.cp(["dve", "act", "pool"][n % 3], w_in_sb[:, kc, 2048:3136], sg[:, 0:1088], [tg], [t_win])
            k.cp("pool", w_in_sb[:, kc, 3136:3200], sg[:, 1024:1088], [tg], [t_win])
            k.cp("pool", w_in_sb[:, kc, 3200:3208], sg[:, 1088:1096], [tg], [t_win])
            n += 1
        for (dst, src, tk) in ((wabd, wabd_d, t_wabd), (wxbd, wxbd_d, t_wxbd)):
            sg = cv_ld[n % 3]; tg = t_cvld[n % 3]
            k.dma("sp", sg[:, 0:512], src[:, :], [], [tg])
            k.cp("dve", dst[:].rearrange("p c m -> p (c m)"), sg[:, 0:512], [tg], [tk])
            n += 1

        xt = [A.alloc([128, D], F32) for _ in range(2)]; t_xt = [T("xt0"), T("xt1")]
        xn = [A.alloc([128, D], BF16) for _ in range(2)]; t_xn = [T("xn0"), T("xn1")]
        junkb = A.alloc([128, D], BF16); t_junkb = T("junkb")
        st1 = A.alloc([128, 8], F32); t_st1 = T("st1")
        hT = [A.alloc([128, KC, 512], BF16) for _ in range(2)]; t_hT = [T("hT0"), T("hT1")]
        vsb = [A.alloc([128, 8, 65], BF16) for _ in range(2)]; t_vsb = [T("vsb0"), T("vsb1")]
        wisb = [A.alloc([128, 8], F32) for _ in range(2)]; t_wisb = [T("wisb0"), T("wisb1")]
        xr_ext = A.alloc([128, 4, 516], F32); t_xre = [T(f"xre{c}") for c in range(4)]
        carry = A.alloc([128, 4], F32); t_carry = T("carry")
        xtail = A.alloc([128, 4, 4], F32); t_xtail = T("xtail")
        NW = 8
        wf = [A.alloc([128, 512], F32) for _ in range(NW)]; t_wf = [T(f"wf{i}") for i in range(NW)]
        NWB = 6
        wb = [A.alloc([128, 512], BF16) for _ in range(NWB)]; t_wb = [T(f"wb{i}") for i in range(NWB)]
        yv = A.alloc([128, 4, 512], F32); t_yv = [T(f"yv{i}") for i in range(4)]
        xc_all = A.alloc([128, 4, 512], F32); t_xc = [T(f"xc{i}") for i in range(4)]
        xcb_all = A.alloc([128, 4, 512], BF16); t_xcb = [T(f"xcb{i}") for i in range(4)]
        ysq = A.alloc([128, 4, 512], BF16); t_ysq = [T(f"ysq{i}") for i in range(4)]
        for b in range(2):
            k.memset("pool", vsb[b][:], 1.0, [t_vsb[b]])
        k.memset("pool", xr_ext[:], 0.0, t_xre)
        k.memset("pool", carry[:], 0.0, [t_carry])
        wfi = [0]; wbi = [0]; zi = [0]

        def new_wf():
            i = wfi[0] % NW; wfi[0] += 1
            return wf[i], t_wf[i]

        def new_wb():
            i = wbi[0] % NWB; wbi[0] += 1
            return wb[i], t_wb[i]

        zbufs = [(PSH[1][0], t_psh[1][0]), (PSH[1][1], t_psh[1][1]), (PSH[2][0], t_psh[2][0])]

        def new_z():
            i = zi[0] % 3; zi[0] += 1
            return zbufs[i]

        ps_wi, t_pswi = PSH[2][1], t_psh[2][1]
        ps_ss, t_psss = PSH[3][0], t_psh[3][0]
        ps_v, t_psv = PSH[3][1], t_psh[3][1]
        gm1 = modfm[:, 48:56]
        sh1 = modfm[:, 0:8]

        for G in range(NG):
            h_T = hT[G % 2]; th = t_hT[G % 2]
            for j in range(4):
                i = 4 * G + j
                x_t = xt[i % 2]; tx = t_xt[i % 2]
                x_n = xn[i % 2]; txn = t_xn[i % 2]
                k.dma("sp", x_t[:], x_d[i * 128:(i + 1) * 128, :], [], [tx])
                k.act(junkb[:], x_t[:], AF.Square, [tx], [t_junkb, t_st1], accum=st1[:, 0:1])
                k.act(st1[:, 1:2], st1[:, 0:1], AF.Sqrt, [t_st1], [t_st1], scale=1.0 / D, bias=EPS)
                k.recip(st1[:, 2:3], st1[:, 1:2], [t_st1], [t_st1])
                k.act(x_n[:], x_t[:], AF.Copy, [tx, t_st1], [txn], scale=st1[:, 2:3])
                pt = psT[0][:, (i % 2) * 512:(i % 2 + 1) * 512].bitcast(BF16)
                tpt = t_psh[0][i % 2]
                for kc in range(KC):
                    k.tr(pt[:, kc * 128:(kc + 1) * 128], x_n[:, kc * 128:(kc + 1) * 128], ident[:],
                         [txn, t_id], [tpt])
                for kc in range(KC):
                    dst = h_T[:, kc, j * 128:(j + 1) * 128]
                    src = pt[:, kc * 128:(kc + 1) * 128]
                    if kc % 2 == 0:
                        k.act(dst, src, AF.Identity, [tpt, t_mod], [th], scale=gm1[:, kc:kc + 1],
                              bias=sh1[:, kc:kc + 1])
                    else:
                        k.ts("dve", dst, src, gm1[:, kc:kc + 1], sh1[:, kc:kc + 1], ALU.mult, ALU.add,
                             [tpt, t_mod], [th])
                if CUT < 2:
                    continue
                for kc in range(KC):
                    k.mm(ps_v[:, 0:512], h_T[:, kc, j * 128:(j + 1) * 128], w_in_sb[:, kc, 2048:2560],
                         kc == 0, kc == KC - 1, [th, t_win], [t_psv])
                for kc in range(KC):
                    k.mm(ps_wi[:, 0:8], h_T[:, kc, j * 128:(j + 1) * 128], w_in_sb[:, kc, 3200:3208],
                         kc == 0, kc == KC - 1, [th, t_win], [t_pswi])
                vb = vsb[i % 2]; tvb = t_vsb[i % 2]
                k.cp("act", vb[:, :, 0:64], ps_v[:, 0:512].rearrange("p (h d) -> p h d", d=64), [t_psv], [tvb])
                k.dma("pool", KV[i, :, 512:1032], vb[:].rearrange("p h d -> p (h d)"), [tvb], [t_dram["KV"]])
                wbt = wisb[i % 2]; twb = t_wisb[i % 2]
                k.cp("dve", wbt[:], ps_wi[:, 0:8], [t_pswi], [twb])
                k.dma("pool", WI[i], wbt[:], [twb], [t_dram["WI"]])

            if CUT < 3:
                continue
            cols = slice(G * 512, (G + 1) * 512)

            def zchunk(col0, M=128):
                pz, tz = new_z()
                for kc in range(KC):
                    k.mm(pz[0:M, :], w_in_sb[:, kc, col0:col0 + M], h_T[:, kc, :], kc == 0, kc == KC - 1,
                         [th, t_win], [tz])
                return pz, tz

            rnn_state = []
            for c in range(4):
                pz, tz = zchunk(c * 128)
                if G > 0:
                    k.cp("pool", xr_ext[:, c, 0:3], xtail[:, c, 0:3], [t_xtail], [t_xre[c]])
                k.cp("act", xr_ext[:, c, 3:515], pz[:, :], [tz], [t_xre[c]])
                t0, tt0 = xc_all[:, c, :], t_xc[c]
                txre = t_xre[c]
                k.ts("dve", t0[:], xr_ext[:, c, 0:512], convw[:, c * 4:c * 4 + 1], vec4[:, CB + c:CB + c + 1],
                     ALU.mult, ALU.add, [txre, t_convw, t_vec4], [tt0])
                for tap in (1, 2, 3):
                    k.stt(t0[:], xr_ext[:, c, tap:tap + 512], convw[:, c * 4 + tap:c * 4 + tap + 1], t0[:],
                          ALU.mult, ALU.add, [txre, t_convw, tt0], [tt0])
                k.cp("pool", xtail[:, c, 0:3], xr_ext[:, c, 512:515], [txre], [t_xtail])
                xcb, txcb = xcb_all[:, c, :], t_xcb[c]
                k.cp("pool", xcb, t0, [tt0], [txcb])
                rnn_state.append((t0, tt0, xcb, txcb))
            if CUT < 4:
                continue
            for c in range(4):
                pz, tz = zchunk(512 + c * 128)
                k.act(yv[:, c, :], pz[:, :], AF.Gelu_apprx_tanh, [tz], [t_yv[c]])
            if CUT < 5:
                continue
            for (base_col, gcol, dst, tkd) in ((1024, QG, QT, t_dram["QT"]), (1536, KG, None, t_dram["KV"])):
                for c in range(4):
                    pz, tz = zchunk(base_col + c * 128)
                    sq, tsq = new_wb()
                    k.act(sq[:], pz[:, :], AF.Square, [tz], [tsq])
                    qf, tqf = new_wf()
                    k.ts("dve", qf[:], pz[:, :], vec4[:, gcol:gcol + 1], None, ALU.mult, None, [tz, t_vec4], [tqf])
                    k.mm(ps_ss[:, :], bo64[:], sq[:], True, True, [t_bo, tsq], [t_psss])
                    rs, trs = new_wf()
                    k.act(rs[:], ps_ss[:, :], AF.Sqrt, [t_psss], [trs], scale=1.0 / 64, bias=EPS)
                    k.recip(rs[:], rs[:], [trs], [trs])
                    qn, tqn = new_wb()
                    k.tt("dve", qn[:], qf[:], rs[:], ALU.mult, [tqf, trs], [tqn])
                    if dst is not None:
                        k.dma("pool", dst[c, :, cols], qn[:], [tqn], [tkd])
                    else:
                        k.dma("pool", KV[4 * G:4 * G + 4, :, c * 128:(c + 1) * 128].rearrange("t p s -> p t s"),
                              qn[:].rearrange("p (t s) -> p t s", t=4), [tqn], [tkd])
            if CUT < 6:
                continue
            for c in range(4 * int(_os.environ.get("DUP", "1"))):
                c = c % 4
                pz, tz = zchunk(2560 + c * 128)
                qn, tqn = new_wb()
                k.cp("act", qn[:], pz[:, :], [tz], [tqn])
                k.dma("pool", QIT[c, :, cols], qn[:], [tqn], [t_dram["QIT"]])
            for _d in range(int(_os.environ.get("DVEDUP", "0"))):
                qf, tqf = new_wf()
                k.ts(_os.environ.get("DUPENG", "dve"), qf[:], xc_all[:, 0, :], 2.0, None, ALU.mult, None, [t_xc[0]], [tqf])
            if CUT < 7:
                continue
            SK = set(_os.environ.get("SKIP", "").split(","))
            pz, tz = zchunk(int(_os.environ.get("KICOL", "3072")))
            sq, tsq = new_wb()
            if "a" not in SK:
                k.act(sq[:], pz[:, :], AF.Square, [tz], [tsq])
            qf, tqf = new_wf()
            if "b" not in SK:
                k.ts("dve", qf[:], pz[:, :], vec4[:, KIG:KIG + 1], None, ALU.mult, None, [tz, t_vec4], [tqf])
            if "c" not in SK:
                k.mm(ps_ss[:, :], bo64[:], sq[:], True, True, [t_bo, tsq], [t_psss])
            rs, trs = new_wf()
            if "d" not in SK:
                k.act(rs[:], ps_ss[:, :], AF.Sqrt, [t_psss], [trs], scale=1.0 / 64, bias=EPS)
            if "e" not in SK:
                k.recip(rs[:], rs[:], [trs], [trs])
            qn, tqn = new_wb()
            if "f" not in SK:
                k.tt("dve", qn[:], qf[:], rs[:], ALU.mult, [tqf, trs], [tqn])
            if "g" not in SK:
                k.dma("sp", KIT[:, cols], qn[:], [tqn], [t_dram["KIT"]])
            if CUT < 8:
                continue
            for c in range(4):
                xc, txc, xcb, txcb = rnn_state[c]
                pa, tpa = new_z()
                k.mm(pa[:, :], wabd[:, c, :], xcb[:], True, True, [t_wabd, txcb], [tpa])
                r, tr_ = new_wf()
                k.act(r[:], pa[:, :], AF.Sigmoid, [tpa, t_vec4], [tr_], bias=vec4[:, BA + c:BA + c + 1])
                px, tpx = new_z()
                k.mm(px[:, :], wxbd[:, c, :], xcb[:], True, True, [t_wxbd, txcb], [tpx])
                ig, tig = new_wf()
                k.act(ig[:], px[:, :], AF.Sigmoid, [tpx, t_vec4], [tig], bias=vec4[:, BX + c:BX + c + 1])
                a, ta = new_wf()
                k.act(a[:], r[:], AF.Exp, [tr_, t_nsp], [ta], scale=nsp[:, c:c + 1])
                k.tt("pool", r[:], a[:], a[:], ALU.mult, [ta], [tr_])
                k.act(r[:], r[:], AF.Sqrt, [tr_], [tr_], scale=-1.0, bias=1.0)
                k.tt("dve", ig[:], ig[:], r[:], ALU.mult, [tig, tr_], [tig])
                k.tt("dve", ig[:], ig[:], xc[:], ALU.mult, [tig, txc], [tig])
                hsc, thsc = new_wf()
                P.add("dve", (lambda o_, a_, u_, i_: (lambda e: e.tensor_tensor_scan(
                    out=o_, data0=a_, data1=u_, initial=i_, op0=ALU.mult, op1=ALU.add)))(
                        hsc[:], a[:], ig[:], carry[:, c:c + 1]), [ta, tig, t_carry], [thsc])
                k.cp("dve", carry[:, c:c + 1], hsc[:, 511:512], [thsc], [t_carry])
                k.tt("dve", yv[:, c, :], yv[:, c, :], hsc[:], ALU.mult, [t_yv[c], thsc], [t_yv[c]])
                k.act(ysq[:, c, :], yv[:, c, :], AF.Square, [t_yv[c]], [t_ysq[c]])
            if CUT < 9:
                continue
            for c in range(4):
                k.mm(ps_ss[:, :], ones_b[:, 0:128], ysq[:, c, :], c == 0, c == 3, [t_ones, t_ysq[c]], [t_psss])
            rs, trs = new_wf()
            k.act(rs[:], ps_ss[:, :], AF.Sqrt, [t_psss], [trs], scale=1.0 / 512, bias=EPS)
            k.recip(rs[:], rs[:], [trs], [trs])
            for c in range(4):
                yn, tyn = new_wb()
                k.stt(yn[:], yv[:, c, :], vec4[:, RGG + c:RGG + c + 1], rs[:], ALU.mult, ALU.mult,
                      [t_yv[c], t_vec4, trs], [tyn])
                k.dma("pool", YR[c, :, cols], yn[:], [tyn], [t_dram["YR"]])
        P.barrier()
        A.release()
        A.release()

        if stages >= 2:
            A.mark()
            kiT = A.alloc([128, S], BF16); t_kiT = T("kiT")
            score = [A.alloc([128, S], F32) for _ in range(2)]; t_score = [T("score0"), T("score1")]
            negm = [A.alloc([128, S], BF16) for _ in range(3)]; t_negm = [T("negm0"), T("negm1"), T("negm2")]
            junk8 = A.alloc([128, S], U8)
            I4 = A.alloc([128, 512], BF16); t_I4 = T("I4")
            zb = A.alloc([128, 260], BF16); t_zb = T("zb")
            pow2 = A.alloc([128, NIT + 1], F32); t_pow2 = T("pow2")
            qT_t = [A.alloc([128, 4, 128], BF16) for _ in range(2)]; t_qT = [T("qT0"), T("qT1")]
            qiT_t = [A.alloc([128, 4, 128], BF16) for _ in range(3)]; t_qiT = [T("qiT0"), T("qiT1"), T("qiT2")]
            wi_t = [A.alloc([128, 8], F32) for _ in range(3)]; t_wi = [T("wi0"), T("wi1"), T("wi2")]
            NTR = 4
            trelu = [A.alloc([128, 512], F32) for _ in range(NTR)]; t_trelu = [T(f"trelu{i}") for i in range(NTR)]
            pm = [A.alloc([128, 1024], BF16) for _ in range(2)]; t_pm = [T("pm0"), T("pm1")]
            NKV = 4
            kv = [A.alloc([128, 1032], BF16) for _ in range(NKV)]; t_kv = [T(f"kv{i}") for i in range(NKV)]
            bis = [A.alloc([128, 8], F32) for _ in range(2)]; t_bis = [T("bis0"), T("bis1")]
            dall = [A.alloc([128, NIT + 1], F32) for _ in range(2)]; t_dall = [T("dall0"), T("dall1")]
            yaf = A.alloc([128, 8, 64], F32); t_yaf = T("yaf")
            yab = A.alloc([128, 512], BF16); t_yab = T("yab")
            yaT = A.alloc([128, 4, 128], BF16); t_yaT = T("yaT")
            fin = A.alloc([128, 16], F32); t_fin = T("fin")

            k.dma("sp", kiT[:, :], KIT[:, :], [t_dram["KIT"]], [t_kiT])
            for r4 in range(4):
                k.cp("pool", I4[:, r4 * 128:(r4 + 1) * 128], ident[:], [t_id], [t_I4])
            k.memset("pool", zb[:], 0.0, [t_zb])
            for it in range(NIT + 1):
                k.memset("pool", pow2[:, it:it + 1], float(2.0 ** (-it)), [t_pow2])

            ps_i = [PSH[3][0], PSH[3][1]]; t_psi = [t_psh[3][0], t_psh[3][1]]
            psl = [psT[0], psT[1]]
            pso = [PSH[2][0], PSH[2][1]]; t_pso = [t_psh[2][0], t_psh[2][1]]
            cnt_i = [0]; cnt_v = [0]

            def A_pieces(qt):
                L = (qt + 1) * 128
                b = qt % 3
                s2 = qt % 2
                sc_ = score[s2]; tsc = t_score[s2]
                bs = bis[s2]; tbs = t_bis[s2]
                dl = dall[s2]; tdl = t_dall[s2]
                pcs = []

                def ld():
                    k.dma("sp", qiT_t[b][:], QIT.rearrange("c p s -> p c s")[:, :, qt * 128:(qt + 1) * 128],
                          [t_dram["QIT"]], [t_qiT[b]])
                    k.dma("sp", wi_t[b][:], WI[qt], [t_dram["WI"]], [t_wi[b]])
                pcs.append(ld)
                items = [(g, min(512, L - g * 512), h) for g in range((L + 511) // 512) for h in range(8)]
                LAG = 2
                used_tr = {}

                def mk(idx):
                    def pc():
                        if idx < len(items):
                            g, n_, h = items[idx]
                            c = h // 2; base = (h % 2) * 64
                            ib = cnt_i[0] % 2; itr = cnt_i[0] % NTR; cnt_i[0] += 1
                            used_tr[idx] = itr
                            k.mm(ps_i[ib][:, 0:n_], qiT_t[b][base:base + 64, c, :],
                                 kiT[base:base + 64, g * 512:g * 512 + n_], True, True,
                                 [t_qiT[b], t_kiT], [t_psi[ib]])
                            k.act(trelu[itr][:, 0:n_], ps_i[ib][:, 0:n_], AF.Relu, [t_psi[ib]], [t_trelu[itr]])
                        j = idx - LAG
                        if j >= 0:
                            g, n_, h = items[j]
                            itr = used_tr[j]
                            sc = sc_[:, g * 512:g * 512 + n_]
                            if h == 0:
                                k.ts("dve", sc, trelu[itr][:, 0:n_], wi_t[b][:, 0:1], None, ALU.mult, None,
                                     [t_trelu[itr], t_wi[b]], [tsc])
                            else:
                                k.stt(sc, trelu[itr][:, 0:n_], wi_t[b][:, h:h + 1], sc, ALU.mult, ALU.add,
                                      [t_trelu[itr], t_wi[b], tsc], [tsc])
                    return pc
                for idx in range(len(items) + LAG):
                    pcs.append(mk(idx))

                def prep():
                    if L > NSEL:
                        P.add("dve", lambda e: e.tensor_reduce(out=bs[:, 0:1], in_=sc_[:, 0:L], axis=AX.X,
                                                               op=ALU.max, apply_absolute_value=True),
                              [tsc], [tbs])
                        k.ts("dve", dl[:], pow2[:], bs[:, 0:1], None, ALU.mult, None, [t_pow2, tbs], [tdl])
                        k.memset("dve", bs[:, 1:2], 0.0, [tbs])
                        k.memset("dve", bs[:, 5:6], 0.0, [tbs])
                    else:
                        k.memset("dve", bs[:, 4:5], -1e29, [tbs])
                    k.memset("dve", sc_[0:64, L - 64:L], -1e30, [tsc])
                pcs.append(prep)
                nsplit = len(pcs)
                use_act = (qt % 2 == 1)
                if L > NSEL:
                    for it in range(NIT):
                        def pc(it=it):
                            if not use_act:
                                k.ts("dve", junk8[:, 0:L], sc_[:, 0:L], bs[:, 1:2], None, ALU.is_gt, ALU.add,
                                     [tsc, tbs], [tbs], accum=bs[:, 2:3])
                                k.ts("dve", bs[:, 3:4], bs[:, 2:3], float(NSEL), -0.5, ALU.is_ge, ALU.add,
                                     [tbs], [tbs])
                            else:
                                k.act(negm[b][:, 0:L], sc_[:, 0:L], AF.Sign, [tsc, tbs], [t_negm[b], tbs],
                                      bias=bs[:, 5:6], accum=bs[:, 2:3])
                                k.ts("dve", bs[:, 3:4], bs[:, 2:3], float(2 * NSEL - L), -0.5, ALU.is_ge, ALU.add,
                                     [tbs], [tbs])
                            k.stt(bs[:, 1:2], bs[:, 3:4], dl[:, it:it + 1], bs[:, 1:2], ALU.mult, ALU.add,
                                  [tbs, tdl], [tbs])
                            if use_act:
                                k.ts("dve", bs[:, 5:6], bs[:, 1:2], -1.0, None, ALU.mult, None, [tbs], [tbs])
                        pcs.append(pc)
                    nsplit += NIT // 4

                def fin_():
                    if L > NSEL:
                        k.tt("dve", bs[:, 4:5], bs[:, 1:2], dl[:, NIT:NIT + 1], ALU.subtract,
                             [tbs, tdl], [tbs])
                    k.ts("dve", negm[b][:, 0:L], sc_[:, 0:L], bs[:, 4:5], -30000.0, ALU.is_le, ALU.mult,
                         [tsc, tbs], [t_negm[b]])
                pcs.append(fin_)
                return pcs[:nsplit], pcs[nsplit:]

            def ld_q(qt):
                b = qt % 2
                k.dma("sp", qT_t[b][:], QT.rearrange("c p s -> p c s")[:, :, qt * 128:(qt + 1) * 128],
                      [t_dram["QT"]], [t_qT[b]])

            def B_pieces(qt):
                b = qt % 2
                nb3 = qt % 3
                pcs = []

                def init():
                    if qt + 1 < NT:
                        ld_q(qt + 1)
                    for hb in range(2):
                        k.mm(pso[hb][:, 0:260], zb[:, 0:128], zb[:, 0:260], True, False, [t_zb], [t_pso[hb]])
                pcs.append(init)
                st_ = {}

                def mkb(kt):
                    def pc():
                        if kt <= qt:
                            iv = cnt_v[0] % NKV; lb = cnt_v[0] % 2; cnt_v[0] += 1
                            st_[kt] = (iv, lb)
                            k.dma("sp", kv[iv][:], KV[kt], [t_dram["KV"]], [t_kv[iv]])
                            tl = t_psh[lb][0]
                            for half in range(2):
                                k.mm(psl[lb][:, half * 512:(half + 1) * 512], negm[nb3][:, kt * 128:(kt + 1) * 128],
                                     I4[:, :], True, False, [t_negm[nb3], t_I4], [tl])
                            for h in range(8):
                                c = h // 2; base = (h % 2) * 64
                                j = (h % 2) * 4 + h // 2
                                k.mm(psl[lb][:, j * 128:(j + 1) * 128], kv[iv][base:base + 64, c * 128:(c + 1) * 128],
                                     qT_t[b][base:base + 64, c, :], False, (h >= 6), [t_kv[iv], t_qT[b]], [tl])
                            k.act(pm[lb][:], psl[lb][:, :], AF.Exp, [tl], [t_pm[lb]], scale=0.125)
                        kp = kt - 1
                        if kp >= 0:
                            iv, lb = st_[kp]
                            for h in range(8):
                                hb = h // 4; o = (h % 4) * 65
                                j = (h % 2) * 4 + h // 2
                                k.mm(pso[hb][:, o:o + 65], pm[lb][:, j * 128:(j + 1) * 128],
                                     kv[iv][:, 512 + h * 65:512 + (h + 1) * 65], False, kp == qt,
                                     [t_pm[lb], t_kv[iv]], [t_pso[hb]])
                    return pc
                for kt in range(qt + 2):
                    pcs.append(mkb(kt))
                return pcs

            def finalize(qt):
                cols = slice(qt * 128, (qt + 1) * 128)
                for hb in range(2):
                    v3 = pso[hb][:, 0:260].rearrange("p (h e) -> p h e", e=65)
                    k.recip(fin[:, hb * 4:(hb + 1) * 4], v3[:, :, 64], [t_pso[hb]], [t_fin])
                    k.tt("dve", yaf[:, hb * 4:(hb + 1) * 4, :], v3[:, :, 0:64],
                         fin[:, hb * 4:(hb + 1) * 4].unsqueeze(2).to_broadcast([128, 4, 64]), ALU.mult,
                         [t_pso[hb], t_fin], [t_yaf])
                yf = yaf[:].rearrange("p h d -> p (h d)")
                k.act(yab[:], yf, AF.Square, [t_yaf], [t_yab, t_fin], accum=fin[:, 8:9])
                k.act(fin[:, 9:10], fin[:, 8:9], AF.Sqrt, [t_fin], [t_fin], scale=1.0 / 512, bias=EPS)
                k.recip(fin[:, 10:11], fin[:, 9:10], [t_fin], [t_fin])
                k.act(yab[:], yf, AF.Copy, [t_yaf, t_fin], [t_yab], scale=fin[:, 10:11])
                ptb = PSH[3][0].bitcast(BF16)
                for c in range(4):
                    k.tr(ptb[:, c * 128:(c + 1) * 128], yab[:, c * 128:(c + 1) * 128], ident[:],
                         [t_yab, t_id], [t_psh[3][0]])
                for c in range(4):
                    k.act(yaT[:, c, :], ptb[:, c * 128:(c + 1) * 128], AF.Copy, [t_psh[3][0], t_vec4], [t_yaT],
                          scale=vec4[:, AOG + c:AOG + c + 1])
                k.dma("pool", YA.rearrange("c p s -> p c s")[:, :, cols], yaT[:], [t_yaT], [t_dram["YA"]])

            def run_pieces(lists):
                tot = max(len(l_) for l_ in lists)
                idx = [0] * len(lists)
                for step in range(tot):
                    for li, l_ in enumerate(lists):
                        tgt = (step + 1) * len(l_) // tot
                        while idx[li] < tgt:
                            l_[idx[li]]()
                            idx[li] += 1

            ld_q(0)
            AP_ = {}

            def get_A(q):
                if q not in AP_:
                    AP_[q] = A_pieces(q)
                return AP_[q]

            run_pieces([get_A(0)[0]])
            lists0 = [get_A(0)[1]]
            if NT > 1:
                lists0.append(get_A(1)[0])
            run_pieces(lists0)
            for qt in range(NT):
                lists = [B_pieces(qt)]
                if qt + 1 < NT:
                    lists.append(get_A(qt + 1)[1])
                if qt + 2 < NT:
                    lists.append(get_A(qt + 2)[0])
                run_pieces(lists)
                finalize(qt)
                AP_.pop(qt, None)
            P.barrier()
            A.release()

        if stages >= 3:
            A.mark()
            maskd = A.alloc([128, NT, NE], F32); t_maskd = T("maskd")
            gated = A.alloc([128, NT, NE], F32); t_gated = T("gated")
            rankd = A.alloc([128, NT, NE], F32); t_rankd = T("rankd")
            basec = A.alloc([128, NE], F32); t_basec = T("basec")
            A.mark()
            g1bc = A.alloc([128, D], F32); t_g1bc = T("g1bc")
            gm2bc = A.alloc([128, D], F32); t_gm2bc = T("gm2bc")
            sh2bc = A.alloc([128, D], F32); t_sh2bc = T("sh2bc")
            bc_row(g1bc, 16, t_g1bc)
            bc_row(gm2bc, 56, t_gm2bc)
            bc_row(sh2bc, 24, t_sh2bc)
            w_out_sb = A.alloc([128, KC, D], BF16); t_wout = T("w_out")
            w_rt = A.alloc([128, KC, NE], F32); t_wrt = T("w_rt")
            brt = A.alloc([128, NE], F32); t_brt = T("brt")
            stg = [A.alloc([128, D], F32) for _ in range(2)]; t_stg = [T("stg0"), T("stg1")]
            for kc in range(KC):
                k.dma("sp", stg[kc % 2][:], w_out_d[kc * 128:(kc + 1) * 128, :], [], [t_stg[kc % 2]])
                k.cp(["dve", "act"][kc % 2], w_out_sb[:, kc, :], stg[kc % 2][:], [t_stg[kc % 2]], [t_wout])
            k.dma("sp", w_rt[:].rearrange("p c e -> p (c e)"), w_rt_d[:, :], [], [t_wrt])
            k.dma("sp", brt[:], b_rt_d[0:1, :].partition_broadcast(128), [], [t_brt])
            k.memset("pool", basec[:], 0.0, [t_basec])
            zt = A.alloc([128, 4, D], BF16); t_zt = T("zt")
            k.memset("pool", zt[:], 0.0, [t_zt])
            for jb in range(NBLK):
                k.dma("pool", HG[jb * BLK:(jb + 1) * BLK, :].rearrange("(a p) d -> p a d", p=128), zt[:],
                      [t_zt], [t_dram["HG"]])
            x_t = [A.alloc([128, D], F32) for _ in range(2)]; t_x2 = [T("x2a"), T("x2b")]
            cat = [A.alloc([128, 8, 128], BF16) for _ in range(2)]; t_cat = [T("cat0"), T("cat1")]
            x1 = [A.alloc([128, D], F32) for _ in range(2)]; t_x1 = [T("x1a"), T("x1b")]
            h2 = [A.alloc([128, D], F32) for _ in range(2)]; t_h2 = [T("h2a"), T("h2b")]
            h2b = [A.alloc([128, D], BF16) for _ in range(2)]; t_h2b = [T("h2ba"), T("h2bb")]
            h2T = [A.alloc([128, KC, 128], F32) for _ in range(2)]; t_h2T = [T("h2Ta"), T("h2Tb")]
            rt = [A.alloc([128, 64], F32) for _ in range(2)]; t_rt = [T("rta"), T("rtb")]
            lg = [A.alloc([128, NE], F32) for _ in range(2)]; t_lg = [T("lga"), T("lgb")]
            ex = [A.alloc([128, NE], F32) for _ in range(2)]; t_ex = [T("exa"), T("exb")]
            mb = [A.alloc([128, NE], BF16) for _ in range(2)]; t_mb = [T("mba"), T("mbb")]
            jk2 = [A.alloc([128, D], BF16) for _ in range(2)]; t_jk2 = [T("jk2a"), T("jk2b")]
            ps_mix = [PSH[0][0], PSH[0][1]]; t_psmix = [t_psh[0][0], t_psh[0][1]]
            ps_trs = [psT[1], psT[3]]; t_pstrs = [t_psh[1][0], t_psh[3][0]]
            ps_rs = [PSH[2][0], PSH[2][1]]; t_psrs = [t_psh[2][0], t_psh[2][1]]

            def tile_pieces(i):
                b = i % 2
                cols = slice(i * 128, (i + 1) * 128)
                ps_tr = ps_trs[b]; t_pstr = t_pstrs[b]
                ps_r = ps_rs[b]; t_psr = t_psrs[b]
                pcs = []

                def p0():
                    k.dma("sp", x_t[b][:], x_d[cols, :], [], [t_x2[b]])
                    k.dma("sp", cat[b][:, 0:4, :], YR.rearrange("c p s -> p c s")[:, :, cols], [t_dram["YR"]], [t_cat[b]])
                    k.dma("sp", cat[b][:, 4:8, :], YA.rearrange("c p s -> p c s")[:, :, cols], [t_dram["YA"]], [t_cat[b]])
                pcs.append(p0)

                def p1():
                    for half in range(2):
                        for c in range(8):
                            k.mm(ps_mix[half][:, :], cat[b][:, c, :], w_out_sb[:, c, half * 512:(half + 1) * 512],
                                 c == 0, c == 7, [t_cat[b], t_wout], [t_psmix[half]])
                    for half in range(2):
                        hs_ = slice(half * 512, (half + 1) * 512)
                        k.tt("dve", x1[b][:, hs_], ps_mix[half][:, :], g1bc[:, hs_], ALU.mult,
                             [t_psmix[half], t_g1bc], [t_x1[b]])
                pcs.append(p1)

                def p2():
                    k.tt("pool", x1[b][:], x1[b][:], x_t[b][:], ALU.add, [t_x1[b], t_x2[b]], [t_x1[b]])
                    k.dma("pool", X1[cols, :], x1[b][:], [t_x1[b]], [t_dram["X1"]])
                    k.act(jk2[b][:], x1[b][:], AF.Square, [t_x1[b]], [t_jk2[b], t_rt[b]], accum=rt[b][:, 0:1])
                pcs.append(p2)
                pcs.append(lambda: k.act(rt[b][:, 1:2], rt[b][:, 0:1], AF.Sqrt, [t_rt[b]], [t_rt[b]], scale=1.0 / D, bias=EPS))
                pcs.append(lambda: k.recip(rt[b][:, 2:3], rt[b][:, 1:2], [t_rt[b]], [t_rt[b]]))
                pcs.append(lambda: k.stt(h2[b][:], x1[b][:], rt[b][:, 2:3], gm2bc[:], ALU.mult, ALU.mult,
                                         [t_x1[b], t_rt[b], t_gm2bc], [t_h2[b]]))
                pcs.append(lambda: k.tt("pool", h2[b][:], h2[b][:], sh2bc[:], ALU.add, [t_h2[b], t_sh2bc], [t_h2[b]]))

                def p3():
                    k.cp("act", h2b[b][:], h2[b][:], [t_h2[b]], [t_h2b[b]])
                    k.dma("pool", H2[cols, :], h2b[b][:], [t_h2b[b]], [t_dram["H2"]])
                    for kc in range(KC):
                        k.tr(ps_tr[:, kc * 128:(kc + 1) * 128], h2[b][:, kc * 128:(kc + 1) * 128], identf[:],
                             [t_h2[b], t_idf], [t_pstr])
                pcs.append(p3)
                pcs.append(lambda: k.cp("act", h2T[b][:].rearrange("p c t -> p (c t)"), ps_tr[:, :], [t_pstr], [t_h2T[b]]))

                def p4():
                    for kc in range(KC):
                        k.mm(ps_r[:, 0:NE], h2T[b][:, kc, :], w_rt[:, kc, :], kc == 0, kc == KC - 1,
                             [t_h2T[b], t_wrt], [t_psr])
                pcs.append(p4)
                pcs.append(lambda: k.tt("dve", lg[b][:], ps_r[:, 0:NE], brt[:], ALU.add, [t_psr, t_brt], [t_lg[b]]))
                pcs.append(lambda: P.add("dve", (lambda o_, i_: (lambda e: e.max(out=o_, in_=i_)))(rt[b][:, 8:16], lg[b][:]),
                                         [t_lg[b]], [t_rt[b]]))
                pcs.append(lambda: k.ts("dve", maskd[:, i, :], lg[b][:], rt[b][:, 11:12], None, ALU.is_ge, None,
                                        [t_lg[b], t_rt[b]], [t_maskd]))

                def p5():
                    k.cp("dve", mb[b][:], maskd[:, i, :], [t_maskd], [t_mb[b]])
                    k.ts("dve", rt[b][:, 3:4], rt[b][:, 8:9], -1.0, None, ALU.mult, None, [t_rt[b]], [t_rt[b]])
                pcs.append(p5)

                def p6():
                    k.act(ex[b][:], lg[b][:], AF.Exp, [t_lg[b], t_rt[b]], [t_ex[b]], bias=rt[b][:, 3:4])
                    k.mm(ps_r[:, 32:64], ustr[:], mb[b][:], True, True, [t_ustr, t_mb[b]], [t_psr])
                    k.mm(ps_r[:, 64:96], ones_b[:, 0:128], mb[b][:], True, True, [t_ones, t_mb[b]], [t_psr])
                pcs.append(p6)
                pcs.append(lambda: k.stt(ex[b][:], ex[b][:], 1.0, maskd[:, i, :], ALU.mult, ALU.mult,
                                         [t_ex[b], t_maskd], [t_ex[b], t_rt[b]], accum=rt[b][:, 4:5]))
                pcs.append(lambda: k.recip(rt[b][:, 5:6], rt[b][:, 4:5], [t_rt[b]], [t_rt[b]]))
                pcs.append(lambda: k.ts("dve", gated[:, i, :], ex[b][:], rt[b][:, 5:6], None, ALU.mult, None,
                                        [t_ex[b], t_rt[b]], [t_gated]))

                def p7():
                    k.tt("dve", rankd[:, i, :], ps_r[:, 32:64], basec[:], ALU.add, [t_psr, t_basec], [t_rankd])
                    k.tt("dve", basec[:], ps_r[:, 64:96], basec[:], ALU.add, [t_psr, t_basec], [t_basec])
                pcs.append(p7)
                return pcs

            for i0_ in range(0, NT, 2):
                run_pieces([tile_pieces(i0_), tile_pieces(i0_ + 1)])
            P.barrier()
            A.release()

        if stages >= 4:
            A.mark()
            jrow = A.alloc([128, JMAX], F32); t_jrow = T("jrow")
            cmp3 = A.alloc([128, NE, JMAX], F32); t_cmp3 = T("cmp3")
            nb = A.alloc([128, NE], F32); t_nb = T("nb")
            incl = A.alloc([128, NE], F32); t_incl = T("incl")
            pst = A.alloc([128, NE], F32); t_pst = T("pst")
            onesf = A.alloc([128, NE], F32); t_onesf = T("onesf")
            jb_ = A.alloc([128, NBLK], F32); t_jb = T("jb")
            cmpb = A.alloc([128, NBLK, NE], F32); t_cmpb = T("cmpb")
            be = A.alloc([128, NBLK], F32); t_be = T("be")
            widxf = A.alloc([128, NBLK, 8], F32); t_widxf = T("widxf")
            pbig = A.alloc([128, 1], F32); t_pbig = T("pbig")
            bef = A.alloc([128, NBLK], F32); t_bef = T("bef")
            key3 = A.alloc([128, NT, NE], F32); t_key3 = T("key3")
            top8 = A.alloc([128, 8], F32); t_top8 = T("top8")
            d4f = A.alloc([128, NT, 4], F32); t_d4f = T("d4f")
            jk32 = A.alloc([128, NE], F32); t_jk32 = T("jk32")
            h2l = [A.alloc([128, D], BF16) for _ in range(3)]; t_h2l = [T(f"h2l{i}") for i in range(3)]
            k.iota(jrow[:], [[BLK, JMAX]], 0, 0, [t_jrow])
            k.iota(jb_[:], [[1, NBLK]], 0, 0, [t_jb])
            k.memset("pool", onesf[:], 1.0, [t_onesf])
            k.tt("dve", cmp3[:], jrow[:].unsqueeze(1).to_broadcast([128, NE, JMAX]),
                 basec[:].unsqueeze(2).to_broadcast([128, NE, JMAX]), ALU.is_lt, [t_jrow, t_basec], [t_cmp3])
            P.add("dve", lambda e: e.tensor_reduce(out=nb[:], in_=cmp3[:], axis=AX.X, op=ALU.add), [t_cmp3], [t_nb])
            P.add("dve", lambda e: e.tensor_tensor_scan(out=incl[:], data0=onesf[:], data1=nb[:], initial=0.0,
                                                        op0=ALU.mult, op1=ALU.add), [t_onesf, t_nb], [t_incl])
            k.tt("dve", pst[:], incl[:], nb[:], ALU.subtract, [t_incl, t_nb], [t_pst])
            k.ts("dve", pst[:], pst[:], float(BLK), None, ALU.mult, None, [t_pst], [t_pst])
            k.tt("dve", cmpb[:], incl[:].unsqueeze(1).to_broadcast([128, NBLK, NE]),
                 jb_[:].unsqueeze(2).to_broadcast([128, NBLK, NE]), ALU.is_le, [t_incl, t_jb], [t_cmpb])
            P.add("dve", lambda e: e.tensor_reduce(out=be[:], in_=cmpb[:], axis=AX.X, op=ALU.add), [t_cmpb], [t_be])
            k.ts("dve", be[:], be[:], float(NE - 1), None, ALU.min, None, [t_be], [t_be])
            k.cp("dve", bidx[:], be[:], [t_be], [t_bidx])
            k.ts("dve", be[:], be[:], 1024.0, pidx[:, 0:1], ALU.mult, ALU.add, [t_be, t_pidx], [t_be])
            for kc in range(KC):
                k.ts("dve", widxf[:, :, kc], be[:], float(kc * 128), None, ALU.add, None, [t_be], [t_widxf])
            k.cp("dve", widx[:].rearrange("p b c -> p (b c)"), widxf[:].rearrange("p b c -> p (b c)"),
                 [t_widxf], [t_widx])
            k.tt("dve", key3[:], rankd[:], pst[:].unsqueeze(1).to_broadcast([128, NT, NE]), ALU.add,
                 [t_rankd, t_pst], [t_key3])
            k.ts("dve", key3[:], key3[:], -1.0, BIGC, ALU.mult, ALU.add, [t_key3], [t_key3])
            k.tt("dve", key3[:], key3[:], maskd[:], ALU.mult, [t_key3, t_maskd], [t_key3])
            for i in range(NT):
                P.add("dve", (lambda o_, i_: (lambda e: e.max(out=o_, in_=i_)))(top8[:], key3[:, i, :]),
                      [t_key3], [t_top8])
                k.ts("dve", d4f[:, i, :], top8[:, 0:4], -1.0, BIGC, ALU.mult, ALU.add, [t_top8], [t_d4f])
                for k4 in range(4):
                    k.stt(jk32[:], key3[:, i, :], top8[:, k4:k4 + 1], gated[:, i, :], ALU.is_equal, ALU.mult,
                          [t_key3, t_top8, t_gated], [t_jk32, t_gate4], accum=gate4[:, i, k4:k4 + 1])
            k.cp("dve", dest4[:].rearrange("p t f -> p (t f)"), d4f[:].rearrange("p t f -> p (t f)"),
                 [t_d4f], [t_dest4])
            if debug:
                k.dma("sp", RTD[:, 0:NT * 4], d4f[:].rearrange("p t f -> p (t f)"), [t_d4f], [t_dram["RTD"]])
                k.dma("sp", RTD[:, NT * 4:NT * 8], gate4[:].rearrange("p t f -> p (t f)"), [t_gate4], [t_dram["RTD"]])
            for i in range(NT):
                hb_ = h2l[i % 3]; thb = t_h2l[i % 3]
                k.dma("sp", hb_[:], H2[i * 128:(i + 1) * 128, :], [t_dram["H2"]], [thb])
                for k4 in range(4):
                    k.scatter(HG[:, :], hb_[:], dest4[:, i, k4:k4 + 1], [thb, t_dest4, t_dram["HG"]], [t_dram["HG"]])
            P.barrier()
            A.release()
            A.release()

            A.mark()
            w1sb = [A.alloc([128, 9 * 2048], BF16) for _ in range(2)]; t_w1sb = [T("w1sb0"), T("w1sb1")]
            w2sb = [A.alloc([128, 9 * 1024], BF16) for _ in range(2)]; t_w2sb = [T("w2sb0"), T("w2sb1")]
            hg = [A.alloc([128, 4, D], BF16) for _ in range(2)]; t_hg = [T("hg0"), T("hg1")]
            hgT = A.alloc([128, KC, 512], BF16); t_hgT = [T(f"hgT{c}") for c in range(KC)]
            actT = A.alloc([128, KC, 512], BF16); t_actT = [T(f"actT{c}") for c in range(KC)]
            NE4 = 6
            ew = [A.alloc([128, 512], F32) for _ in range(NE4)]; t_ew = [T(f"ew{i}") for i in range(NE4)]
            ysb = [A.alloc([128, D], F32) for _ in range(2)]; t_ysb = [T("ysb0"), T("ysb1")]
            ewi = [0]

            def new_ew():
                i = ewi[0] % NE4; ewi[0] += 1
                return ew[i], t_ew[i]

            ps_t4 = [PSH[0][0].bitcast(BF16), PSH[0][1].bitcast(BF16)]; t_pst4 = [t_psh[0][0], t_psh[0][1]]
            ps_gl = [(PSH[1][0], t_psh[1][0], PSH[1][1], t_psh[1][1]), (PSH[2][0], t_psh[2][0], PSH[2][1], t_psh[2][1])]
            ps_y = [PSH[3][0], PSH[3][1]]; t_psy = [t_psh[3][0], t_psh[3][1]]
            ntr = 0; ngl = 0; ny = 0; nwf = 0
            NWS = 4
            wst = [A.alloc([128, 2048], F32) for _ in range(NWS)]; t_wst = [T(f"wst{i}") for i in range(NWS)]
            w1rows = w1_d.rearrange("e k n -> (e k) n")
            w2rows = w2_d.rearrange("e k n -> (e k) n")
            nwf_ = [0]

            def wsteps(jb):
                b = jb % 2
                for kc in range(KC):
                    s_ = nwf_[0] % NWS; nwf_[0] += 1
                    k.gather(wst[s_][:, :], w1rows[:, :], widx[:, jb, kc:kc + 1], [t_widx], [t_wst[s_]])
                    k.cp("act",
                         w1sb[b][:, kc * 2048:(kc + 1) * 2048].rearrange("p (two f) -> p two f", two=2),
                         wst[s_][:, :].rearrange("p (f two) -> p two f", two=2), [t_wst[s_]], [t_w1sb[b]])
                    yield
                s_ = nwf_[0] % NWS; nwf_[0] += 1
                k.gather(wst[s_][:, :], b1_d[:, :], bidx[:, jb:jb + 1], [t_bidx], [t_wst[s_]])
                k.cp("dve", w1sb[b][0:1, 8 * 2048:9 * 2048].rearrange("p (two f) -> p two f", two=2),
                     wst[s_][0:1, :].rearrange("p (f two) -> p two f", two=2), [t_wst[s_]], [t_w1sb[b]])
                yield
                for kc in range(KC):
                    s_ = nwf_[0] % NWS; nwf_[0] += 1
                    k.gather(wst[s_][:, 0:1024], w2rows[:, :], widx[:, jb, kc:kc + 1], [t_widx], [t_wst[s_]])
                    k.cp("dve", w2sb[b][:, kc * 1024:(kc + 1) * 1024], wst[s_][:, 0:1024], [t_wst[s_]], [t_w2sb[b]])
                    yield
                s_ = nwf_[0] % NWS; nwf_[0] += 1
                k.gather(wst[s_][:, 0:1024], b2_d[:, :], bidx[:, jb:jb + 1], [t_bidx], [t_wst[s_]])
                k.ts("dve", w2sb[b][0:1, 8 * 1024:9 * 1024], wst[s_][0:1, 0:1024], 1.702, None, ALU.mult, None,
                     [t_wst[s_]], [t_w2sb[b]])
                yield

            def adv(gen, n_=1):
                if gen is None:
                    return
                for _ in range(n_):
                    try:
                        next(gen)
                    except StopIteration:
                        return

            g0 = wsteps(0)
            adv(g0, 100)
            for jb in range(NBLK):
                b = jb % 2
                wgen = wsteps(jb + 1) if jb + 1 < NBLK else None
                k.dma("sp", hg[b][:], HG[jb * BLK:(jb + 1) * BLK, :].rearrange("(a p) d -> p a d", p=128),
                      [t_dram["HG"]], [t_hg[b]])
                for kc in range(KC):
                    pt_ = ps_t4[ntr % 2]; tpt_ = t_pst4[ntr % 2]; ntr += 1
                    for a_ in range(4):
                        k.tr(pt_[:, a_ * 128:(a_ + 1) * 128], hg[b][:, a_, kc * 128:(kc + 1) * 128], ident[:],
                             [t_hg[b], t_id], [tpt_])
                    k.cp("act" if kc % 2 == 0 else "dve", hgT[:, kc, :], pt_[:, 0:512], [tpt_], [t_hgT[kc]])
                    adv(wgen)
                for fc in range(KC):
                    pg, tpg, pl, tpl = ps_gl[ngl % 2]; ngl += 1
                    for (pz_, tz_, off) in ((pg, tpg, 0), (pl, tpl, 1024)):
                        for kc in range(KC):
                            k.mm(pz_[:, :], w1sb[b][:, kc * 2048 + off + fc * 128:kc * 2048 + off + (fc + 1) * 128],
                                 hgT[:, kc, :], kc == 0, False, [t_w1sb[b], t_hgT[kc]], [tz_])
                        k.mm(pz_[:, :], w1sb[b][0:1, 8 * 2048 + off + fc * 128:8 * 2048 + off + (fc + 1) * 128],
                             ones_b[0:1, 0:512], False, True, [t_w1sb[b], t_ones], [tz_])
                    g_, tg_ = new_ew()
                    k.ts("dve", g_[:], pg[:, :], 7.0, None, ALU.min, None, [tpg], [tg_])
                    sl, tsl = new_ew()
                    k.act(sl[:], g_[:], AF.Silu, [tg_], [tsl], scale=1.702)
                    l_, tl_ = new_ew()
                    k.ts("dve", l_[:], pl[:, :], -7.0, 7.0, ALU.max, ALU.min, [tpl], [tl_])
                    k.stt(actT[:, fc, :], l_[:], 1.0, sl[:], ALU.add, ALU.mult, [tl_, tsl], [t_actT[fc]])
                    adv(wgen)
                for a_ in range(4):
                    yb = ysb[ny % 2]; tyb = t_ysb[ny % 2]; ny += 1
                    for dh in range(2):
                        py = ps_y[dh]; tpy = t_psy[dh]
                        for fc in range(KC):
                            k.mm(py[:, :], actT[:, fc, a_ * 128:(a_ + 1) * 128],
                                 w2sb[b][:, fc * 1024 + dh * 512:fc * 1024 + (dh + 1) * 512], fc == 0, False,
                                 [t_actT[fc], t_w2sb[b]], [tpy])
                        k.mm(py[:, :], ones_b[0:1, 0:128], w2sb[b][0:1, 8 * 1024 + dh * 512:8 * 1024 + (dh + 1) * 512],
                             False, True, [t_ones, t_w2sb[b]], [tpy])
                        k.act(yb[:, dh * 512:(dh + 1) * 512], py[:, :], AF.Copy, [tpy], [tyb], scale=1.0 / 1.702)
                    r0 = jb * BLK + a_ * 128
                    k.dma("sp", YY[r0:r0 + 128, :], yb[:], [tyb], [t_dram["YY"]])
                    adv(wgen)
                adv(wgen, 100)
            P.barrier()
            A.release()

            A.mark()
            g2bc = A.alloc([128, D], F32); t_g2bc = T("g2bc")
            bc_row(g2bc, 40, t_g2bc)
            x1l = [A.alloc([128, D], F32) for _ in range(2)]; t_x1l = [T("x1l0"), T("x1l1")]
            yg = [[A.alloc([128, D], F32) for _ in range(4)] for _ in range(2)]
            t_yg = [[T(f"yg{b}{q}") for q in range(4)] for b in range(2)]
            acc = [A.alloc([128, D], F32) for _ in range(2)]; t_acc = [T("acc0"), T("acc1")]
            for i in range(NT):
                b = i % 2
                cols = slice(i * 128, (i + 1) * 128)
                k.dma("sp", x1l[b][:], X1[cols, :], [t_dram["X1"]], [t_x1l[b]])
                for k4 in range(4):
                    k.gather(yg[b][k4][:, :], YY[:, :], dest4[:, i, k4:k4 + 1], [t_dest4, t_dram["YY"]], [t_yg[b][k4]])
                k.ts("dve", acc[b][:], yg[b][0][:], gate4[:, i, 0:1], None, ALU.mult, None,
                     [t_yg[b][0], t_gate4], [t_acc[b]])
                for k4 in range(1, 4):
                    k.stt(acc[b][:], yg[b][k4][:], gate4[:, i, k4:k4 + 1], acc[b][:], ALU.mult, ALU.add,
                          [t_yg[b][k4], t_gate4, t_acc[b]], [t_acc[b]])
                k.tt("pool", acc[b][:], acc[b][:], g2bc[:], ALU.mult, [t_acc[b], t_g2bc], [t_acc[b]])
                k.tt("dve", acc[b][:], acc[b][:], x1l[b][:], ALU.add, [t_acc[b], t_x1l[b]], [t_acc[b]])
                k.dma("sp", out_d[cols, :], acc[b][:], [t_acc[b]], [t_dram["OUT"]])
            A.release()
        elif stages >= 1:
            A.mark()
            zt2 = A.alloc([128, D], F32); t_zt2 = T("zt2")
            k.memset("pool", zt2[:], 0.0, [t_zt2])
            k.dma("sp", out_d[0:128, :], zt2[:], [t_zt2], [t_dram["OUT"]])
            A.release()
        P.emit()
    return nc


def _fm(v, nchunk):
    return np.ascontiguousarray(np.asarray(v, np.float32).reshape(nchunk, 128).T)


def prep_shared(inp, small=False):
    L = 0
    f32 = np.float32
    sh = {}
    sh["w_ada"] = np.ascontiguousarray(inp["w_ada"][L], f32)
    sh["b_ada_fm"] = _fm(inp["b_ada"][L], 48)
    sh["n1g_fm"] = _fm(inp["norm1_g"][L], 8)
    sh["n2g_fm"] = _fm(inp["norm2_g"][L], 8)
    sh["w_in"] = np.ascontiguousarray(inp["w_in"][L], f32)
    cw = np.asarray(inp["conv_w"][L], f32)
    sh["convw_fm"] = np.ascontiguousarray(cw.T.reshape(4, 128, 4).transpose(1, 0, 2).reshape(128, 16))
    v4 = np.zeros((128, 28), f32)
    for j, name in enumerate(["conv_b", "b_rg_a", "b_rg_x", "lru_lambda", "rg_out_g", "attn_out_g"]):
        v4[:, j * 4:(j + 1) * 4] = _fm(inp[name][L], 4)
    v4[:, 24] = np.tile(np.asarray(inp["q_norm_g"][L], f32), 2)
    v4[:, 25] = np.tile(np.asarray(inp["k_norm_g"][L], f32), 2)
    v4[:, 26] = np.tile(np.asarray(inp["kidx_norm_g"][L], f32), 2)
    sh["vec4_fm"] = v4
    for nm, key in (("wabd", "w_rg_a"), ("wxbd", "w_rg_x")):
        w = np.asarray(inp[key][L], f32)
        bd = np.zeros((128, 4, 128), f32)
        for c in range(4):
            bd[0:64, c, 0:64] = w[2 * c]
            bd[64:128, c, 64:128] = w[2 * c + 1]
        sh[nm] = bd.reshape(128, 512)
    sh["w_out"] = np.ascontiguousarray(inp["w_out"][L], f32)
    wr = np.asarray(inp["w_router"][L], f32)
    sh["w_rt_fm"] = np.ascontiguousarray(wr.reshape(8, 128, 32).transpose(1, 0, 2).reshape(128, 256))
    sh["b_rt"] = np.asarray(inp["b_router"][L], f32).reshape(1, 32)
    if not small:
        sh["w1"] = np.ascontiguousarray(inp["w1"][L], f32)
        sh["b1"] = np.ascontiguousarray(inp["b1"][L], f32)
        sh["w2"] = np.ascontiguousarray(inp["w2"][L], f32)
        sh["b2"] = np.ascontiguousarray(inp["b2"][L], f32)
    return sh


def kernel(**inputs):
    x = np.asarray(inputs["x"], np.float32)
    c = np.asarray(inputs["c"], np.float32)
    B, S, _ = x.shape
    sh = prep_shared(inputs)
    nc = build_nc(S)
    in_maps = []
    for b in range(B):
        m = dict(sh)
        m["x"] = np.ascontiguousarray(x[b])
        m["c_fm"] = _fm(c[b], 8)
        in_maps.append(m)
    res = run_bass_kernel_spmd(nc, in_maps, core_ids=list(range(B)))
    return np.stack([np.asarray(r["out"], np.float32) for r in res.results], axis=0)
```

```python
import numpy as np
import concourse.bass as bass
import concourse.mybir as mybir
from concourse.bass_utils import run_bass_kernel_spmd

F32 = mybir.dt.float32
BF16 = mybir.dt.bfloat16
I32 = mybir.dt.int32
U32 = mybir.dt.uint32
U8 = mybir.dt.uint8
ALU = mybir.AluOpType
AF = mybir.ActivationFunctionType
AX = mybir.AxisListType
DSZ = {F32: 4, BF16: 2, I32: 4, U32: 4, U8: 1}


class Tok:
    __slots__ = ("name", "lw", "rd", "excl")

    def __init__(self, name, excl=False):
        self.name = name
        self.lw = None
        self.rd = []
        self.excl = excl


class _Op:
    __slots__ = ("eng", "fn", "deps", "idx", "dma", "sem", "val", "used", "slot_prev")

    def __init__(self, eng, fn, deps, idx, dma):
        self.eng = eng
        self.fn = fn
        self.deps = deps
        self.idx = idx
        self.dma = dma
        self.sem = None
        self.val = 0
        self.used = False
        self.slot_prev = None


class Prog:
    ENGS = ("pe", "act", "dve", "pool", "sp")
    NSLOT = 12
    SEG = 20000

    def __init__(self, nc):
        self.nc = nc
        self.ops = []
        self.by_eng = {e: [] for e in self.ENGS}
        self.pending_barrier = {e: [] for e in self.ENGS}
        self.recent_dma = {e: [] for e in self.ENGS}

    def add(self, eng, fn, rd=(), wr=(), dma=False):
        ex = [t for t in rd if t.excl]
        if ex:
            rd = [t for t in rd if not t.excl]
            wr = list(wr) + ex
        deps = set()
        for t in rd:
            if t.lw is not None:
                deps.add(t.lw)
        for t in wr:
            if t.lw is not None:
                deps.add(t.lw)
            deps.update(t.rd)
        if self.pending_barrier[eng]:
            deps.update(self.pending_barrier[eng])
            self.pending_barrier[eng] = []
        idx = len(self.ops)
        op = _Op(eng, fn, sorted(deps), idx, dma)
        self.ops.append(op)
        self.by_eng[eng].append(op)
        for t in rd:
            t.rd.append(idx)
        for t in wr:
            t.lw = idx
            t.rd = []
        if dma:
            r = self.recent_dma[eng]
            r.append(idx)
            if len(r) > self.NSLOT:
                r.pop(0)
        return idx

    def barrier(self):
        last = []
        for e in self.ENGS:
            ops = self.by_eng[e]
            for o in reversed(ops):
                if not o.dma:
                    last.append(o.idx)
                    break
            last.extend(self.recent_dma[e])
        for e in self.ENGS:
            self.pending_barrier[e] = list(set(self.pending_barrier[e]) | set(last))

    def emit(self, final_wait_eng="sp"):
        nc = self.nc
        ops = self.ops
        self.barrier()
        fin = self.pending_barrier[final_wait_eng]
        for o in ops:
            for d in o.deps:
                ops[d].used = True
        for d in fin:
            ops[d].used = True
        nsem = 0
        plan = {}
        for e in self.ENGS:
            ncomp = sum(1 for o in self.by_eng[e] if (not o.dma) and o.used)
            nseg = (ncomp + self.SEG - 1) // self.SEG
            ndma = self.NSLOT if any(o.dma for o in self.by_eng[e]) else 0
            plan[e] = (nseg, ndma)
            nsem += nseg + ndma
        import contextlib

        with contextlib.ExitStack() as st:
            sems = [st.enter_context(nc.semaphore(f"s{i}")) for i in range(nsem)]
            si = 0
            for e in self.ENGS:
                nseg, ndma = plan[e]
                seg = sems[si:si + nseg]
                si += nseg
                slots = sems[si:si + ndma]
                si += ndma
                slot_cnt = [0] * ndma
                k = 0
                c = 0
                for o in self.by_eng[e]:
                    if o.dma:
                        s = k % ndma
                        k += 1
                        o.slot_prev = (slots[s], slot_cnt[s]) if slot_cnt[s] else None
                        slot_cnt[s] += 16
                        o.sem, o.val = slots[s], slot_cnt[s]
                    elif o.used:
                        o.sem = seg[c // self.SEG]
                        o.val = (c % self.SEG) + 1
                        c += 1
            block = st.enter_context(nc.Block())

            def run(eng_name):
                def body(e):
                    waited = {}

                    def wait(sem, val):
                        key = id(sem)
                        if waited.get(key, 0) >= val:
                            return
                        waited[key] = val
                        e.wait_ge(sem, val)

                    for o in self.by_eng[eng_name]:
                        for d in o.deps:
                            p = ops[d]
                            if p.eng == "pe" and eng_name == "pe" and not p.dma:
                                continue
                            wait(p.sem, p.val)
                        if o.slot_prev is not None:
                            wait(*o.slot_prev)
                        ins = o.fn(e)
                        if o.dma:
                            ins.then_inc(o.sem, 16)
                        elif o.used:
                            ins.then_inc(o.sem, 1)
                    if eng_name == final_wait_eng:
                        for d in fin:
                            wait(ops[d].sem, ops[d].val)
                return body

            block.tensor(run("pe"))
            block.scalar(run("act"))
            block.vector(run("dve"))
            block.gpsimd(run("pool"))
            block.sync(run("sp"))


class Arena:
    def __init__(self, nc, st, nbytes, name="arena"):
        self.t = st.enter_context(nc.sbuf_tensor(name, [128, nbytes], U8))
        self.nbytes = nbytes
        self.off = 0
        self.marks = []

    def alloc(self, shape, dtype, name=None):
        assert shape[0] <= 128
        free = int(np.prod(shape[1:]))
        nb = free * DSZ[dtype]
        nb_al = (nb + 63) // 64 * 64
        assert self.off + nb_al <= self.nbytes, (self.off, nb_al, self.nbytes, name)
        ap = self.t[0:shape[0], self.off:self.off + nb]
        self.off += nb_al
        if dtype != U8:
            ap = ap.bitcast(dtype)
        if len(shape) > 2:
            names = " ".join(f"d{i}" for i in range(1, len(shape)))
            kw = {f"d{i}": shape[i] for i in range(1, len(shape))}
            ap = ap.rearrange(f"p ({names}) -> p {names}", **kw)
        return ap

    def mark(self):
        self.marks.append(self.off)

    def release(self):
        self.off = self.marks.pop()

import contextlib

D = 1024
KC = 8
NE = 32
EPS = 1e-6
BLK = 512
NIT = 14
BIGC = float(2 ** 20)


class K:
    def __init__(self, P):
        self.P = P

    def mm(self, out, lhsT, rhs, start, stop, rd, wr):
        self.P.add("pe", lambda e: e.matmul(out, lhsT=lhsT, rhs=rhs, start=start, stop=stop,
                                            skip_group_check=True), rd, wr)

    def tr(self, out, in_, ident, rd, wr):
        self.P.add("pe", lambda e: e.transpose(out=out, in_=in_, identity=ident), rd, wr)

    def act(self, out, in_, func, rd, wr, scale=1.0, bias=0.0, accum=None, eng="act"):
        if accum is None:
            self.P.add(eng, lambda e: e.activation(out=out, in_=in_, func=func, scale=scale, bias=bias), rd, wr)
        else:
            self.P.add(eng, lambda e: e.activation(out=out, in_=in_, func=func, scale=scale, bias=bias,
                                                   accum_out=accum), rd, wr)

    def ts(self, eng, out, in0, s1, s2, op0, op1, rd, wr, accum=None):
        if op1 is None:
            self.P.add(eng, lambda e: e.tensor_scalar(out=out, in0=in0, scalar1=s1, scalar2=None, op0=op0), rd, wr)
        elif accum is None:
            self.P.add(eng, lambda e: e.tensor_scalar(out=out, in0=in0, scalar1=s1, scalar2=s2, op0=op0, op1=op1), rd, wr)
        else:
            self.P.add(eng, lambda e: e.tensor_scalar(out=out, in0=in0, scalar1=s1, scalar2=s2, op0=op0, op1=op1,
                                                      accum_out=accum), rd, wr)

    def tt(self, eng, out, in0, in1, op, rd, wr):
        self.P.add(eng, lambda e: e.tensor_tensor(out=out, in0=in0, in1=in1, op=op), rd, wr)

    def stt(self, out, in0, scalar, in1, op0, op1, rd, wr, accum=None):
        if accum is None:
            self.P.add("dve", lambda e: e.scalar_tensor_tensor(out=out, in0=in0, scalar=scalar, in1=in1,
                                                              op0=op0, op1=op1), rd, wr)
        else:
            self.P.add("dve", lambda e: e.scalar_tensor_tensor(out=out, in0=in0, scalar=scalar, in1=in1,
                                                              op0=op0, op1=op1, accum_out=accum), rd, wr)

    def cp(self, eng, out, in_, rd, wr):
        if eng == "act":
            self.P.add("act", lambda e: e.activation(out=out, in_=in_, func=AF.Copy), rd, wr)
        else:
            self.P.add(eng, lambda e: e.tensor_copy(out=out, in_=in_), rd, wr)

    def memset(self, eng, ap, val, wr):
        self.P.add(eng, lambda e: e.memset(ap, val), (), wr)

    def dma(self, q, out, in_, rd, wr):
        self.P.add(q, lambda e: e.dma_start(out=out, in_=in_), rd, wr, dma=True)

    def gather(self, out, in_, idx, rd, wr, bounds=None):
        if bounds is None:
            self.P.add("pool", lambda e: e.indirect_dma_start(
                out=out, out_offset=None, in_=in_,
                in_offset=bass.IndirectOffsetOnAxis(ap=idx, axis=0)), rd, wr, dma=True)
        else:
            self.P.add("pool", lambda e: e.indirect_dma_start(
                out=out, out_offset=None, in_=in_,
                in_offset=bass.IndirectOffsetOnAxis(ap=idx, axis=0),
                bounds_check=bounds, oob_is_err=False), rd, wr, dma=True)

    def scatter(self, out, in_, idx, rd, wr):
        self.P.add("pool", lambda e: e.indirect_dma_start(
            out=out, out_offset=bass.IndirectOffsetOnAxis(ap=idx, axis=0),
            in_=in_, in_offset=None), rd, wr, dma=True)

    def recip(self, out, in_, rd, wr):
        self.P.add("dve", lambda e: e.reciprocal(out=out, in_=in_), rd, wr)

    def iota(self, out, pattern, base, cm, wr):
        self.P.add("pool", lambda e: e.iota(out, pattern=pattern, base=base, channel_multiplier=cm,
                                            allow_small_or_imprecise_dtypes=True), (), wr)


def build_nc(S, stages=5, debug=False):
    NT = S // 128
    NG = S // 512
    NSEL = min(256, S // 4)
    NSLOT = 4 * S + NE * BLK
    NBLK = NSLOT // BLK
    JMAX = S // BLK

    nc = bass.Bass("TRN2", target_bir_lowering=False)

    def din(name, shape, dt=F32):
        return nc.dram_tensor(name, list(shape), dt, kind="ExternalInput").ap()

    def dscr(name, shape, dt):
        kind = "ExternalOutput" if debug else "Internal"
        return nc.dram_tensor(name, list(shape), dt, kind=kind).ap()

    x_d = din("x", [S, D])
    c_d = din("c_fm", [128, KC])
    w_ada_d = din("w_ada", [D, 6 * D])
    b_ada_d = din("b_ada_fm", [128, 48])
    n1g_d = din("n1g_fm", [128, KC])
    n2g_d = din("n2g_fm", [128, KC])
    w_in_d = din("w_in", [D, 3144])
    convw_d = din("convw_fm", [128, 16])
    vec4_d = din("vec4_fm", [128, 28])
    wabd_d = din("wabd", [128, 4 * 128])
    wxbd_d = din("wxbd", [128, 4 * 128])
    w_out_d = din("w_out", [D, D])
    w_rt_d = din("w_rt_fm", [128, KC * NE])
    b_rt_d = din("b_rt", [1, NE])
    if stages >= 4:
        w1_d = din("w1", [NE, D, 2 * D])
        b1_d = din("b1", [NE, 2 * D])
        w2_d = din("w2", [NE, D, D])
        b2_d = din("b2", [NE, D])
    import os as _os
    CUT = int(_os.environ.get("P1CUT", "99"))
    out_d = nc.dram_tensor("out", [S, D], F32, kind="ExternalOutput").ap()

    MODROW = dscr("modrow", [64, 128], F32)
    QT = dscr("qt", [4, 128, S], BF16)
    KV = dscr("kv", [NT, 128, 1032], BF16)
    QIT = dscr("qit", [4, 128, S], BF16)
    KIT = dscr("kit", [128, S], BF16)
    WI = dscr("wi", [NT, 128, 8], F32)
    YR = dscr("yr", [4, 128, S], BF16)
    YA = dscr("ya", [4, 128, S], BF16)
    X1 = dscr("x1", [S, D], F32)
    H2 = dscr("h2", [S, D], BF16)
    HG = dscr("hg", [NSLOT, D], BF16)
    YY = dscr("yy", [NSLOT, D], F32)
    RTD = dscr("rtd", [128, NT * 8], F32)

    P = Prog(nc)
    k = K(P)
    T = Tok
    st = contextlib.ExitStack()
    with st:
        A = Arena(nc, st, 206 * 1024)
        psT = [st.enter_context(nc.psum_tensor(f"ps{i}", [128, 1024], F32)) for i in range(4)]
        PSH = [[psT[i][:, 0:512], psT[i][:, 512:1024]] for i in range(4)]
        t_psh = [[Tok(f"ps{i}a", True), Tok(f"ps{i}b", True)] for i in range(4)]
        t_dram = {n_: T(n_) for n_ in ("QT", "KV", "QIT", "KIT", "WI", "YR", "YA", "X1", "H2", "HG", "YY",
                                      "MODROW", "RTD", "OUT")}

        io = A.alloc([128, 128], F32); t_io = T("io")
        identf = A.alloc([128, 128], F32); t_idf = T("identf")
        ident = A.alloc([128, 128], BF16); t_id = T("ident")
        ones_b = A.alloc([128, 512], BF16); t_ones = T("ones")
        bo64 = A.alloc([128, 128], BF16); t_bo = T("bo64")
        ustr = A.alloc([128, 128], BF16); t_ustr = T("ustr")
        modfm = A.alloc([128, 64], F32); t_mod = T("modfm")
        vec4 = A.alloc([128, 28], F32); t_vec4 = T("vec4")
        convw = A.alloc([128, 16], F32); t_convw = T("convw")
        nsp = A.alloc([128, 4], F32); t_nsp = T("nsp")
        pidx = A.alloc([128, 1], F32); t_pidx = T("pidx")
        dest4 = A.alloc([128, NT, 4], I32); t_dest4 = T("dest4")
        gate4 = A.alloc([128, NT, 4], F32); t_gate4 = T("gate4")
        widx = A.alloc([128, NBLK, 8], I32); t_widx = T("widx")
        bidx = A.alloc([128, NBLK], I32); t_bidx = T("bidx")

        k.iota(io[:], [[1, 128]], 0, -1, [t_io])
        k.ts("dve", identf[:], io[:], 0.0, None, ALU.is_equal, None, [t_io], [t_idf])
        k.cp("dve", ident[:], identf[:], [t_idf], [t_id])
        k.ts("dve", ustr[:], io[:], 0.0, None, ALU.is_gt, None, [t_io], [t_ustr])
        k.iota(pidx[:], [[0, 1]], 0, 1, [t_pidx])
        k.memset("pool", ones_b[:], 1.0, [t_ones])
        k.memset("pool", bo64[:], 0.0, [t_bo])
        k.memset("pool", bo64[0:64, 0:64], 1.0, [t_bo])
        k.memset("pool", bo64[64:128, 64:128], 1.0, [t_bo])
        k.dma("sp", vec4[:], vec4_d[:, :], [], [t_vec4])
        k.dma("sp", convw[:], convw_d[:, :], [], [t_convw])
        CB, BA, BX, LAM, RGG, AOG = 0, 4, 8, 12, 16, 20
        QG, KG, KIG = 24, 25, 26

        A.mark()
        csb = A.alloc([128, KC], F32); t_c = T("c")
        scs = A.alloc([128, KC], F32); t_scs = T("scs")
        bada = A.alloc([128, 48], F32); t_bada = T("bada")
        n1g = A.alloc([128, KC], F32); t_n1g = T("n1g")
        n2g = A.alloc([128, KC], F32); t_n2g = T("n2g")
        wa_buf = [A.alloc([128, KC, 768], F32) for _ in range(2)]
        t_wa = [T("wa0"), T("wa1")]
        modT = A.alloc([64, 128], F32); t_modT = T("modT")
        tmp4 = A.alloc([128, 4], F32); t_tmp4 = T("tmp4")
        k.dma("sp", csb[:], c_d[:, :], [], [t_c])
        k.dma("sp", bada[:], b_ada_d[:, :], [], [t_bada])
        k.dma("sp", n1g[:], n1g_d[:, :], [], [t_n1g])
        k.dma("sp", n2g[:], n2g_d[:, :], [], [t_n2g])
        k.act(scs[:], csb[:], AF.Silu, [t_c], [t_scs])
        pmod = PSH[0][0]
        for jg in range(8):
            wb_ = wa_buf[jg % 2]
            k.dma("sp", wb_[:], w_ada_d[:, jg * 768:(jg + 1) * 768].rearrange("(kc p) n -> p kc n", p=128),
                  [], [t_wa[jg % 2]])
            for jj in range(6):
                j = jg * 6 + jj
                for kc in range(KC):
                    k.mm(pmod[:, j:j + 1], wb_[:, kc, jj * 128:(jj + 1) * 128], scs[:, kc:kc + 1],
                         kc == 0, kc == KC - 1, [t_wa[jg % 2], t_scs], [t_psh[0][0]])
        k.tt("dve", modfm[:, 0:48], pmod[:, 0:48], bada[:], ALU.add, [t_psh[0][0], t_bada], [t_mod])
        k.stt(modfm[:, 48:56], modfm[:, 8:16], 1.0, n1g[:], ALU.add, ALU.mult, [t_mod, t_n1g], [t_mod])
        k.stt(modfm[:, 56:64], modfm[:, 32:40], 1.0, n2g[:], ALU.add, ALU.mult, [t_mod, t_n2g], [t_mod])
        pTm = PSH[0][1]
        k.tr(pTm[0:64, 0:128], modfm[:, 0:64], identf[:], [t_mod, t_idf], [t_psh[0][1]])
        k.cp("dve", modT[:], pTm[0:64, 0:128], [t_psh[0][1]], [t_modT])
        k.dma("sp", MODROW[:, :], modT[:], [t_modT], [])
        mr = MODROW.rearrange("j p -> (j p)")

        def bc_row(dst, j0, tok):
            src = mr[j0 * 128:(j0 + 8) * 128].rearrange("(o n) -> o n", o=1).partition_broadcast(128)
            k.dma("sp", dst[:], src, [t_dram["MODROW"]], [tok])

        k.act(tmp4[:], vec4[:, LAM:LAM + 4], AF.Exp, [t_vec4], [t_tmp4], scale=-1.0)
        k.act(nsp[:], tmp4[:], AF.Ln, [t_tmp4], [t_nsp], bias=1.0)
        k.ts("dve", nsp[:], nsp[:], -8.0, None, ALU.mult, None, [t_nsp], [t_nsp])
        P.barrier()
        A.release()

        if stages == 0:
            A.mark()
            zt2 = A.alloc([128, D], F32); t_zt2 = T("zt2")
            k.memset("pool", zt2[:], 0.0, [t_zt2])
            k.dma("sp", out_d[0:128, :], zt2[:], [t_zt2], [])
            P.emit()
            return nc
        A.mark()
        cv_ld = [A.alloc([128, 2048], F32) for _ in range(3)]
        t_cvld = [T(f"cvld{i}") for i in range(3)]

        A.mark()
        w_in_sb = A.alloc([128, KC, 3208], BF16); t_win = T("w_in")
        wabd = A.alloc([128, 4, 128], BF16); t_wabd = T("wabd")
        wxbd = A.alloc([128, 4, 128], BF16); t_wxbd = T("wxbd")
        n = 0
        for kc in range(KC):
            sg = cv_ld[n % 3]; tg = t_cvld[n % 3]
            k.dma("sp", sg[:, 0:2048], w_in_d[kc * 128:(kc + 1) * 128, 0:2048], [], [tg])
            k.cp(["dve", "act", "pool"][n % 3], w_in_sb[:, kc, 0:2048], sg[:, 0:2048], [tg], [t_win])
            n += 1
            sg = cv_ld[n % 3]; tg = t_cvld[n % 3]
            k.dma("sp", sg[:, 0:1096], w_in_d[kc * 128:(kc + 1) * 128, 2048:3144], [], [tg])
            k.cp(["dve", "act", "pool"][n % 3], w_in_sb[:, kc, 2048:3136], sg[:, 0:1088], [tg], [t_win])
            k.cp("pool", w_in_sb[:, kc, 3136:3200], sg[:, 1024:1088], [tg], [t_win])
            k.cp("pool", w_in_sb[:, kc, 3200:3208], sg[:, 1088:1096], [tg], [t_win])
            n += 1
        for (dst, src, tk) in ((wabd, wabd_d, t_wabd), (wxbd, wxbd_d, t_wxbd)):
            sg = cv_ld[n % 3]; tg = t_cvld[n % 3]
            k.dma("sp", sg[:, 0:512], src[:, :], [], [tg])
            k.cp("dve", dst[:].rearrange("p c m -> p (c m)"), sg[:, 0:512], [tg], [tk])
            n += 1

        xt = [A.alloc([128, D], F32) for _ in range(2)]; t_xt = [T("xt0"), T("xt1")]
        xn = [A.alloc([128, D], BF16) for _ in range(2)]; t_xn = [T("xn0"), T("xn1")]
        junkb = A.alloc([128, D], BF16); t_junkb = T("junkb")
        st1 = A.alloc([128, 8], F32); t_st1 = T("st1")
        hT = [A.alloc([128, KC, 512], BF16) for _ in range(2)]; t_hT = [T("hT0"), T("hT1")]
        vsb = [A.alloc([128, 8, 65], BF16) for _ in range(2)]; t_vsb = [T("vsb0"), T("vsb1")]
        wisb = [A.alloc([128, 8], F32) for _ in range(2)]; t_wisb = [T("wisb0"), T("wisb1")]
        xr_ext = A.alloc([128, 4, 516], F32); t_xre = [T(f"xre{c}") for c in range(4)]
        carry = A.alloc([128, 4], F32); t_carry = T("carry")
        xtail = A.alloc([128, 4, 4], F32); t_xtail = T("xtail")
        NW = 8
        wf = [A.alloc([128, 512], F32) for _ in range(NW)]; t_wf = [T(f"wf{i}") for i in range(NW)]
        NWB = 6
        wb = [A.alloc([128, 512], BF16) for _ in range(NWB)]; t_wb = [T(f"wb{i}") for i in range(NWB)]
        yv = A.alloc([128, 4, 512], F32); t_yv = [T(f"yv{i}") for i in range(4)]
        xc_all = A.alloc([128, 4, 512], F32); t_xc = [T(f"xc{i}") for i in range(4)]
        xcb_all = A.alloc([128, 4, 512], BF16); t_xcb = [T(f"xcb{i}") for i in range(4)]
        ysq = A.alloc([128, 4, 512], BF16); t_ysq = [T(f"ysq{i}") for i in range(4)]
        for b in range(2):
            k.memset("pool", vsb[b][:], 1.0, [t_vsb[b]])
        k.memset("pool", xr_ext[:], 0.0, t_xre)
        k.memset("pool", carry[:], 0.0, [t_carry])
        wfi = [0]; wbi = [0]; zi = [0]

        def new_wf():
            i = wfi[0] % NW; wfi[0] += 1
            return wf[i], t_wf[i]

        def new_wb():
            i = wbi[0] % NWB; wbi[0] += 1
            return wb[i], t_wb[i]

        zbufs = [(PSH[1][0], t_psh[1][0]), (PSH[1][1], t_psh[1][1]), (PSH[2][0], t_psh[2][0])]

        def new_z():
            i = zi[0] % 3; zi[0] += 1
            return zbufs[i]

        ps_wi, t_pswi = PSH[2][1], t_psh[2][1]
        ps_ss, t_psss = PSH[3][0], t_psh[3][0]
        ps_v, t_psv = PSH[3][1], t_psh[3][1]
        gm1 = modfm[:, 48:56]
        sh1 = modfm[:, 0:8]

        for G in range(NG):
            h_T = hT[G % 2]; th = t_hT[G % 2]
            for j in range(4):
                i = 4 * G + j
                x_t = xt[i % 2]; tx = t_xt[i % 2]
                x_n = xn[i % 2]; txn = t_xn[i % 2]
                k.dma("sp", x_t[:], x_d[i * 128:(i + 1) * 128, :], [], [tx])
                k.act(junkb[:], x_t[:], AF.Square, [tx], [t_junkb, t_st1], accum=st1[:, 0:1])
                k.act(st1[:, 1:2], st1[:, 0:1], AF.Sqrt, [t_st1], [t_st1], scale=1.0 / D, bias=EPS)
                k.recip(st1[:, 2:3], st1[:, 1:2], [t_st1], [t_st1])
                k.act(x_n[:], x_t[:], AF.Copy, [tx, t_st1], [txn], scale=st1[:, 2:3])
                pt = psT[0][:, (i % 2) * 512:(i % 2 + 1) * 512].bitcast(BF16)
                tpt = t_psh[0][i % 2]
                for kc in range(KC):
                    k.tr(pt[:, kc * 128:(kc + 1) * 128], x_n[:, kc * 128:(kc + 1) * 128], ident[:],
                         [txn, t_id], [tpt])
                for kc in range(KC):
                    dst = h_T[:, kc, j * 128:(j + 1) * 128]
                    src = pt[:, kc * 128:(kc + 1) * 128]
                    if kc % 2 == 0:
                        k.act(dst, src, AF.Identity, [tpt, t_mod], [th], scale=gm1[:, kc:kc + 1],
                              bias=sh1[:, kc:kc + 1])
                    else:
                        k.ts("dve", dst, src, gm1[:, kc:kc + 1], sh1[:, kc:kc + 1], ALU.mult, ALU.add,
                             [tpt, t_mod], [th])
                if CUT < 2:
                    continue
                for kc in range(KC):
                    k.mm(ps_v[:, 0:512], h_T[:, kc, j * 128:(j + 1) * 128], w_in_sb[:, kc, 2048:2560],
                         kc == 0, kc == KC - 1, [th, t_win], [t_psv])
                for kc in range(KC):
                    k.mm(ps_wi[:, 0:8], h_T[:, kc, j * 128:(j + 1) * 128], w_in_sb[:, kc, 3200:3208],
                         kc == 0, kc == KC - 1, [th, t_win], [t_pswi])
                vb = vsb[i % 2]; tvb = t_vsb[i % 2]
                k.cp("act", vb[:, :, 0:64], ps_v[:, 0:512].rearrange("p (h d) -> p h d", d=64), [t_psv], [tvb])
                k.dma("pool", KV[i, :, 512:1032], vb[:].rearrange("p h d -> p (h d)"), [tvb], [])
                wbt = wisb[i % 2]; twb = t_wisb[i % 2]
                k.cp("dve", wbt[:], ps_wi[:, 0:8], [t_pswi], [twb])
                k.dma("pool", WI[i], wbt[:], [twb], [])

            if CUT < 3:
                continue
            cols = slice(G * 512, (G + 1) * 512)

            def zchunk(col0, M=128):
                pz, tz = new_z()
                for kc in range(KC):
                    k.mm(pz[0:M, :], w_in_sb[:, kc, col0:col0 + M], h_T[:, kc, :], kc == 0, kc == KC - 1,
                         [th, t_win], [tz])
                return pz, tz

            rnn_state = []
            for c in range(4):
                pz, tz = zchunk(c * 128)
                if G > 0:
                    k.cp("pool", xr_ext[:, c, 0:3], xtail[:, c, 0:3], [t_xtail], [t_xre[c]])
                k.cp("act", xr_ext[:, c, 3:515], pz[:, :], [tz], [t_xre[c]])
                t0, tt0 = xc_all[:, c, :], t_xc[c]
                txre = t_xre[c]
                k.ts("dve", t0[:], xr_ext[:, c, 0:512], convw[:, c * 4:c * 4 + 1], vec4[:, CB + c:CB + c + 1],
                     ALU.mult, ALU.add, [txre, t_convw, t_vec4], [tt0])
                for tap in (1, 2, 3):
                    k.stt(t0[:], xr_ext[:, c, tap:tap + 512], convw[:, c * 4 + tap:c * 4 + tap + 1], t0[:],
                          ALU.mult, ALU.add, [txre, t_convw, tt0], [tt0])
                k.cp("pool", xtail[:, c, 0:3], xr_ext[:, c, 512:515], [txre], [t_xtail])
                xcb, txcb = xcb_all[:, c, :], t_xcb[c]
                k.cp("pool", xcb, t0, [tt0], [txcb])
                rnn_state.append((t0, tt0, xcb, txcb))
            if CUT < 4:
                continue
            for c in range(4):
                pz, tz = zchunk(512 + c * 128)
                k.act(yv[:, c, :], pz[:, :], AF.Gelu_apprx_tanh, [tz], [t_yv[c]])
            if CUT < 5:
                continue
            for (base_col, gcol, dst, tkd) in ((1024, QG, QT, t_dram["QT"]), (1536, KG, None, t_dram["KV"])):
                for c in range(4):
                    pz, tz = zchunk(base_col + c * 128)
                    sq, tsq = new_wb()
                    k.act(sq[:], pz[:, :], AF.Square, [tz], [tsq])
                    qf, tqf = new_wf()
                    k.ts("dve", qf[:], pz[:, :], vec4[:, gcol:gcol + 1], None, ALU.mult, None, [tz, t_vec4], [tqf])
                    k.mm(ps_ss[:, :], bo64[:], sq[:], True, True, [t_bo, tsq], [t_psss])
                    rs, trs = new_wf()
                    k.act(rs[:], ps_ss[:, :], AF.Sqrt, [t_psss], [trs], scale=1.0 / 64, bias=EPS)
                    k.recip(rs[:], rs[:], [trs], [trs])
                    qn, tqn = new_wb()
                    k.tt("dve", qn[:], qf[:], rs[:], ALU.mult, [tqf, trs], [tqn])
                    if dst is not None:
                        k.dma("pool", dst[c, :, cols], qn[:], [tqn], [tkd])
                    else:
                        k.dma("pool", KV[4 * G:4 * G + 4, :, c * 128:(c + 1) * 128].rearrange("t p s -> p t s"),
                              qn[:].rearrange("p (t s) -> p t s", t=4), [tqn], [tkd])
            if CUT < 6:
                continue
            for c in range(4 * int(_os.environ.get("DUP", "1"))):
                c = c % 4
                pz, tz = zchunk(2560 + c * 128)
                qn, tqn = new_wb()
                k.cp("act", qn[:], pz[:, :], [tz], [tqn])
                k.dma("pool", QIT[c, :, cols], qn[:], [tqn], [])
            for _d in range(int(_os.environ.get("DVEDUP", "0"))):
                qf, tqf = new_wf()
                k.ts(_os.environ.get("DUPENG", "dve"), qf[:], xc_all[:, 0, :], 2.0, None, ALU.mult, None, [t_xc[0]], [tqf])
            if CUT < 7:
                continue
            SK = set(_os.environ.get("SKIP", "").split(","))
            pz, tz = zchunk(int(_os.environ.get("KICOL", "3072")))
            sq, tsq = new_wb()
            if "a" not in SK:
                k.act(sq[:], pz[:, :], AF.Square, [tz], [tsq])
            qf, tqf = new_wf()
            if "b" not in SK:
                k.ts("dve", qf[:], pz[:, :], vec4[:, KIG:KIG + 1], None, ALU.mult, None, [tz, t_vec4], [tqf])
            if "c" not in SK:
                k.mm(ps_ss[:, :], bo64[:], sq[:], True, True, [t_bo, tsq], [t_psss])
            rs, trs = new_wf()
            if "d" not in SK:
                k.act(rs[:], ps_ss[:, :], AF.Sqrt, [t_psss], [trs], scale=1.0 / 64, bias=EPS)
            if "e" not in SK:
                k.recip(rs[:], rs[:], [trs], [trs])
            qn, tqn = new_wb()
            if "f" not in SK:
                k.tt("dve", qn[:], qf[:], rs[:], ALU.mult, [tqf, trs], [tqn])
            if "g" not in SK:
                k.dma("sp", KIT[:, cols], qn[:], [tqn], [])
            if CUT < 8:
                continue
            for c in range(4):
                xc, txc, xcb, txcb = rnn_state[c]
                pa, tpa = new_z()
                k.mm(pa[:, :], wabd[:, c, :], xcb[:], True, True, [t_wabd, txcb], [tpa])
                r, tr_ = new_wf()
                k.act(r[:], pa[:, :], AF.Sigmoid, [tpa, t_vec4], [tr_], bias=vec4[:, BA + c:BA + c + 1])
                px, tpx = new_z()
                k.mm(px[:, :], wxbd[:, c, :], xcb[:], True, True, [t_wxbd, txcb], [tpx])
                ig, tig = new_wf()
                k.act(ig[:], px[:, :], AF.Sigmoid, [tpx, t_vec4], [tig], bias=vec4[:, BX + c:BX + c + 1])
                a, ta = new_wf()
                k.act(a[:], r[:], AF.Exp, [tr_, t_nsp], [ta], scale=nsp[:, c:c + 1])
                k.tt("pool", r[:], a[:], a[:], ALU.mult, [ta], [tr_])
                k.act(r[:], r[:], AF.Sqrt, [tr_], [tr_], scale=-1.0, bias=1.0)
                k.tt("dve", ig[:], ig[:], r[:], ALU.mult, [tig, tr_], [tig])
                k.tt("dve", ig[:], ig[:], xc[:], ALU.mult, [tig, txc], [tig])
                hsc, thsc = new_wf()
                P.add("dve", (lambda o_, a_, u_, i_: (lambda e: e.tensor_tensor_scan(
                    out=o_, data0=a_, data1=u_, initial=i_, op0=ALU.mult, op1=ALU.add)))(
                        hsc[:], a[:], ig[:], carry[:, c:c + 1]), [ta, tig, t_carry], [thsc])
                k.cp("dve", carry[:, c:c + 1], hsc[:, 511:512], [thsc], [t_carry])
                k.tt("dve", yv[:, c, :], yv[:, c, :], hsc[:], ALU.mult, [t_yv[c], thsc], [t_yv[c]])
                k.act(ysq[:, c, :], yv[:, c, :], AF.Square, [t_yv[c]], [t_ysq[c]])
            if CUT < 9:
                continue
            for c in range(4):
                k.mm(ps_ss[:, :], ones_b[:, 0:128], ysq[:, c, :], c == 0, c == 3, [t_ones, t_ysq[c]], [t_psss])
            rs, trs = new_wf()
            k.act(rs[:], ps_ss[:, :], AF.Sqrt, [t_psss], [trs], scale=1.0 / 512, bias=EPS)
            k.recip(rs[:], rs[:], [trs], [trs])
            for c in range(4):
                yn, tyn = new_wb()
                k.stt(yn[:], yv[:, c, :], vec4[:, RGG + c:RGG + c + 1], rs[:], ALU.mult, ALU.mult,
                      [t_yv[c], t_vec4, trs], [tyn])
                k.dma("pool", YR[c, :, cols], yn[:], [tyn], [])
        P.barrier()
        A.release()
        A.release()

        if stages >= 2:
            A.mark()
            kiT = A.alloc([128, S], BF16); t_kiT = T("kiT")
            score = [A.alloc([128, S], F32) for _ in range(2)]; t_score = [T("score0"), T("score1")]
            negm = [A.alloc([128, S], BF16) for _ in range(3)]; t_negm = [T("negm0"), T("negm1"), T("negm2")]
            junk8 = A.alloc([128, S], U8)
            I4 = A.alloc([128, 512], BF16); t_I4 = T("I4")
            zb = A.alloc([128, 260], BF16); t_zb = T("zb")
            pow2 = A.alloc([128, NIT + 1], F32); t_pow2 = T("pow2")
            qT_t = [A.alloc([128, 4, 128], BF16) for _ in range(2)]; t_qT = [T("qT0"), T("qT1")]
            qiT_t = [A.alloc([128, 4, 128], BF16) for _ in range(3)]; t_qiT = [T("qiT0"), T("qiT1"), T("qiT2")]
            wi_t = [A.alloc([128, 8], F32) for _ in range(3)]; t_wi = [T("wi0"), T("wi1"), T("wi2")]
            NTR = 6
            trelu = [A.alloc([128, 512], F32) for _ in range(NTR)]; t_trelu = [T(f"trelu{i}") for i in range(NTR)]
            pm = [A.alloc([128, 1024], BF16) for _ in range(2)]; t_pm = [T("pm0"), T("pm1")]
            NKV = 4
            kv = [A.alloc([128, 1032], BF16) for _ in range(NKV)]; t_kv = [T(f"kv{i}") for i in range(NKV)]
            bis = [A.alloc([128, 8], F32) for _ in range(2)]; t_bis = [T("bis0"), T("bis1")]
            dall = [A.alloc([128, NIT + 1], F32) for _ in range(2)]; t_dall = [T("dall0"), T("dall1")]
            yaf = A.alloc([128, 8, 64], F32); t_yaf = T("yaf")
            yab = A.alloc([128, 512], BF16); t_yab = T("yab")
            yaT = A.alloc([128, 4, 128], BF16); t_yaT = T("yaT")
            fin = A.alloc([128, 16], F32); t_fin = T("fin")

            k.dma("sp", kiT[:, :], KIT[:, :], [t_dram["KIT"]], [t_kiT])
            for r4 in range(4):
                k.cp("pool", I4[:, r4 * 128:(r4 + 1) * 128], ident[:], [t_id], [t_I4])
            k.memset("pool", zb[:], 0.0, [t_zb])
            for it in range(NIT + 1):
                k.memset("pool", pow2[:, it:it + 1], float(2.0 ** (-it)), [t_pow2])

            ps_i = [PSH[3][0], PSH[3][1]]; t_psi = [t_psh[3][0], t_psh[3][1]]
            psl = [psT[0], psT[1]]
            pso = [PSH[2][0], PSH[2][1]]; t_pso = [t_psh[2][0], t_psh[2][1]]
            cnt_i = [0]; cnt_v = [0]

            def A_pieces(qt):
                L = (qt + 1) * 128
                b = qt % 3
                s2 = qt % 2
                sc_ = score[s2]; tsc = t_score[s2]
                bs = bis[s2]; tbs = t_bis[s2]
                dl = dall[s2]; tdl = t_dall[s2]
                pcs = []

                def ld():
                    k.dma("sp", qiT_t[b][:], QIT.rearrange("c p s -> p c s")[:, :, qt * 128:(qt + 1) * 128],
                          [t_dram["QIT"]], [t_qiT[b]])
                    k.dma("sp", wi_t[b][:], WI[qt], [t_dram["WI"]], [t_wi[b]])
                pcs.append(ld)
                items = [(g, min(512, L - g * 512), h) for g in range((L + 511) // 512) for h in range(8)]
                LAG = 3
                used_tr = {}

                def mk(idx):
                    def pc():
                        if idx < len(items):
                            g, n_, h = items[idx]
                            c = h // 2; base = (h % 2) * 64
                            ib = cnt_i[0] % 2; itr = cnt_i[0] % NTR; cnt_i[0] += 1
                            used_tr[idx] = itr
                            k.mm(ps_i[ib][:, 0:n_], qiT_t[b][base:base + 64, c, :],
                                 kiT[base:base + 64, g * 512:g * 512 + n_], True, True,
                                 [t_qiT[b], t_kiT], [t_psi[ib]])
                            k.act(trelu[itr][:, 0:n_], ps_i[ib][:, 0:n_], AF.Relu, [t_psi[ib]], [t_trelu[itr]])
                        j = idx - LAG
                        if j >= 0:
                            g, n_, h = items[j]
                            itr = used_tr[j]
                            sc = sc_[:, g * 512:g * 512 + n_]
                            if h == 0:
                                k.ts("dve", sc, trelu[itr][:, 0:n_], wi_t[b][:, 0:1], None, ALU.mult, None,
                                     [t_trelu[itr], t_wi[b]], [tsc])
                            else:
                                k.stt(sc, trelu[itr][:, 0:n_], wi_t[b][:, h:h + 1], sc, ALU.mult, ALU.add,
                                      [t_trelu[itr], t_wi[b], tsc], [tsc])
                    return pc
                for idx in range(len(items) + LAG):
                    pcs.append(mk(idx))

                def prep():
                    if L > NSEL:
                        P.add("dve", lambda e: e.tensor_reduce(out=bs[:, 0:1], in_=sc_[:, 0:L], axis=AX.X,
                                                               op=ALU.max, apply_absolute_value=True),
                              [tsc], [tbs])
                        k.ts("dve", dl[:], pow2[:], bs[:, 0:1], None, ALU.mult, None, [t_pow2, tbs], [tdl])
                        k.memset("dve", bs[:, 1:2], 0.0, [tbs])
                        k.memset("dve", bs[:, 5:6], 0.0, [tbs])
                    else:
                        k.memset("dve", bs[:, 4:5], -1e29, [tbs])
                    k.memset("dve", sc_[0:64, L - 64:L], -1e30, [tsc])
                pcs.append(prep)
                nsplit = len(pcs)
                use_act = (qt % 2 == 1)
                if L > NSEL:
                    for it in range(NIT):
                        def pc(it=it):
                            if not use_act:
                                k.ts("dve", junk8[:, 0:L], sc_[:, 0:L], bs[:, 1:2], None, ALU.is_gt, ALU.add,
                                     [tsc, tbs], [tbs], accum=bs[:, 2:3])
                                k.ts("dve", bs[:, 3:4], bs[:, 2:3], float(NSEL), -0.5, ALU.is_ge, ALU.add,
                                     [tbs], [tbs])
                            else:
                                k.act(negm[b][:, 0:L], sc_[:, 0:L], AF.Sign, [tsc, tbs], [t_negm[b], tbs],
                                      bias=bs[:, 5:6], accum=bs[:, 2:3])
                                k.ts("dve", bs[:, 3:4], bs[:, 2:3], float(2 * NSEL - L), -0.5, ALU.is_ge, ALU.add,
                                     [tbs], [tbs])
                            k.stt(bs[:, 1:2], bs[:, 3:4], dl[:, it:it + 1], bs[:, 1:2], ALU.mult, ALU.add,
                                  [tbs, tdl], [tbs])
                            if use_act:
                                k.ts("dve", bs[:, 5:6], bs[:, 1:2], -1.0, None, ALU.mult, None, [tbs], [tbs])
                        pcs.append(pc)
                    nsplit += NIT // 4

                def fin_():
                    if L > NSEL:
                        k.tt("dve", bs[:, 4:5], bs[:, 1:2], dl[:, NIT:NIT + 1], ALU.subtract,
                             [tbs, tdl], [tbs])
                    k.ts("dve", negm[b][:, 0:L], sc_[:, 0:L], bs[:, 4:5], -30000.0, ALU.is_le, ALU.mult,
                         [tsc, tbs], [t_negm[b]])
                pcs.append(fin_)
                return pcs[:nsplit], pcs[nsplit:]

            def ld_q(qt):
                b = qt % 2
                k.dma("sp", qT_t[b][:], QT.rearrange("c p s -> p c s")[:, :, qt * 128:(qt + 1) * 128],
                      [t_dram["QT"]], [t_qT[b]])

            def B_pieces(qt):
                b = qt % 2
                nb3 = qt % 3
                pcs = []

                def init():
                    if qt + 1 < NT:
                        ld_q(qt + 1)
                    for hb in range(2):
                        k.mm(pso[hb][:, 0:260], zb[:, 0:128], zb[:, 0:260], True, False, [t_zb], [t_pso[hb]])
                pcs.append(init)
                st_ = {}

                def mkb(kt):
                    def pc():
                        if kt <= qt:
                            iv = cnt_v[0] % NKV; lb = cnt_v[0] % 2; cnt_v[0] += 1
                            st_[kt] = (iv, lb)
                            k.dma("sp", kv[iv][:], KV[kt], [t_dram["KV"]], [t_kv[iv]])
                            tl = t_psh[lb][0]
                            for half in range(2):
                                k.mm(psl[lb][:, half * 512:(half + 1) * 512], negm[nb3][:, kt * 128:(kt + 1) * 128],
                                     I4[:, :], True, False, [t_negm[nb3], t_I4], [tl])
                            for h in range(8):
                                c = h // 2; base = (h % 2) * 64
                                j = (h % 2) * 4 + h // 2
                                k.mm(psl[lb][:, j * 128:(j + 1) * 128], kv[iv][base:base + 64, c * 128:(c + 1) * 128],
                                     qT_t[b][base:base + 64, c, :], False, (h >= 6), [t_kv[iv], t_qT[b]], [tl])
                            k.act(pm[lb][:], psl[lb][:, :], AF.Exp, [tl], [t_pm[lb]], scale=0.125)
                        kp = kt - 1
                        if kp >= 0:
                            iv, lb = st_[kp]
                            for h in range(8):
                                hb = h // 4; o = (h % 4) * 65
                                j = (h % 2) * 4 + h // 2
                                k.mm(pso[hb][:, o:o + 65], pm[lb][:, j * 128:(j + 1) * 128],
                                     kv[iv][:, 512 + h * 65:512 + (h + 1) * 65], False, kp == qt,
                                     [t_pm[lb], t_kv[iv]], [t_pso[hb]])
                    return pc
                for kt in range(qt + 2):
                    pcs.append(mkb(kt))
                return pcs

            def finalize(qt):
                cols = slice(qt * 128, (qt + 1) * 128)
                for hb in range(2):
                    v3 = pso[hb][:, 0:260].rearrange("p (h e) -> p h e", e=65)
                    k.recip(fin[:, hb * 4:(hb + 1) * 4], v3[:, :, 64], [t_pso[hb]], [t_fin])
                    k.tt("dve", yaf[:, hb * 4:(hb + 1) * 4, :], v3[:, :, 0:64],
                         fin[:, hb * 4:(hb + 1) * 4].unsqueeze(2).to_broadcast([128, 4, 64]), ALU.mult,
                         [t_pso[hb], t_fin], [t_yaf])
                yf = yaf[:].rearrange("p h d -> p (h d)")
                k.act(yab[:], yf, AF.Square, [t_yaf], [t_yab, t_fin], accum=fin[:, 8:9])
                k.act(fin[:, 9:10], fin[:, 8:9], AF.Sqrt, [t_fin], [t_fin], scale=1.0 / 512, bias=EPS)
                k.recip(fin[:, 10:11], fin[:, 9:10], [t_fin], [t_fin])
                k.act(yab[:], yf, AF.Copy, [t_yaf, t_fin], [t_yab], scale=fin[:, 10:11])
                ptb = PSH[3][0].bitcast(BF16)
                for c in range(4):
                    k.tr(ptb[:, c * 128:(c + 1) * 128], yab[:, c * 128:(c + 1) * 128], ident[:],
                         [t_yab, t_id], [t_psh[3][0]])
                for c in range(4):
                    k.act(yaT[:, c, :], ptb[:, c * 128:(c + 1) * 128], AF.Copy, [t_psh[3][0], t_vec4], [t_yaT],
                          scale=vec4[:, AOG + c:AOG + c + 1])
                k.dma("pool", YA.rearrange("c p s -> p c s")[:, :, cols], yaT[:], [t_yaT], [])

            def run_pieces(lists):
                tot = max(len(l_) for l_ in lists)
                idx = [0] * len(lists)
                for step in range(tot):
                    for li, l_ in enumerate(lists):
                        tgt = (step + 1) * len(l_) // tot
                        while idx[li] < tgt:
                            l_[idx[li]]()
                            idx[li] += 1

            ld_q(0)
            AP_ = {}

            def get_A(q):
                if q not in AP_:
                    AP_[q] = A_pieces(q)
                return AP_[q]

            run_pieces([get_A(0)[0]])
            lists0 = [get_A(0)[1]]
            if NT > 1:
                lists0.append(get_A(1)[0])
            run_pieces(lists0)
            for qt in range(NT):
                lists = [B_pieces(qt)]
                if qt + 1 < NT:
                    lists.append(get_A(qt + 1)[1])
                if qt + 2 < NT:
                    lists.append(get_A(qt + 2)[0])
                run_pieces(lists)
                finalize(qt)
                AP_.pop(qt, None)
            P.barrier()
            A.release()

        if stages >= 3:
            A.mark()
            maskd = A.alloc([128, NT, NE], F32); t_maskd = T("maskd")
            gated = A.alloc([128, NT, NE], F32); t_gated = T("gated")
            rankd = A.alloc([128, NT, NE], F32); t_rankd = T("rankd")
            basec = A.alloc([128, NE], F32); t_basec = T("basec")
            A.mark()
            g1bc = A.alloc([128, D], F32); t_g1bc = T("g1bc")
            gm2bc = A.alloc([128, D], F32); t_gm2bc = T("gm2bc")
            sh2bc = A.alloc([128, D], F32); t_sh2bc = T("sh2bc")
            bc_row(g1bc, 16, t_g1bc)
            bc_row(gm2bc, 56, t_gm2bc)
            bc_row(sh2bc, 24, t_sh2bc)
            w_out_sb = A.alloc([128, KC, D], BF16); t_wout = T("w_out")
            w_rt = A.alloc([128, KC, NE], F32); t_wrt = T("w_rt")
            brt = A.alloc([128, NE], F32); t_brt = T("brt")
            stg = [A.alloc([128, D], F32) for _ in range(2)]; t_stg = [T("stg0"), T("stg1")]
            for kc in range(KC):
                k.dma("sp", stg[kc % 2][:], w_out_d[kc * 128:(kc + 1) * 128, :], [], [t_stg[kc % 2]])
                k.cp(["dve", "act"][kc % 2], w_out_sb[:, kc, :], stg[kc % 2][:], [t_stg[kc % 2]], [t_wout])
            k.dma("sp", w_rt[:].rearrange("p c e -> p (c e)"), w_rt_d[:, :], [], [t_wrt])
            k.dma("sp", brt[:], b_rt_d[0:1, :].partition_broadcast(128), [], [t_brt])
            k.memset("pool", basec[:], 0.0, [t_basec])
            zt = A.alloc([128, 4, D], BF16); t_zt = T("zt")
            k.memset("pool", zt[:], 0.0, [t_zt])
            for jb in range(NBLK):
                k.dma("pool", HG[jb * BLK:(jb + 1) * BLK, :].rearrange("(a p) d -> p a d", p=128), zt[:],
                      [t_zt], [])
            x_t = [A.alloc([128, D], F32) for _ in range(2)]; t_x2 = [T("x2a"), T("x2b")]
            cat = [A.alloc([128, 8, 128], BF16) for _ in range(2)]; t_cat = [T("cat0"), T("cat1")]
            x1 = [A.alloc([128, D], F32) for _ in range(2)]; t_x1 = [T("x1a"), T("x1b")]
            h2 = [A.alloc([128, D], F32) for _ in range(2)]; t_h2 = [T("h2a"), T("h2b")]
            h2b = [A.alloc([128, D], BF16) for _ in range(2)]; t_h2b = [T("h2ba"), T("h2bb")]
            h2T = [A.alloc([128, KC, 128], F32) for _ in range(2)]; t_h2T = [T("h2Ta"), T("h2Tb")]
            rt = [A.alloc([128, 64], F32) for _ in range(2)]; t_rt = [T("rta"), T("rtb")]
            lg = [A.alloc([128, NE], F32) for _ in range(2)]; t_lg = [T("lga"), T("lgb")]
            ex = [A.alloc([128, NE], F32) for _ in range(2)]; t_ex = [T("exa"), T("exb")]
            mb = [A.alloc([128, NE], BF16) for _ in range(2)]; t_mb = [T("mba"), T("mbb")]
            jk2 = [A.alloc([128, D], BF16) for _ in range(2)]; t_jk2 = [T("jk2a"), T("jk2b")]
            ps_mix = [PSH[0][0], PSH[0][1]]; t_psmix = [t_psh[0][0], t_psh[0][1]]
            ps_trs = [psT[1], psT[3]]; t_pstrs = [t_psh[1][0], t_psh[3][0]]
            ps_rs = [PSH[2][0], PSH[2][1]]; t_psrs = [t_psh[2][0], t_psh[2][1]]

            def tile_pieces(i):
                b = i % 2
                cols = slice(i * 128, (i + 1) * 128)
                ps_tr = ps_trs[b]; t_pstr = t_pstrs[b]
                ps_r = ps_rs[b]; t_psr = t_psrs[b]
                pcs = []

                def p0():
                    k.dma("sp", x_t[b][:], x_d[cols, :], [], [t_x2[b]])
                    k.dma("sp", cat[b][:, 0:4, :], YR.rearrange("c p s -> p c s")[:, :, cols], [t_dram["YR"]], [t_cat[b]])
                    k.dma("sp", cat[b][:, 4:8, :], YA.rearrange("c p s -> p c s")[:, :, cols], [t_dram["YA"]], [t_cat[b]])
                pcs.append(p0)

                def p1():
                    for half in range(2):
                        for c in range(8):
                            k.mm(ps_mix[half][:, :], cat[b][:, c, :], w_out_sb[:, c, half * 512:(half + 1) * 512],
                                 c == 0, c == 7, [t_cat[b], t_wout], [t_psmix[half]])
                    for half in range(2):
                        hs_ = slice(half * 512, (half + 1) * 512)
                        k.tt("dve", x1[b][:, hs_], ps_mix[half][:, :], g1bc[:, hs_], ALU.mult,
                             [t_psmix[half], t_g1bc], [t_x1[b]])
                pcs.append(p1)

                def p2():
                    k.tt("pool", x1[b][:], x1[b][:], x_t[b][:], ALU.add, [t_x1[b], t_x2[b]], [t_x1[b]])
                    k.dma("pool", X1[cols, :], x1[b][:], [t_x1[b]], [])
                    k.act(jk2[b][:], x1[b][:], AF.Square, [t_x1[b]], [t_jk2[b], t_rt[b]], accum=rt[b][:, 0:1])
                pcs.append(p2)
                pcs.append(lambda: k.act(rt[b][:, 1:2], rt[b][:, 0:1], AF.Sqrt, [t_rt[b]], [t_rt[b]], scale=1.0 / D, bias=EPS))
                pcs.append(lambda: k.recip(rt[b][:, 2:3], rt[b][:, 1:2], [t_rt[b]], [t_rt[b]]))
                pcs.append(lambda: k.stt(h2[b][:], x1[b][:], rt[b][:, 2:3], gm2bc[:], ALU.mult, ALU.mult,
                                         [t_x1[b], t_rt[b], t_gm2bc], [t_h2[b]]))
                pcs.append(lambda: k.tt("pool", h2[b][:], h2[b][:], sh2bc[:], ALU.add, [t_h2[b], t_sh2bc], [t_h2[b]]))

                def p3():
                    k.cp("act", h2b[b][:], h2[b][:], [t_h2[b]], [t_h2b[b]])
                    k.dma("pool", H2[cols, :], h2b[b][:], [t_h2b[b]], [])
                    for kc in range(KC):
                        k.tr(ps_tr[:, kc * 128:(kc + 1) * 128], h2[b][:, kc * 128:(kc + 1) * 128], identf[:],
                             [t_h2[b], t_idf], [t_pstr])
                pcs.append(p3)
                pcs.append(lambda: k.cp("act", h2T[b][:].rearrange("p c t -> p (c t)"), ps_tr[:, :], [t_pstr], [t_h2T[b]]))

                def p4():
                    for kc in range(KC):
                        k.mm(ps_r[:, 0:NE], h2T[b][:, kc, :], w_rt[:, kc, :], kc == 0, kc == KC - 1,
                             [t_h2T[b], t_wrt], [t_psr])
                pcs.append(p4)
                pcs.append(lambda: k.tt("dve", lg[b][:], ps_r[:, 0:NE], brt[:], ALU.add, [t_psr, t_brt], [t_lg[b]]))
                pcs.append(lambda: P.add("dve", (lambda o_, i_: (lambda e: e.max(out=o_, in_=i_)))(rt[b][:, 8:16], lg[b][:]),
                                         [t_lg[b]], [t_rt[b]]))
                pcs.append(lambda: k.ts("dve", maskd[:, i, :], lg[b][:], rt[b][:, 11:12], None, ALU.is_ge, None,
                                        [t_lg[b], t_rt[b]], [t_maskd]))

                def p5():
                    k.cp("dve", mb[b][:], maskd[:, i, :], [t_maskd], [t_mb[b]])
                    k.ts("dve", rt[b][:, 3:4], rt[b][:, 8:9], -1.0, None, ALU.mult, None, [t_rt[b]], [t_rt[b]])
                pcs.append(p5)

                def p6():
                    k.act(ex[b][:], lg[b][:], AF.Exp, [t_lg[b], t_rt[b]], [t_ex[b]], bias=rt[b][:, 3:4])
                    k.mm(ps_r[:, 32:64], ustr[:], mb[b][:], True, True, [t_ustr, t_mb[b]], [t_psr])
                    k.mm(ps_r[:, 64:96], ones_b[:, 0:128], mb[b][:], True, True, [t_ones, t_mb[b]], [t_psr])
                pcs.append(p6)
                pcs.append(lambda: k.stt(ex[b][:], ex[b][:], 1.0, maskd[:, i, :], ALU.mult, ALU.mult,
                                         [t_ex[b], t_maskd], [t_ex[b], t_rt[b]], accum=rt[b][:, 4:5]))
                pcs.append(lambda: k.recip(rt[b][:, 5:6], rt[b][:, 4:5], [t_rt[b]], [t_rt[b]]))
                pcs.append(lambda: k.ts("dve", gated[:, i, :], ex[b][:], rt[b][:, 5:6], None, ALU.mult, None,
                                        [t_ex[b], t_rt[b]], [t_gated]))

                def p7():
                    k.tt("dve", rankd[:, i, :], ps_r[:, 32:64], basec[:], ALU.add, [t_psr, t_basec], [t_rankd])
                    k.tt("dve", basec[:], ps_r[:, 64:96], basec[:], ALU.add, [t_psr, t_basec], [t_basec])
                pcs.append(p7)
                return pcs

            for i0_ in range(0, NT, 2):
                run_pieces([tile_pieces(i0_), tile_pieces(i0_ + 1)])
            P.barrier()
            A.release()

        if stages >= 4:
            A.mark()
            jrow = A.alloc([128, JMAX], F32); t_jrow = T("jrow")
            cmp3 = A.alloc([128, NE, JMAX], F32); t_cmp3 = T("cmp3")
            nb = A.alloc([128, NE], F32); t_nb = T("nb")
            incl = A.alloc([128, NE], F32); t_incl = T("incl")
            pst = A.alloc([128, NE], F32); t_pst = T("pst")
            onesf = A.alloc([128, NE], F32); t_onesf = T("onesf")
            jb_ = A.alloc([128, NBLK], F32); t_jb = T("jb")
            cmpb = A.alloc([128, NBLK, NE], F32); t_cmpb = T("cmpb")
            be = A.alloc([128, NBLK], F32); t_be = T("be")
            widxf = A.alloc([128, NBLK, 8], F32); t_widxf = T("widxf")
            pbig = A.alloc([128, 1], F32); t_pbig = T("pbig")
            bef = A.alloc([128, NBLK], F32); t_bef = T("bef")
            key3 = A.alloc([128, NT, NE], F32); t_key3 = T("key3")
            top8 = A.alloc([128, 8], F32); t_top8 = T("top8")
            d4f = A.alloc([128, NT, 4], F32); t_d4f = T("d4f")
            jk32 = A.alloc([128, NE], F32); t_jk32 = T("jk32")
            h2l = [A.alloc([128, D], BF16) for _ in range(3)]; t_h2l = [T(f"h2l{i}") for i in range(3)]
            k.iota(jrow[:], [[BLK, JMAX]], 0, 0, [t_jrow])
            k.iota(jb_[:], [[1, NBLK]], 0, 0, [t_jb])
            k.memset("pool", onesf[:], 1.0, [t_onesf])
            k.tt("dve", cmp3[:], jrow[:].unsqueeze(1).to_broadcast([128, NE, JMAX]),
                 basec[:].unsqueeze(2).to_broadcast([128, NE, JMAX]), ALU.is_lt, [t_jrow, t_basec], [t_cmp3])
            P.add("dve", lambda e: e.tensor_reduce(out=nb[:], in_=cmp3[:], axis=AX.X, op=ALU.add), [t_cmp3], [t_nb])
            P.add("dve", lambda e: e.tensor_tensor_scan(out=incl[:], data0=onesf[:], data1=nb[:], initial=0.0,
                                                        op0=ALU.mult, op1=ALU.add), [t_onesf, t_nb], [t_incl])
            k.tt("dve", pst[:], incl[:], nb[:], ALU.subtract, [t_incl, t_nb], [t_pst])
            k.ts("dve", pst[:], pst[:], float(BLK), None, ALU.mult, None, [t_pst], [t_pst])
            k.tt("dve", cmpb[:], incl[:].unsqueeze(1).to_broadcast([128, NBLK, NE]),
                 jb_[:].unsqueeze(2).to_broadcast([128, NBLK, NE]), ALU.is_le, [t_incl, t_jb], [t_cmpb])
            P.add("dve", lambda e: e.tensor_reduce(out=be[:], in_=cmpb[:], axis=AX.X, op=ALU.add), [t_cmpb], [t_be])
            k.ts("dve", be[:], be[:], float(NE - 1), None, ALU.min, None, [t_be], [t_be])
            k.cp("dve", bidx[:], be[:], [t_be], [t_bidx])
            k.ts("dve", be[:], be[:], 1024.0, pidx[:, 0:1], ALU.mult, ALU.add, [t_be, t_pidx], [t_be])
            for kc in range(KC):
                k.ts("dve", widxf[:, :, kc], be[:], float(kc * 128), None, ALU.add, None, [t_be], [t_widxf])
            k.cp("dve", widx[:].rearrange("p b c -> p (b c)"), widxf[:].rearrange("p b c -> p (b c)"),
                 [t_widxf], [t_widx])
            k.tt("dve", key3[:], rankd[:], pst[:].unsqueeze(1).to_broadcast([128, NT, NE]), ALU.add,
                 [t_rankd, t_pst], [t_key3])
            k.ts("dve", key3[:], key3[:], -1.0, BIGC, ALU.mult, ALU.add, [t_key3], [t_key3])
            k.tt("dve", key3[:], key3[:], maskd[:], ALU.mult, [t_key3, t_maskd], [t_key3])
            for i in range(NT):
                P.add("dve", (lambda o_, i_: (lambda e: e.max(out=o_, in_=i_)))(top8[:], key3[:, i, :]),
                      [t_key3], [t_top8])
                k.ts("dve", d4f[:, i, :], top8[:, 0:4], -1.0, BIGC, ALU.mult, ALU.add, [t_top8], [t_d4f])
                for k4 in range(4):
                    k.stt(jk32[:], key3[:, i, :], top8[:, k4:k4 + 1], gated[:, i, :], ALU.is_equal, ALU.mult,
                          [t_key3, t_top8, t_gated], [t_jk32, t_gate4], accum=gate4[:, i, k4:k4 + 1])
            k.cp("dve", dest4[:].rearrange("p t f -> p (t f)"), d4f[:].rearrange("p t f -> p (t f)"),
                 [t_d4f], [t_dest4])
            if debug:
                k.dma("sp", RTD[:, 0:NT * 4], d4f[:].rearrange("p t f -> p (t f)"), [t_d4f], [])
                k.dma("sp", RTD[:, NT * 4:NT * 8], gate4[:].rearrange("p t f -> p (t f)"), [t_gate4], [])
            for i in range(NT):
                hb_ = h2l[i % 3]; thb = t_h2l[i % 3]
                k.dma("sp", hb_[:], H2[i * 128:(i + 1) * 128, :], [t_dram["H2"]], [thb])
                for k4 in range(4):
                    k.scatter(HG[:, :], hb_[:], dest4[:, i, k4:k4 + 1], [thb, t_dest4], [])
            P.barrier()
            A.release()
            A.release()

            A.mark()
            w1sb = [A.alloc([128, 9 * 2048], BF16) for _ in range(2)]; t_w1sb = [T("w1sb0"), T("w1sb1")]
            w2sb = [A.alloc([128, 9 * 1024], BF16) for _ in range(2)]; t_w2sb = [T("w2sb0"), T("w2sb1")]
            hg = [A.alloc([128, 4, D], BF16) for _ in range(2)]; t_hg = [T("hg0"), T("hg1")]
            hgT = A.alloc([128, KC, 512], BF16); t_hgT = [T(f"hgT{c}") for c in range(KC)]
            actT = A.alloc([128, KC, 512], BF16); t_actT = [T(f"actT{c}") for c in range(KC)]
            NE4 = 6
            ew = [A.alloc([128, 512], F32) for _ in range(NE4)]; t_ew = [T(f"ew{i}") for i in range(NE4)]
            ysb = [A.alloc([128, D], F32) for _ in range(2)]; t_ysb = [T("ysb0"), T("ysb1")]
            ewi = [0]

            def new_ew():
                i = ewi[0] % NE4; ewi[0] += 1
                return ew[i], t_ew[i]

            ps_t4 = [PSH[0][0].bitcast(BF16), PSH[0][1].bitcast(BF16)]; t_pst4 = [t_psh[0][0], t_psh[0][1]]
            ps_gl = [(PSH[1][0], t_psh[1][0], PSH[1][1], t_psh[1][1]), (PSH[2][0], t_psh[2][0], PSH[2][1], t_psh[2][1])]
            ps_y = [PSH[3][0], PSH[3][1]]; t_psy = [t_psh[3][0], t_psh[3][1]]
            ntr = 0; ngl = 0; ny = 0; nwf = 0
            NWS = 4
            wst = [A.alloc([128, 2048], F32) for _ in range(NWS)]; t_wst = [T(f"wst{i}") for i in range(NWS)]
            w1rows = w1_d.rearrange("e k n -> (e k) n")
            w2rows = w2_d.rearrange("e k n -> (e k) n")
            nwf_ = [0]

            def wsteps(jb):
                b = jb % 2
                for kc in range(KC):
                    s_ = nwf_[0] % NWS; nwf_[0] += 1
                    k.gather(wst[s_][:, :], w1rows[:, :], widx[:, jb, kc:kc + 1], [t_widx], [t_wst[s_]])
                    k.cp("act",
                         w1sb[b][:, kc * 2048:(kc + 1) * 2048].rearrange("p (two f) -> p two f", two=2),
                         wst[s_][:, :].rearrange("p (f two) -> p two f", two=2), [t_wst[s_]], [t_w1sb[b]])
                    yield
                s_ = nwf_[0] % NWS; nwf_[0] += 1
                k.gather(wst[s_][:, :], b1_d[:, :], bidx[:, jb:jb + 1], [t_bidx], [t_wst[s_]])
                k.cp("dve", w1sb[b][0:1, 8 * 2048:9 * 2048].rearrange("p (two f) -> p two f", two=2),
                     wst[s_][0:1, :].rearrange("p (f two) -> p two f", two=2), [t_wst[s_]], [t_w1sb[b]])
                yield
                for kc in range(KC):
                    s_ = nwf_[0] % NWS; nwf_[0] += 1
                    k.gather(wst[s_][:, 0:1024], w2rows[:, :], widx[:, jb, kc:kc + 1], [t_widx], [t_wst[s_]])
                    k.cp("dve", w2sb[b][:, kc * 1024:(kc + 1) * 1024], wst[s_][:, 0:1024], [t_wst[s_]], [t_w2sb[b]])
                    yield
                s_ = nwf_[0] % NWS; nwf_[0] += 1
                k.gather(wst[s_][:, 0:1024], b2_d[:, :], bidx[:, jb:jb + 1], [t_bidx], [t_wst[s_]])
                k.ts("dve", w2sb[b][0:1, 8 * 1024:9 * 1024], wst[s_][0:1, 0:1024], 1.702, None, ALU.mult, None,
                     [t_wst[s_]], [t_w2sb[b]])
                yield

            def adv(gen, n_=1):
                if gen is None:
                    return
                for _ in range(n_):
                    try:
                        next(gen)
                    except StopIteration:
                        return

            g0 = wsteps(0)
            adv(g0, 100)
            for jb in range(NBLK):
                b = jb % 2
                wgen = wsteps(jb + 1) if jb + 1 < NBLK else None
                k.dma("sp", hg[b][:], HG[jb * BLK:(jb + 1) * BLK, :].rearrange("(a p) d -> p a d", p=128),
                      [t_dram["HG"]], [t_hg[b]])
                for kc in range(KC):
                    pt_ = ps_t4[ntr % 2]; tpt_ = t_pst4[ntr % 2]; ntr += 1
                    for a_ in range(4):
                        k.tr(pt_[:, a_ * 128:(a_ + 1) * 128], hg[b][:, a_, kc * 128:(kc + 1) * 128], ident[:],
                             [t_hg[b], t_id], [tpt_])
                    k.cp("act" if kc % 2 == 0 else "dve", hgT[:, kc, :], pt_[:, 0:512], [tpt_], [t_hgT[kc]])
                    adv(wgen)
                for fc in range(KC):
                    pg, tpg, pl, tpl = ps_gl[ngl % 2]; ngl += 1
                    for (pz_, tz_, off) in ((pg, tpg, 0), (pl, tpl, 1024)):
                        for kc in range(KC):
                            k.mm(pz_[:, :], w1sb[b][:, kc * 2048 + off + fc * 128:kc * 2048 + off + (fc + 1) * 128],
                                 hgT[:, kc, :], kc == 0, False, [t_w1sb[b], t_hgT[kc]], [tz_])
                        k.mm(pz_[:, :], w1sb[b][0:1, 8 * 2048 + off + fc * 128:8 * 2048 + off + (fc + 1) * 128],
                             ones_b[0:1, 0:512], False, True, [t_w1sb[b], t_ones], [tz_])
                    g_, tg_ = new_ew()
                    k.ts("dve", g_[:], pg[:, :], 7.0, None, ALU.min, None, [tpg], [tg_])
                    sl, tsl = new_ew()
                    k.act(sl[:], g_[:], AF.Silu, [tg_], [tsl], scale=1.702)
                    l_, tl_ = new_ew()
                    k.ts("dve", l_[:], pl[:, :], -7.0, 7.0, ALU.max, ALU.min, [tpl], [tl_])
                    k.stt(actT[:, fc, :], l_[:], 1.0, sl[:], ALU.add, ALU.mult, [tl_, tsl], [t_actT[fc]])
                    adv(wgen)
                for a_ in range(4):
                    yb = ysb[ny % 2]; tyb = t_ysb[ny % 2]; ny += 1
                    for dh in range(2):
                        py = ps_y[dh]; tpy = t_psy[dh]
                        for fc in range(KC):
                            k.mm(py[:, :], actT[:, fc, a_ * 128:(a_ + 1) * 128],
                                 w2sb[b][:, fc * 1024 + dh * 512:fc * 1024 + (dh + 1) * 512], fc == 0, False,
                                 [t_actT[fc], t_w2sb[b]], [tpy])
                        k.mm(py[:, :], ones_b[0:1, 0:128], w2sb[b][0:1, 8 * 1024 + dh * 512:8 * 1024 + (dh + 1) * 512],
                             False, True, [t_ones, t_w2sb[b]], [tpy])
                        k.act(yb[:, dh * 512:(dh + 1) * 512], py[:, :], AF.Copy, [tpy], [tyb], scale=1.0 / 1.702)
                    r0 = jb * BLK + a_ * 128
                    k.dma("sp", YY[r0:r0 + 128, :], yb[:], [tyb], [])
                    adv(wgen)
                adv(wgen, 100)
            P.barrier()
            A.release()

            A.mark()
            g2bc = A.alloc([128, D], F32); t_g2bc = T("g2bc")
            bc_row(g2bc, 40, t_g2bc)
            x1l = [A.alloc([128, D], F32) for _ in range(2)]; t_x1l = [T("x1l0"), T("x1l1")]
            yg = [[A.alloc([128, D], F32) for _ in range(4)] for _ in range(2)]
            t_yg = [[T(f"yg{b}{q}") for q in range(4)] for b in range(2)]
            acc = [A.alloc([128, D], F32) for _ in range(2)]; t_acc = [T("acc0"), T("acc1")]
            for i in range(NT):
                b = i % 2
                cols = slice(i * 128, (i + 1) * 128)
                k.dma("sp", x1l[b][:], X1[cols, :], [t_dram["X1"]], [t_x1l[b]])
                for k4 in range(4):
                    k.gather(yg[b][k4][:, :], YY[:, :], dest4[:, i, k4:k4 + 1], [t_dest4, t_dram["YY"]], [t_yg[b][k4]])
                k.ts("dve", acc[b][:], yg[b][0][:], gate4[:, i, 0:1], None, ALU.mult, None,
                     [t_yg[b][0], t_gate4], [t_acc[b]])
                for k4 in range(1, 4):
                    k.stt(acc[b][:], yg[b][k4][:], gate4[:, i, k4:k4 + 1], acc[b][:], ALU.mult, ALU.add,
                          [t_yg[b][k4], t_gate4, t_acc[b]], [t_acc[b]])
                k.tt("pool", acc[b][:], acc[b][:], g2bc[:], ALU.mult, [t_acc[b], t_g2bc], [t_acc[b]])
                k.tt("dve", acc[b][:], acc[b][:], x1l[b][:], ALU.add, [t_acc[b], t_x1l[b]], [t_acc[b]])
                k.dma("sp", out_d[cols, :], acc[b][:], [t_acc[b]], [])
            A.release()
        elif stages >= 1:
            A.mark()
            zt2 = A.alloc([128, D], F32); t_zt2 = T("zt2")
            k.memset("pool", zt2[:], 0.0, [t_zt2])
            k.dma("sp", out_d[0:128, :], zt2[:], [t_zt2], [])
            A.release()
        P.emit()
    return nc


def _fm(v, nchunk):
    return np.ascontiguousarray(np.asarray(v, np.float32).reshape(nchunk, 128).T)


def prep_shared(inp, small=False):
    L = 0
    f32 = np.float32
    sh = {}
    sh["w_ada"] = np.ascontiguousarray(inp["w_ada"][L], f32)
    sh["b_ada_fm"] = _fm(inp["b_ada"][L], 48)
    sh["n1g_fm"] = _fm(inp["norm1_g"][L], 8)
    sh["n2g_fm"] = _fm(inp["norm2_g"][L], 8)
    sh["w_in"] = np.ascontiguousarray(inp["w_in"][L], f32)
    cw = np.asarray(inp["conv_w"][L], f32)
    sh["convw_fm"] = np.ascontiguousarray(cw.T.reshape(4, 128, 4).transpose(1, 0, 2).reshape(128, 16))
    v4 = np.zeros((128, 28), f32)
    for j, name in enumerate(["conv_b", "b_rg_a", "b_rg_x", "lru_lambda", "rg_out_g", "attn_out_g"]):
        v4[:, j * 4:(j + 1) * 4] = _fm(inp[name][L], 4)
    v4[:, 24] = np.tile(np.asarray(inp["q_norm_g"][L], f32), 2)
    v4[:, 25] = np.tile(np.asarray(inp["k_norm_g"][L], f32), 2)
    v4[:, 26] = np.tile(np.asarray(inp["kidx_norm_g"][L], f32), 2)
    sh["vec4_fm"] = v4
    for nm, key in (("wabd", "w_rg_a"), ("wxbd", "w_rg_x")):
        w = np.asarray(inp[key][L], f32)
        bd = np.zeros((128, 4, 128), f32)
        for c in range(4):
            bd[0:64, c, 0:64] = w[2 * c]
            bd[64:128, c, 64:128] = w[2 * c + 1]
        sh[nm] = bd.reshape(128, 512)
    sh["w_out"] = np.ascontiguousarray(inp["w_out"][L], f32)
    wr = np.asarray(inp["w_router"][L], f32)
    sh["w_rt_fm"] = np.ascontiguousarray(wr.reshape(8, 128, 32).transpose(1, 0, 2).reshape(128, 256))
    sh["b_rt"] = np.asarray(inp["b_router"][L], f32).reshape(1, 32)
    if not small:
        sh["w1"] = np.ascontiguousarray(inp["w1"][L], f32)
        sh["b1"] = np.ascontiguousarray(inp["b1"][L], f32)
        sh["w2"] = np.ascontiguousarray(inp["w2"][L], f32)
        sh["b2"] = np.ascontiguousarray(inp["b2"][L], f32)
    return sh


def kernel(**inputs):
    x = np.asarray(inputs["x"], np.float32)
    c = np.asarray(inputs["c"], np.float32)
    B, S, _ = x.shape
    sh = prep_shared(inputs)
    nc = build_nc(S)
    in_maps = []
    for b in range(B):
        m = dict(sh)
        m["x"] = np.ascontiguousarray(x[b])
        m["c_fm"] = _fm(c[b], 8)
        in_maps.append(m)
    res = run_bass_kernel_spmd(nc, in_maps, core_ids=list(range(B)))
    return np.stack([np.asarray(r["out"], np.float32) for r in res.results], axis=0)
```

```python
import numpy as np
import concourse.bass as bass
import concourse.mybir as mybir
from concourse.bass_utils import run_bass_kernel_spmd

F32 = mybir.dt.float32
BF16 = mybir.dt.bfloat16
I32 = mybir.dt.int32
U32 = mybir.dt.uint32
U8 = mybir.dt.uint8
ALU = mybir.AluOpType
AF = mybir.ActivationFunctionType
AX = mybir.AxisListType
DSZ = {F32: 4, BF16: 2, I32: 4, U32: 4, U8: 1}


class Tok:
    __slots__ = ("name", "lw", "rd", "excl")

    def __init__(self, name, excl=False):
        self.name = name
        self.lw = None
        self.rd = []
        self.excl = excl


class _Op:
    __slots__ = ("eng", "fn", "deps", "idx", "dma", "sem", "val", "used", "slot_prev")

    def __init__(self, eng, fn, deps, idx, dma):
        self.eng = eng
        self.fn = fn
        self.deps = deps
        self.idx = idx
        self.dma = dma
        self.sem = None
        self.val = 0
        self.used = False
        self.slot_prev = None


class Prog:
    ENGS = ("pe", "act", "dve", "pool", "sp")
    NSLOT = 12
    SEG = 20000

    def __init__(self, nc):
        self.nc = nc
        self.ops = []
        self.by_eng = {e: [] for e in self.ENGS}
        self.pending_barrier = {e: [] for e in self.ENGS}
        self.recent_dma = {e: [] for e in self.ENGS}

    def add(self, eng, fn, rd=(), wr=(), dma=False):
        ex = [t for t in rd if t.excl]
        if ex:
            rd = [t for t in rd if not t.excl]
            wr = list(wr) + ex
        deps = set()
        for t in rd:
            if t.lw is not None:
                deps.add(t.lw)
        for t in wr:
            if t.lw is not None:
                deps.add(t.lw)
            deps.update(t.rd)
        if self.pending_barrier[eng]:
            deps.update(self.pending_barrier[eng])
            self.pending_barrier[eng] = []
        idx = len(self.ops)
        op = _Op(eng, fn, sorted(deps), idx, dma)
        self.ops.append(op)
        self.by_eng[eng].append(op)
        for t in rd:
            t.rd.append(idx)
        for t in wr:
            t.lw = idx
            t.rd = []
        if dma:
            r = self.recent_dma[eng]
            r.append(idx)
            if len(r) > self.NSLOT:
                r.pop(0)
        return idx

    def barrier(self):
        last = []
        for e in self.ENGS:
            ops = self.by_eng[e]
            for o in reversed(ops):
                if not o.dma:
                    last.append(o.idx)
                    break
            last.extend(self.recent_dma[e])
        for e in self.ENGS:
            self.pending_barrier[e] = list(set(self.pending_barrier[e]) | set(last))

    def emit(self, final_wait_eng="sp"):
        nc = self.nc
        ops = self.ops
        self.barrier()
        fin = self.pending_barrier[final_wait_eng]
        for o in ops:
            for d in o.deps:
                ops[d].used = True
        for d in fin:
            ops[d].used = True
        nsem = 0
        plan = {}
        for e in self.ENGS:
            ncomp = sum(1 for o in self.by_eng[e] if (not o.dma) and o.used)
            nseg = (ncomp + self.SEG - 1) // self.SEG
            ndma = self.NSLOT if any(o.dma for o in self.by_eng[e]) else 0
            plan[e] = (nseg, ndma)
            nsem += nseg + ndma
        import contextlib

        with contextlib.ExitStack() as st:
            sems = [st.enter_context(nc.semaphore(f"s{i}")) for i in range(nsem)]
            si = 0
            for e in self.ENGS:
                nseg, ndma = plan[e]
                seg = sems[si:si + nseg]
                si += nseg
                slots = sems[si:si + ndma]
                si += ndma
                slot_cnt = [0] * ndma
                k = 0
                c = 0
                for o in self.by_eng[e]:
                    if o.dma:
                        s = k % ndma
                        k += 1
                        o.slot_prev = (slots[s], slot_cnt[s]) if slot_cnt[s] else None
                        slot_cnt[s] += 16
                        o.sem, o.val = slots[s], slot_cnt[s]
                    elif o.used:
                        o.sem = seg[c // self.SEG]
                        o.val = (c % self.SEG) + 1
                        c += 1
            block = st.enter_context(nc.Block())

            def run(eng_name):
                def body(e):
                    waited = {}

                    def wait(sem, val):
                        key = id(sem)
                        if waited.get(key, 0) >= val:
                            return
                        waited[key] = val
                        e.wait_ge(sem, val)

                    for o in self.by_eng[eng_name]:
                        for d in o.deps:
                            p = ops[d]
                            if p.eng == "pe" and eng_name == "pe" and not p.dma:
                                continue
                            wait(p.sem, p.val)
                        if o.slot_prev is not None:
                            wait(*o.slot_prev)
                        ins = o.fn(e)
                        if o.dma:
                            ins.then_inc(o.sem, 16)
                        elif o.used:
                            ins.then_inc(o.sem, 1)
                    if eng_name == final_wait_eng:
                        for d in fin:
                            wait(ops[d].sem, ops[d].val)
                return body

            block.tensor(run("pe"))
            block.scalar(run("act"))
            block.vector(run("dve"))
            block.gpsimd(run("pool"))
            block.sync(run("sp"))


class Arena:
    def __init__(self, nc, st, nbytes, name="arena"):
        self.t = st.enter_context(nc.sbuf_tensor(name, [128, nbytes], U8))
        self.nbytes = nbytes
        self.off = 0
        self.marks = []

    def alloc(self, shape, dtype, name=None):
        assert shape[0] <= 128
        free = int(np.prod(shape[1:]))
        nb = free * DSZ[dtype]
        nb_al = (nb + 63) // 64 * 64
        assert self.off + nb_al <= self.nbytes, (self.off, nb_al, self.nbytes, name)
        ap = self.t[0:shape[0], self.off:self.off + nb]
        self.off += nb_al
        if dtype != U8:
            ap = ap.bitcast(dtype)
        if len(shape) > 2:
            names = " ".join(f"d{i}" for i in range(1, len(shape)))
            kw = {f"d{i}": shape[i] for i in range(1, len(shape))}
            ap = ap.rearrange(f"p ({names}) -> p {names}", **kw)
        return ap

    def mark(self):
        self.marks.append(self.off)

    def release(self):
        self.off = self.marks.pop()

import contextlib

D = 1024
KC = 8
NE = 32
EPS = 1e-6
BLK = 512
NIT = 14
BIGC = float(2 ** 20)


class K:
    def __init__(self, P):
        self.P = P

    def mm(self, out, lhsT, rhs, start, stop, rd, wr):
        self.P.add("pe", lambda e: e.matmul(out, lhsT=lhsT, rhs=rhs, start=start, stop=stop,
                                            skip_group_check=True), rd, wr)

    def tr(self, out, in_, ident, rd, wr):
        self.P.add("pe", lambda e: e.transpose(out=out, in_=in_, identity=ident), rd, wr)

    def act(self, out, in_, func, rd, wr, scale=1.0, bias=0.0, accum=None, eng="act"):
        if accum is None:
            self.P.add(eng, lambda e: e.activation(out=out, in_=in_, func=func, scale=scale, bias=bias), rd, wr)
        else:
            self.P.add(eng, lambda e: e.activation(out=out, in_=in_, func=func, scale=scale, bias=bias,
                                                   accum_out=accum), rd, wr)

    def ts(self, eng, out, in0, s1, s2, op0, op1, rd, wr, accum=None):
        if op1 is None:
            self.P.add(eng, lambda e: e.tensor_scalar(out=out, in0=in0, scalar1=s1, scalar2=None, op0=op0), rd, wr)
        elif accum is None:
            self.P.add(eng, lambda e: e.tensor_scalar(out=out, in0=in0, scalar1=s1, scalar2=s2, op0=op0, op1=op1), rd, wr)
        else:
            self.P.add(eng, lambda e: e.tensor_scalar(out=out, in0=in0, scalar1=s1, scalar2=s2, op0=op0, op1=op1,
                                                      accum_out=accum), rd, wr)

    def tt(self, eng, out, in0, in1, op, rd, wr):
        self.P.add(eng, lambda e: e.tensor_tensor(out=out, in0=in0, in1=in1, op=op), rd, wr)

    def stt(self, out, in0, scalar, in1, op0, op1, rd, wr, accum=None):
        if accum is None:
            self.P.add("dve", lambda e: e.scalar_tensor_tensor(out=out, in0=in0, scalar=scalar, in1=in1,
                                                              op0=op0, op1=op1), rd, wr)
        else:
            self.P.add("dve", lambda e: e.scalar_tensor_tensor(out=out, in0=in0, scalar=scalar, in1=in1,
                                                              op0=op0, op1=op1, accum_out=accum), rd, wr)

    def cp(self, eng, out, in_, rd, wr):
        if eng == "act":
            self.P.add("act", lambda e: e.activation(out=out, in_=in_, func=AF.Copy), rd, wr)
        else:
            self.P.add(eng, lambda e: e.tensor_copy(out=out, in_=in_), rd, wr)

    def memset(self, eng, ap, val, wr):
        self.P.add(eng, lambda e: e.memset(ap, val), (), wr)

    def dma(self, q, out, in_, rd, wr):
        self.P.add(q, lambda e: e.dma_start(out=out, in_=in_), rd, wr, dma=True)

    def gather(self, out, in_, idx, rd, wr, bounds=None):
        if bounds is None:
            self.P.add("pool", lambda e: e.indirect_dma_start(
                out=out, out_offset=None, in_=in_,
                in_offset=bass.IndirectOffsetOnAxis(ap=idx, axis=0)), rd, wr, dma=True)
        else:
            self.P.add("pool", lambda e: e.indirect_dma_start(
                out=out, out_offset=None, in_=in_,
                in_offset=bass.IndirectOffsetOnAxis(ap=idx, axis=0),
                bounds_check=bounds, oob_is_err=False), rd, wr, dma=True)

    def scatter(self, out, in_, idx, rd, wr):
        self.P.add("pool", lambda e: e.indirect_dma_start(
            out=out, out_offset=bass.IndirectOffsetOnAxis(ap=idx, axis=0),
            in_=in_, in_offset=None), rd, wr, dma=True)

    def recip(self, out, in_, rd, wr):
        self.P.add("dve", lambda e: e.reciprocal(out=out, in_=in_), rd, wr)

    def iota(self, out, pattern, base, cm, wr):
        self.P.add("pool", lambda e: e.iota(out, pattern=pattern, base=base, channel_multiplier=cm,
                                            allow_small_or_imprecise_dtypes=True), (), wr)


def build_nc(S, stages=5, debug=False):
    NT = S // 128
    NG = S // 512
    NSEL = min(256, S // 4)
    NSLOT = 4 * S + NE * BLK
    NBLK = NSLOT // BLK
    JMAX = S // BLK

    nc = bass.Bass("TRN2", target_bir_lowering=False)

    def din(name, shape, dt=F32):
        return nc.dram_tensor(name, list(shape), dt, kind="ExternalInput").ap()

    def dscr(name, shape, dt):
        kind = "ExternalOutput" if debug else "Internal"
        return nc.dram_tensor(name, list(shape), dt, kind=kind).ap()

    x_d = din("x", [S, D])
    c_d = din("c_fm", [128, KC])
    w_ada_d = din("w_ada", [D, 6 * D])
    b_ada_d = din("b_ada_fm", [128, 48])
    n1g_d = din("n1g_fm", [128, KC])
    n2g_d = din("n2g_fm", [128, KC])
    w_in_d = din("w_in", [D, 3144])
    convw_d = din("convw_fm", [128, 16])
    vec4_d = din("vec4_fm", [128, 28])
    wabd_d = din("wabd", [128, 4 * 128])
    wxbd_d = din("wxbd", [128, 4 * 128])
    w_out_d = din("w_out", [D, D])
    w_rt_d = din("w_rt_fm", [128, KC * NE])
    b_rt_d = din("b_rt", [1, NE])
    if stages >= 4:
        w1_d = din("w1", [NE, D, 2 * D])
        b1_d = din("b1", [NE, 2 * D])
        w2_d = din("w2", [NE, D, D])
        b2_d = din("b2", [NE, D])
    import os as _os
    CUT = int(_os.environ.get("P1CUT", "99"))
    out_d = nc.dram_tensor("out", [S, D], F32, kind="ExternalOutput").ap()

    MODROW = dscr("modrow", [64, 128], F32)
    QT = dscr("qt", [4, 128, S], BF16)
    KV = dscr("kv", [NT, 128, 1032], BF16)
    QIT = dscr("qit", [4, 128, S], BF16)
    KIT = dscr("kit", [128, S], BF16)
    WI = dscr("wi", [NT, 128, 8], F32)
    YR = dscr("yr", [4, 128, S], BF16)
    YA = dscr("ya", [4, 128, S], BF16)
    X1 = dscr("x1", [S, D], F32)
    H2 = dscr("h2", [S, D], BF16)
    HG = dscr("hg", [NSLOT, D], BF16)
    YY = dscr("yy", [NSLOT, D], F32)
    RTD = dscr("rtd", [128, NT * 8], F32)

    P = Prog(nc)
    k = K(P)
    T = Tok
    st = contextlib.ExitStack()
    with st:
        A = Arena(nc, st, 206 * 1024)
        psT = [st.enter_context(nc.psum_tensor(f"ps{i}", [128, 1024], F32)) for i in range(4)]
        PSH = [[psT[i][:, 0:512], psT[i][:, 512:1024]] for i in range(4)]
        t_psh = [[Tok(f"ps{i}a", True), Tok(f"ps{i}b", True)] for i in range(4)]
        t_dram = {n_: T(n_) for n_ in ("QT", "KV", "QIT", "KIT", "WI", "YR", "YA", "X1", "H2", "HG", "YY",
                                      "MODROW", "RTD", "OUT")}

        io = A.alloc([128, 128], F32); t_io = T("io")
        identf = A.alloc([128, 128], F32); t_idf = T("identf")
        ident = A.alloc([128, 128], BF16); t_id = T("ident")
        ones_b = A.alloc([128, 512], BF16); t_ones = T("ones")
        bo64 = A.alloc([128, 128], BF16); t_bo = T("bo64")
        ustr = A.alloc([128, 128], BF16); t_ustr = T("ustr")
        modfm = A.alloc([128, 64], F32); t_mod = T("modfm")
        vec4 = A.alloc([128, 28], F32); t_vec4 = T("vec4")
        convw = A.alloc([128, 16], F32); t_convw = T("convw")
        nsp = A.alloc([128, 4], F32); t_nsp = T("nsp")
        pidx = A.alloc([128, 1], F32); t_pidx = T("pidx")
        dest4 = A.alloc([128, NT, 4], I32); t_dest4 = T("dest4")
        gate4 = A.alloc([128, NT, 4], F32); t_gate4 = T("gate4")
        widx = A.alloc([128, NBLK, 8], I32); t_widx = T("widx")
        bidx = A.alloc([128, NBLK], I32); t_bidx = T("bidx")

        k.iota(io[:], [[1, 128]], 0, -1, [t_io])
        k.ts("dve", identf[:], io[:], 0.0, None, ALU.is_equal, None, [t_io], [t_idf])
        k.cp("dve", ident[:], identf[:], [t_idf], [t_id])
        k.ts("dve", ustr[:], io[:], 0.0, None, ALU.is_gt, None, [t_io], [t_ustr])
        k.iota(pidx[:], [[0, 1]], 0, 1, [t_pidx])
        k.memset("pool", ones_b[:], 1.0, [t_ones])
        k.memset("pool", bo64[:], 0.0, [t_bo])
        k.memset("pool", bo64[0:64, 0:64], 1.0, [t_bo])
        k.memset("pool", bo64[64:128, 64:128], 1.0, [t_bo])
        k.dma("sp", vec4[:], vec4_d[:, :], [], [t_vec4])
        k.dma("sp", convw[:], convw_d[:, :], [], [t_convw])
        CB, BA, BX, LAM, RGG, AOG = 0, 4, 8, 12, 16, 20
        QG, KG, KIG = 24, 25, 26

        A.mark()
        csb = A.alloc([128, KC], F32); t_c = T("c")
        scs = A.alloc([128, KC], F32); t_scs = T("scs")
        bada = A.alloc([128, 48], F32); t_bada = T("bada")
        n1g = A.alloc([128, KC], F32); t_n1g = T("n1g")
        n2g = A.alloc([128, KC], F32); t_n2g = T("n2g")
        wa_buf = [A.alloc([128, KC, 768], F32) for _ in range(2)]
        t_wa = [T("wa0"), T("wa1")]
        modT = A.alloc([64, 128], F32); t_modT = T("modT")
        tmp4 = A.alloc([128, 4], F32); t_tmp4 = T("tmp4")
        k.dma("sp", csb[:], c_d[:, :], [], [t_c])
        k.dma("sp", bada[:], b_ada_d[:, :], [], [t_bada])
        k.dma("sp", n1g[:], n1g_d[:, :], [], [t_n1g])
        k.dma("sp", n2g[:], n2g_d[:, :], [], [t_n2g])
        k.act(scs[:], csb[:], AF.Silu, [t_c], [t_scs])
        pmod = PSH[0][0]
        for jg in range(8):
            wb_ = wa_buf[jg % 2]
            k.dma("sp", wb_[:], w_ada_d[:, jg * 768:(jg + 1) * 768].rearrange("(kc p) n -> p kc n", p=128),
                  [], [t_wa[jg % 2]])
            for jj in range(6):
                j = jg * 6 + jj
                for kc in range(KC):
                    k.mm(pmod[:, j:j + 1], wb_[:, kc, jj * 128:(jj + 1) * 128], scs[:, kc:kc + 1],
                         kc == 0, kc == KC - 1, [t_wa[jg % 2], t_scs], [t_psh[0][0]])
        k.tt("dve", modfm[:, 0:48], pmod[:, 0:48], bada[:], ALU.add, [t_psh[0][0], t_bada], [t_mod])
        k.stt(modfm[:, 48:56], modfm[:, 8:16], 1.0, n1g[:], ALU.add, ALU.mult, [t_mod, t_n1g], [t_mod])
        k.stt(modfm[:, 56:64], modfm[:, 32:40], 1.0, n2g[:], ALU.add, ALU.mult, [t_mod, t_n2g], [t_mod])
        pTm = PSH[0][1]
        k.tr(pTm[0:64, 0:128], modfm[:, 0:64], identf[:], [t_mod, t_idf], [t_psh[0][1]])
        k.cp("dve", modT[:], pTm[0:64, 0:128], [t_psh[0][1]], [t_modT])
        k.dma("sp", MODROW[:, :], modT[:], [t_modT], [])
        mr = MODROW.rearrange("j p -> (j p)")

        def bc_row(dst, j0, tok):
            src = mr[j0 * 128:(j0 + 8) * 128].rearrange("(o n) -> o n", o=1).partition_broadcast(128)
            k.dma("sp", dst[:], src, [t_dram["MODROW"]], [tok])

        k.act(tmp4[:], vec4[:, LAM:LAM + 4], AF.Exp, [t_vec4], [t_tmp4], scale=-1.0)
        k.act(nsp[:], tmp4[:], AF.Ln, [t_tmp4], [t_nsp], bias=1.0)
        k.ts("dve", nsp[:], nsp[:], -8.0, None, ALU.mult, None, [t_nsp], [t_nsp])
        P.barrier()
        A.release()

        if stages == 0:
            A.mark()
            zt2 = A.alloc([128, D], F32); t_zt2 = T("zt2")
            k.memset("pool", zt2[:], 0.0, [t_zt2])
            k.dma("sp", out_d[0:128, :], zt2[:], [t_zt2], [])
            P.emit()
            return nc
        A.mark()
        cv_ld = [A.alloc([128, 2048], F32) for _ in range(3)]
        t_cvld = [T(f"cvld{i}") for i in range(3)]

        A.mark()
        w_in_sb = A.alloc([128, KC, 3208], BF16); t_win = T("w_in")
        wabd = A.alloc([128, 4, 128], BF16); t_wabd = T("wabd")
        wxbd = A.alloc([128, 4, 128], BF16); t_wxbd = T("wxbd")
        n = 0
        for kc in range(KC):
            sg = cv_ld[n % 3]; tg = t_cvld[n % 3]
            k.dma("sp", sg[:, 0:2048], w_in_d[kc * 128:(kc + 1) * 128, 0:2048], [], [tg])
            k.cp(["dve", "act", "pool"][n % 3], w_in_sb[:, kc, 0:2048], sg[:, 0:2048], [tg], [t_win])
            n += 1
            sg = cv_ld[n % 3]; tg = t_cvld[n % 3]
            k.dma("sp", sg[:, 0:1096], w_in_d[kc * 128:(kc + 1) * 128, 2048:3144], [], [tg])
            k.cp(["dve", "act", "pool"][n % 3], w_in_sb[:, kc, 2048:3136], sg[:, 0:1088], [tg], [t_win])
            k.cp("pool", w_in_sb[:, kc, 3136:3200], sg[:, 1024:1088], [tg], [t_win])
            k.cp("pool", w_in_sb[:, kc, 3200:3208], sg[:, 1088:1096], [tg], [t_win])
            n += 1
        for (dst, src, tk) in ((wabd, wabd_d, t_wabd), (wxbd, wxbd_d, t_wxbd)):
            sg = cv_ld[n % 3]; tg = t_cvld[n % 3]
            k.dma("sp", sg[:, 0:512], src[:, :], [], [tg])
            k.cp("dve", dst[:].rearrange("p c m -> p (c m)"), sg[:, 0:512], [tg], [tk])
            n += 1

        xt = [A.alloc([128, D], F32) for _ in range(2)]; t_xt = [T("xt0"), T("xt1")]
        xn = [A.alloc([128, D], BF16) for _ in range(2)]; t_xn = [T("xn0"), T("xn1")]
        junkb = A.alloc([128, D], BF16); t_junkb = T("junkb")
        st1 = A.alloc([128, 8], F32); t_st1 = T("st1")
        hT = [A.alloc([128, KC, 512], BF16) for _ in range(2)]; t_hT = [T("hT0"), T("hT1")]
        vsb = [A.alloc([128, 8, 65], BF16) for _ in range(2)]; t_vsb = [T("vsb0"), T("vsb1")]
        wisb = [A.alloc([128, 8], F32) for _ in range(2)]; t_wisb = [T("wisb0"), T("wisb1")]
        xr_ext = A.alloc([128, 4, 516], F32); t_xre = [T(f"xre{c}") for c in range(4)]
        carry = A.alloc([128, 4], F32); t_carry = T("carry")
        xtail = A.alloc([128, 4, 4], F32); t_xtail = T("xtail")
        NW = 8
        wf = [A.alloc([128, 512], F32) for _ in range(NW)]; t_wf = [T(f"wf{i}") for i in range(NW)]
        NWB = 6
        wb = [A.alloc([128, 512], BF16) for _ in range(NWB)]; t_wb = [T(f"wb{i}") for i in range(NWB)]
        yv = A.alloc([128, 4, 512], F32); t_yv = [T(f"yv{i}") for i in range(4)]
        xc_all = A.alloc([128, 4, 512], F32); t_xc = [T(f"xc{i}") for i in range(4)]
        xcb_all = A.alloc([128, 4, 512], BF16); t_xcb = [T(f"xcb{i}") for i in range(4)]
        ysq = A.alloc([128, 4, 512], BF16); t_ysq = [T(f"ysq{i}") for i in range(4)]
        for b in range(2):
            k.memset("pool", vsb[b][:], 1.0, [t_vsb[b]])
        k.memset("pool", xr_ext[:], 0.0, t_xre)
        k.memset("pool", carry[:], 0.0, [t_carry])
        wfi = [0]; wbi = [0]; zi = [0]

        def new_wf():
            i = wfi[0] % NW; wfi[0] += 1
            return wf[i], t_wf[i]

        def new_wb():
            i = wbi[0] % NWB; wbi[0] += 1
            return wb[i], t_wb[i]

        zbufs = [(PSH[1][0], t_psh[1][0]), (PSH[1][1], t_psh[1][1]), (PSH[2][0], t_psh[2][0])]

        def new_z():
            i = zi[0] % 3; zi[0] += 1
            return zbufs[i]

        ps_wi, t_pswi = PSH[2][1], t_psh[2][1]
        ps_ss, t_psss = PSH[3][0], t_psh[3][0]
        ps_v, t_psv = PSH[3][1], t_psh[3][1]
        gm1 = modfm[:, 48:56]
        sh1 = modfm[:, 0:8]

        for G in range(NG):
            h_T = hT[G % 2]; th = t_hT[G % 2]
            for j in range(4):
                i = 4 * G + j
                x_t = xt[i % 2]; tx = t_xt[i % 2]
                x_n = xn[i % 2]; txn = t_xn[i % 2]
                k.dma("sp", x_t[:], x_d[i * 128:(i + 1) * 128, :], [], [tx])
                k.act(junkb[:], x_t[:], AF.Square, [tx], [t_junkb, t_st1], accum=st1[:, 0:1])
                k.act(st1[:, 1:2], st1[:, 0:1], AF.Sqrt, [t_st1], [t_st1], scale=1.0 / D, bias=EPS)
                k.recip(st1[:, 2:3], st1[:, 1:2], [t_st1], [t_st1])
                k.act(x_n[:], x_t[:], AF.Copy, [tx, t_st1], [txn], scale=st1[:, 2:3])
                pt = psT[0][:, (i % 2) * 512:(i % 2 + 1) * 512].bitcast(BF16)
                tpt = t_psh[0][i % 2]
                for kc in range(KC):
                    k.tr(pt[:, kc * 128:(kc + 1) * 128], x_n[:, kc * 128:(kc + 1) * 128], ident[:],
                         [txn, t_id], [tpt])
                for kc in range(KC):
                    dst = h_T[:, kc, j * 128:(j + 1) * 128]
                    src = pt[:, kc * 128:(kc + 1) * 128]
                    if kc % 2 == 0:
                        k.act(dst, src, AF.Identity, [tpt, t_mod], [th], scale=gm1[:, kc:kc + 1],
                              bias=sh1[:, kc:kc + 1])
                    else:
                        k.ts("dve", dst, src, gm1[:, kc:kc + 1], sh1[:, kc:kc + 1], ALU.mult, ALU.add,
                             [tpt, t_mod], [th])
                if CUT < 2:
                    continue
                for kc in range(KC):
                    k.mm(ps_v[:, 0:512], h_T[:, kc, j * 128:(j + 1) * 128], w_in_sb[:, kc, 2048:2560],
                         kc == 0, kc == KC - 1, [th, t_win], [t_psv])
                for kc in range(KC):
                    k.mm(ps_wi[:, 0:8], h_T[:, kc, j * 128:(j + 1) * 128], w_in_sb[:, kc, 3200:3208],
                         kc == 0, kc == KC - 1, [th, t_win], [t_pswi])
                vb = vsb[i % 2]; tvb = t_vsb[i % 2]
                k.cp("act", vb[:, :, 0:64], ps_v[:, 0:512].rearrange("p (h d) -> p h d", d=64), [t_psv], [tvb])
                k.dma("pool", KV[i, :, 512:1032], vb[:].rearrange("p h d -> p (h d)"), [tvb], [])
                wbt = wisb[i % 2]; twb = t_wisb[i % 2]
                k.cp("dve", wbt[:], ps_wi[:, 0:8], [t_pswi], [twb])
                k.dma("pool", WI[i], wbt[:], [twb], [])

            if CUT < 3:
                continue
            cols = slice(G * 512, (G + 1) * 512)

            def zchunk(col0, M=128):
                pz, tz = new_z()
                for kc in range(KC):
                    k.mm(pz[0:M, :], w_in_sb[:, kc, col0:col0 + M], h_T[:, kc, :], kc == 0, kc == KC - 1,
                         [th, t_win], [tz])
                return pz, tz

            rnn_state = []
            for c in range(4):
                pz, tz = zchunk(c * 128)
                if G > 0:
                    k.cp("pool", xr_ext[:, c, 0:3], xtail[:, c, 0:3], [t_xtail], [t_xre[c]])
                k.cp("act", xr_ext[:, c, 3:515], pz[:, :], [tz], [t_xre[c]])
                t0, tt0 = xc_all[:, c, :], t_xc[c]
                txre = t_xre[c]
                k.ts("dve", t0[:], xr_ext[:, c, 0:512], convw[:, c * 4:c * 4 + 1], vec4[:, CB + c:CB + c + 1],
                     ALU.mult, ALU.add, [txre, t_convw, t_vec4], [tt0])
                for tap in (1, 2, 3):
                    k.stt(t0[:], xr_ext[:, c, tap:tap + 512], convw[:, c * 4 + tap:c * 4 + tap + 1], t0[:],
                          ALU.mult, ALU.add, [txre, t_convw, tt0], [tt0])
                k.cp("pool", xtail[:, c, 0:3], xr_ext[:, c, 512:515], [txre], [t_xtail])
                xcb, txcb = xcb_all[:, c, :], t_xcb[c]
                k.cp("pool", xcb, t0, [tt0], [txcb])
                rnn_state.append((t0, tt0, xcb, txcb))
            if CUT < 4:
                continue
            for c in range(4):
                pz, tz = zchunk(512 + c * 128)
                k.act(yv[:, c, :], pz[:, :], AF.Gelu_apprx_tanh, [tz], [t_yv[c]])
            if CUT < 5:
                continue
            for (base_col, gcol, dst, tkd) in ((1024, QG, QT, t_dram["QT"]), (1536, KG, None, t_dram["KV"])):
                for c in range(4):
                    pz, tz = zchunk(base_col + c * 128)
                    sq, tsq = new_wb()
                    k.act(sq[:], pz[:, :], AF.Square, [tz], [tsq])
                    qf, tqf = new_wf()
                    k.ts("dve", qf[:], pz[:, :], vec4[:, gcol:gcol + 1], None, ALU.mult, None, [tz, t_vec4], [tqf])
                    k.mm(ps_ss[:, :], bo64[:], sq[:], True, True, [t_bo, tsq], [t_psss])
                    rs, trs = new_wf()
                    k.act(rs[:], ps_ss[:, :], AF.Sqrt, [t_psss], [trs], scale=1.0 / 64, bias=EPS)
                    k.recip(rs[:], rs[:], [trs], [trs])
                    qn, tqn = new_wb()
                    k.tt("dve", qn[:], qf[:], rs[:], ALU.mult, [tqf, trs], [tqn])
                    if dst is not None:
                        k.dma("pool", dst[c, :, cols], qn[:], [tqn], [tkd])
                    else:
                        k.dma("pool", KV[4 * G:4 * G + 4, :, c * 128:(c + 1) * 128].rearrange("t p s -> p t s"),
                              qn[:].rearrange("p (t s) -> p t s", t=4), [tqn], [tkd])
            if CUT < 6:
                continue
            for c in range(4 * int(_os.environ.get("DUP", "1"))):
                c = c % 4
                pz, tz = zchunk(2560 + c * 128)
                qn, tqn = new_wb()
                k.cp("act", qn[:], pz[:, :], [tz], [tqn])
                k.dma("pool", QIT[c, :, cols], qn[:], [tqn], [])
            for _d in range(int(_os.environ.get("DVEDUP", "0"))):
                qf, tqf = new_wf()
                k.ts(_os.environ.get("DUPENG", "dve"), qf[:], xc_all[:, 0, :], 2.0, None, ALU.mult, None, [t_xc[0]], [tqf])
            if CUT < 7:
                continue
            SK = set(_os.environ.get("SKIP", "").split(","))
            pz, tz = zchunk(int(_os.environ.get("KICOL", "3072")))
            sq, tsq = new_wb()
            if "a" not in SK:
                k.act(sq[:], pz[:, :], AF.Square, [tz], [tsq])
            qf, tqf = new_wf()
            if "b" not in SK:
                k.ts("dve", qf[:], pz[:, :], vec4[:, KIG:KIG + 1], None, ALU.mult, None, [tz, t_vec4], [tqf])
            if "c" not in SK:
                k.mm(ps_ss[:, :], bo64[:], sq[:], True, True, [t_bo, tsq], [t_psss])
            rs, trs = new_wf()
            if "d" not in SK:
                k.act(rs[:], ps_ss[:, :], AF.Sqrt, [t_psss], [trs], scale=1.0 / 64, bias=EPS)
            if "e" not in SK:
                k.recip(rs[:], rs[:], [trs], [trs])
            qn, tqn = new_wb()
            if "f" not in SK:
                k.tt("dve", qn[:], qf[:], rs[:], ALU.mult, [tqf, trs], [tqn])
            if "g" not in SK:
                k.dma("sp", KIT[:, cols], qn[:], [tqn], [])
            if CUT < 8:
                continue
            for c in range(4):
                xc, txc, xcb, txcb = rnn_state[c]
                pa, tpa = new_z()
                k.mm(pa[:, :], wabd[:, c, :], xcb[:], True, True, [t_wabd, txcb], [tpa])
                r, tr_ = new_wf()
                k.act(r[:], pa[:, :], AF.Sigmoid, [tpa, t_vec4], [tr_], bias=vec4[:, BA + c:BA + c + 1])
                px, tpx = new_z()
                k.mm(px[:, :], wxbd[:, c, :], xcb[:], True, True, [t_wxbd, txcb], [tpx])
                ig, tig = new_wf()
                k.act(ig[:], px[:, :], AF.Sigmoid, [tpx, t_vec4], [tig], bias=vec4[:, BX + c:BX + c + 1])
                a, ta = new_wf()
                k.act(a[:], r[:], AF.Exp, [tr_, t_nsp], [ta], scale=nsp[:, c:c + 1])
                k.tt("pool", r[:], a[:], a[:], ALU.mult, [ta], [tr_])
                k.act(r[:], r[:], AF.Sqrt, [tr_], [tr_], scale=-1.0, bias=1.0)
                k.tt("dve", ig[:], ig[:], r[:], ALU.mult, [tig, tr_], [tig])
                k.tt("dve", ig[:], ig[:], xc[:], ALU.mult, [tig, txc], [tig])
                hsc, thsc = new_wf()
                P.add("dve", (lambda o_, a_, u_, i_: (lambda e: e.tensor_tensor_scan(
                    out=o_, data0=a_, data1=u_, initial=i_, op0=ALU.mult, op1=ALU.add)))(
                        hsc[:], a[:], ig[:], carry[:, c:c + 1]), [ta, tig, t_carry], [thsc])
                k.cp("dve", carry[:, c:c + 1], hsc[:, 511:512], [thsc], [t_carry])
                k.tt("dve", yv[:, c, :], yv[:, c, :], hsc[:], ALU.mult, [t_yv[c], thsc], [t_yv[c]])
                k.act(ysq[:, c, :], yv[:, c, :], AF.Square, [t_yv[c]], [t_ysq[c]])
            if CUT < 9:
                continue
            for c in range(4):
                k.mm(ps_ss[:, :], ones_b[:, 0:128], ysq[:, c, :], c == 0, c == 3, [t_ones, t_ysq[c]], [t_psss])
            rs, trs = new_wf()
            k.act(rs[:], ps_ss[:, :], AF.Sqrt, [t_psss], [trs], scale=1.0 / 512, bias=EPS)
            k.recip(rs[:], rs[:], [trs], [trs])
            for c in range(4):
                yn, tyn = new_wb()
                k.stt(yn[:], yv[:, c, :], vec4[:, RGG + c:RGG + c + 1], rs[:], ALU.mult, ALU.mult,
                      [t_yv[c], t_vec4, trs], [tyn])
                k.dma("pool", YR[c, :, cols], yn[:], [tyn], [])
        P.barrier()
        A.release()
        A.release()

        if stages >= 2:
            A.mark()
            kiT = A.alloc([128, S], BF16); t_kiT = T("kiT")
            score = [A.alloc([128, S], F32) for _ in range(2)]; t_score = [T("score0"), T("score1")]
            negm = [A.alloc([128, S], BF16) for _ in range(3)]; t_negm = [T("negm0"), T("negm1"), T("negm2")]
            junk8 = A.alloc([128, S], U8)
            I4 = A.alloc([128, 512], BF16); t_I4 = T("I4")
            zb = A.alloc([128, 260], BF16); t_zb = T("zb")
            pow2 = A.alloc([128, NIT + 1], F32); t_pow2 = T("pow2")
            qT_t = [A.alloc([128, 4, 128], BF16) for _ in range(2)]; t_qT = [T("qT0"), T("qT1")]
            qiT_t = [A.alloc([128, 4, 128], BF16) for _ in range(3)]; t_qiT = [T("qiT0"), T("qiT1"), T("qiT2")]
            wi_t = [A.alloc([128, 8], F32) for _ in range(3)]; t_wi = [T("wi0"), T("wi1"), T("wi2")]
            NTR = 6
            trelu = [A.alloc([128, 512], F32) for _ in range(NTR)]; t_trelu = [T(f"trelu{i}") for i in range(NTR)]
            pm = [A.alloc([128, 1024], BF16) for _ in range(2)]; t_pm = [T("pm0"), T("pm1")]
            NKV = 4
            kv = [A.alloc([128, 1032], BF16) for _ in range(NKV)]; t_kv = [T(f"kv{i}") for i in range(NKV)]
            bis = [A.alloc([128, 8], F32) for _ in range(2)]; t_bis = [T("bis0"), T("bis1")]
            dall = [A.alloc([128, NIT + 1], F32) for _ in range(2)]; t_dall = [T("dall0"), T("dall1")]
            yaf = A.alloc([128, 8, 64], F32); t_yaf = T("yaf")
            yab = A.alloc([128, 512], BF16); t_yab = T("yab")
            yaT = A.alloc([128, 4, 128], BF16); t_yaT = T("yaT")
            fin = A.alloc([128, 16], F32); t_fin = T("fin")

            k.dma("sp", kiT[:, :], KIT[:, :], [t_dram["KIT"]], [t_kiT])
            for r4 in range(4):
                k.cp("pool", I4[:, r4 * 128:(r4 + 1) * 128], ident[:], [t_id], [t_I4])
            k.memset("pool", zb[:], 0.0, [t_zb])
            for it in range(NIT + 1):
                k.memset("pool", pow2[:, it:it + 1], float(2.0 ** (-it)), [t_pow2])

            ps_i = [PSH[3][0], PSH[3][1]]; t_psi = [t_psh[3][0], t_psh[3][1]]
            psl = [psT[0], psT[1]]
            pso = [PSH[2][0], PSH[2][1]]; t_pso = [t_psh[2][0], t_psh[2][1]]
            cnt_i = [0]; cnt_v = [0]

            def A_pieces(qt):
                L = (qt + 1) * 128
                b = qt % 3
                s2 = qt % 2
                sc_ = score[s2]; tsc = t_score[s2]
                bs = bis[s2]; tbs = t_bis[s2]
                dl = dall[s2]; tdl = t_dall[s2]
                pcs = []

                def ld():
                    k.dma("sp", qiT_t[b][:], QIT.rearrange("c p s -> p c s")[:, :, qt * 128:(qt + 1) * 128],
                          [t_dram["QIT"]], [t_qiT[b]])
                    k.dma("sp", wi_t[b][:], WI[qt], [t_dram["WI"]], [t_wi[b]])
                pcs.append(ld)
                items = [(g, min(512, L - g * 512), h) for g in range((L + 511) // 512) for h in range(8)]
                LAG = 3
                used_tr = {}

                def mk(idx):
                    def pc():
                        if idx < len(items):
                            g, n_, h = items[idx]
                            c = h // 2; base = (h % 2) * 64
                            ib = cnt_i[0] % 2; itr = cnt_i[0] % NTR; cnt_i[0] += 1
                            used_tr[idx] = itr
                            k.mm(ps_i[ib][:, 0:n_], qiT_t[b][base:base + 64, c, :],
                                 kiT[base:base + 64, g * 512:g * 512 + n_], True, True,
                                 [t_qiT[b], t_kiT], [t_psi[ib]])
                            k.act(trelu[itr][:, 0:n_], ps_i[ib][:, 0:n_], AF.Relu, [t_psi[ib]], [t_trelu[itr]])
                        j = idx - LAG
                        if j >= 0:
                            g, n_, h = items[j]
                            itr = used_tr[j]
                            sc = sc_[:, g * 512:g * 512 + n_]
                            if h == 0:
                                k.ts("dve", sc, trelu[itr][:, 0:n_], wi_t[b][:, 0:1], None, ALU.mult, None,
                                     [t_trelu[itr], t_wi[b]], [tsc])
                            else:
                                k.stt(sc, trelu[itr][:, 0:n_], wi_t[b][:, h:h + 1], sc, ALU.mult, ALU.add,
                                      [t_trelu[itr], t_wi[b], tsc], [tsc])
                    return pc
                for idx in range(len(items) + LAG):
                    pcs.append(mk(idx))

                def prep():
                    if L > NSEL:
                        P.add("dve", lambda e: e.tensor_reduce(out=bs[:, 0:1], in_=sc_[:, 0:L], axis=AX.X,
                                                               op=ALU.max, apply_absolute_value=True),
                              [tsc], [tbs])
                        k.ts("dve", dl[:], pow2[:], bs[:, 0:1], None, ALU.mult, None, [t_pow2, tbs], [tdl])
                        k.memset("dve", bs[:, 1:2], 0.0, [tbs])
                        k.memset("dve", bs[:, 5:6], 0.0, [tbs])
                    else:
                        k.memset("dve", bs[:, 4:5], -1e29, [tbs])
                    k.memset("dve", sc_[0:64, L - 64:L], -1e30, [tsc])
                pcs.append(prep)
                nsplit = len(pcs)
                use_act = (qt % 2 == 1)
                if L > NSEL:
                    for it in range(NIT):
                        def pc(it=it):
                            if not use_act:
                                k.ts("dve", junk8[:, 0:L], sc_[:, 0:L], bs[:, 1:2], None, ALU.is_gt, ALU.add,
                                     [tsc, tbs], [tbs], accum=bs[:, 2:3])
                                k.ts("dve", bs[:, 3:4], bs[:, 2:3], float(NSEL), -0.5, ALU.is_ge, ALU.add,
                                     [tbs], [tbs])
                            else:
                                k.act(negm[b][:, 0:L], sc_[:, 0:L], AF.Sign, [tsc, tbs], [t_negm[b], tbs],
                                      bias=bs[:, 5:6], accum=bs[:, 2:3])
                                k.ts("dve", bs[:, 3:4], bs[:, 2:3], float(2 * NSEL - L), -0.5, ALU.is_ge, ALU.add,
                                     [tbs], [tbs])
                            k.stt(bs[:, 1:2], bs[:, 3:4], dl[:, it:it + 1], bs[:, 1:2], ALU.mult, ALU.add,
                                  [tbs, tdl], [tbs])
                            if use_act:
                                k.ts("dve", bs[:, 5:6], bs[:, 1:2], -1.0, None, ALU.mult, None, [tbs], [tbs])
                        pcs.append(pc)
                    nsplit += NIT // 4

                def fin_():
                    if L > NSEL:
                        k.tt("dve", bs[:, 4:5], bs[:, 1:2], dl[:, NIT:NIT + 1], ALU.subtract,
                             [tbs, tdl], [tbs])
                    k.ts("dve", negm[b][:, 0:L], sc_[:, 0:L], bs[:, 4:5], -30000.0, ALU.is_le, ALU.mult,
                         [tsc, tbs], [t_negm[b]])
                pcs.append(fin_)
                return pcs[:nsplit], pcs[nsplit:]

            def ld_q(qt):
                b = qt % 2
                k.dma("sp", qT_t[b][:], QT.rearrange("c p s -> p c s")[:, :, qt * 128:(qt + 1) * 128],
                      [t_dram["QT"]], [t_qT[b]])

            def B_pieces(qt):
                b = qt % 2
                nb3 = qt % 3
                pcs = []

                def init():
                    if qt + 1 < NT:
                        ld_q(qt + 1)
                    for hb in range(2):
                        k.mm(pso[hb][:, 0:260], zb[:, 0:128], zb[:, 0:260], True, False, [t_zb], [t_pso[hb]])
                pcs.append(init)
                st_ = {}

                def mkb(kt):
                    def pc():
                        if kt <= qt:
                            iv = cnt_v[0] % NKV; lb = cnt_v[0] % 2; cnt_v[0] += 1
                            st_[kt] = (iv, lb)
                            k.dma("sp", kv[iv][:], KV[kt], [t_dram["KV"]], [t_kv[iv]])
                            tl = t_psh[lb][0]
                            for half in range(2):
                                k.mm(psl[lb][:, half * 512:(half + 1) * 512], negm[nb3][:, kt * 128:(kt + 1) * 128],
                                     I4[:, :], True, False, [t_negm[nb3], t_I4], [tl])
                            for h in range(8):
                                c = h // 2; base = (h % 2) * 64
                                j = (h % 2) * 4 + h // 2
                                k.mm(psl[lb][:, j * 128:(j + 1) * 128], kv[iv][base:base + 64, c * 128:(c + 1) * 128],
                                     qT_t[b][base:base + 64, c, :], False, (h >= 6), [t_kv[iv], t_qT[b]], [tl])
                            k.act(pm[lb][:], psl[lb][:, :], AF.Exp, [tl], [t_pm[lb]], scale=0.125)
                        kp = kt - 1
                        if kp >= 0:
                            iv, lb = st_[kp]
                            for h in range(8):
                                hb = h // 4; o = (h % 4) * 65
                                j = (h % 2) * 4 + h // 2
                                k.mm(pso[hb][:, o:o + 65], pm[lb][:, j * 128:(j + 1) * 128],
                                     kv[iv][:, 512 + h * 65:512 + (h + 1) * 65], False, kp == qt,
                                     [t_pm[lb], t_kv[iv]], [t_pso[hb]])
                    return pc
                for kt in range(qt + 2):
                    pcs.append(mkb(kt))
                return pcs

            def finalize(qt):
                cols = slice(qt * 128, (qt + 1) * 128)
                for hb in range(2):
                    v3 = pso[hb][:, 0:260].rearrange("p (h e) -> p h e", e=65)
                    k.recip(fin[:, hb * 4:(hb + 1) * 4], v3[:, :, 64], [t_pso[hb]], [t_fin])
                    k.tt("dve", yaf[:, hb * 4:(hb + 1) * 4, :], v3[:, :, 0:64],
                         fin[:, hb * 4:(hb + 1) * 4].unsqueeze(2).to_broadcast([128, 4, 64]), ALU.mult,
                         [t_pso[hb], t_fin], [t_yaf])
                yf = yaf[:].rearrange("p h d -> p (h d)")
                k.act(yab[:], yf, AF.Square, [t_yaf], [t_yab, t_fin], accum=fin[:, 8:9])
                k.act(fin[:, 9:10], fin[:, 8:9], AF.Sqrt, [t_fin], [t_fin], scale=1.0 / 512, bias=EPS)
                k.recip(fin[:, 10:11], fin[:, 9:10], [t_fin], [t_fin])
                k.act(yab[:], yf, AF.Copy, [t_yaf, t_fin], [t_yab], scale=fin[:, 10:11])
                ptb = PSH[3][0].bitcast(BF16)
                for c in range(4):
                    k.tr(ptb[:, c * 128:(c + 1) * 128], yab[:, c * 128:(c + 1) * 128], ident[:],
                         [t_yab, t_id], [t_psh[3][0]])
                for c in range(4):
                    k.act(yaT[:, c, :], ptb[:, c * 128:(c + 1) * 128], AF.Copy, [t_psh[3][0], t_vec4], [t_yaT],
                          scale=vec4[:, AOG + c:AOG + c + 1])
                k.dma("pool", YA.rearrange("c p s -> p c s")[:, :, cols], yaT[:], [t_yaT], [])

            def run_pieces(lists):
                tot = max(len(l_) for l_ in lists)
                idx = [0] * len(lists)
                for step in range(tot):
                    for li, l_ in enumerate(lists):
                        tgt = (step + 1) * len(l_) // tot
                        while idx[li] < tgt:
                            l_[idx[li]]()
                            idx[li] += 1

            ld_q(0)
            AP_ = {}

            def get_A(q):
                if q not in AP_:
                    AP_[q] = A_pieces(q)
                return AP_[q]

            run_pieces([get_A(0)[0]])
            lists0 = [get_A(0)[1]]
            if NT > 1:
                lists0.append(get_A(1)[0])
            run_pieces(lists0)
            for qt in range(NT):
                lists = [B_pieces(qt)]
                if qt + 1 < NT:
                    lists.append(get_A(qt + 1)[1])
                if qt + 2 < NT:
                    lists.append(get_A(qt + 2)[0])
                run_pieces(lists)
                finalize(qt)
                AP_.pop(qt, None)
            P.barrier()
            A.release()

        if stages >= 3:
            A.mark()
            maskd = A.alloc([128, NT, NE], F32); t_maskd = T("maskd")
            gated = A.alloc([128, NT, NE], F32); t_gated = T("gated")
            rankd = A.alloc([128, NT, NE], F32); t_rankd = T("rankd")
            basec = A.alloc([128, NE], F32); t_basec = T("basec")
            A.mark()
            g1bc = A.alloc([128, D], F32); t_g1bc = T("g1bc")
            gm2bc = A.alloc([128, D], F32); t_gm2bc = T("gm2bc")
            sh2bc = A.alloc([128, D], F32); t_sh2bc = T("sh2bc")
            bc_row(g1bc, 16, t_g1bc)
            bc_row(gm2bc, 56, t_gm2bc)
            bc_row(sh2bc, 24, t_sh2bc)
            w_out_sb = A.alloc([128, KC, D], BF16); t_wout = T("w_out")
            w_rt = A.alloc([128, KC, NE], F32); t_wrt = T("w_rt")
            brt = A.alloc([128, NE], F32); t_brt = T("brt")
            stg = [A.alloc([128, D], F32) for _ in range(2)]; t_stg = [T("stg0"), T("stg1")]
            for kc in range(KC):
                k.dma("sp", stg[kc % 2][:], w_out_d[kc * 128:(kc + 1) * 128, :], [], [t_stg[kc % 2]])
                k.cp(["dve", "act"][kc % 2], w_out_sb[:, kc, :], stg[kc % 2][:], [t_stg[kc % 2]], [t_wout])
            k.dma("sp", w_rt[:].rearrange("p c e -> p (c e)"), w_rt_d[:, :], [], [t_wrt])
            k.dma("sp", brt[:], b_rt_d[0:1, :].partition_broadcast(128), [], [t_brt])
            k.memset("pool", basec[:], 0.0, [t_basec])
            zt = A.alloc([128, 4, D], BF16); t_zt = T("zt")
            k.memset("pool", zt[:], 0.0, [t_zt])
            for jb in range(NBLK):
                k.dma("pool", HG[jb * BLK:(jb + 1) * BLK, :].rearrange("(a p) d -> p a d", p=128), zt[:],
                      [t_zt], [])
            x_t = [A.alloc([128, D], F32) for _ in range(2)]; t_x2 = [T("x2a"), T("x2b")]
            cat = [A.alloc([128, 8, 128], BF16) for _ in range(2)]; t_cat = [T("cat0"), T("cat1")]
            x1 = [A.alloc([128, D], F32) for _ in range(2)]; t_x1 = [T("x1a"), T("x1b")]
            h2 = [A.alloc([128, D], F32) for _ in range(2)]; t_h2 = [T("h2a"), T("h2b")]
            h2b = [A.alloc([128, D], BF16) for _ in range(2)]; t_h2b = [T("h2ba"), T("h2bb")]
            h2T = [A.alloc([128, KC, 128], F32) for _ in range(2)]; t_h2T = [T("h2Ta"), T("h2Tb")]
            rt = [A.alloc([128, 64], F32) for _ in range(2)]; t_rt = [T("rta"), T("rtb")]
            lg = [A.alloc([128, NE], F32) for _ in range(2)]; t_lg = [T("lga"), T("lgb")]
            ex = [A.alloc([128, NE], F32) for _ in range(2)]; t_ex = [T("exa"), T("exb")]
            mb = [A.alloc([128, NE], BF16) for _ in range(2)]; t_mb = [T("mba"), T("mbb")]
            jk2 = [A.alloc([128, D], BF16) for _ in range(2)]; t_jk2 = [T("jk2a"), T("jk2b")]
            ps_mix = [PSH[0][0], PSH[0][1]]; t_psmix = [t_psh[0][0], t_psh[0][1]]
            ps_trs = [psT[1], psT[3]]; t_pstrs = [t_psh[1][0], t_psh[3][0]]
            ps_rs = [PSH[2][0], PSH[2][1]]; t_psrs = [t_psh[2][0], t_psh[2][1]]

            def tile_pieces(i):
                b = i % 2
                cols = slice(i * 128, (i + 1) * 128)
                ps_tr = ps_trs[b]; t_pstr = t_pstrs[b]
                ps_r = ps_rs[b]; t_psr = t_psrs[b]
                pcs = []

                def p0():
                    k.dma("sp", x_t[b][:], x_d[cols, :], [], [t_x2[b]])
                    k.dma("sp", cat[b][:, 0:4, :], YR.rearrange("c p s -> p c s")[:, :, cols], [t_dram["YR"]], [t_cat[b]])
                    k.dma("sp", cat[b][:, 4:8, :], YA.rearrange("c p s -> p c s")[:, :, cols], [t_dram["YA"]], [t_cat[b]])
                pcs.append(p0)

                def p1():
                    for half in range(2):
                        for c in range(8):
                            k.mm(ps_mix[half][:, :], cat[b][:, c, :], w_out_sb[:, c, half * 512:(half + 1) * 512],
                                 c == 0, c == 7, [t_cat[b], t_wout], [t_psmix[half]])
                    for half in range(2):
                        hs_ = slice(half * 512, (half + 1) * 512)
                        k.tt("dve", x1[b][:, hs_], ps_mix[half][:, :], g1bc[:, hs_], ALU.mult,
                             [t_psmix[half], t_g1bc], [t_x1[b]])
                pcs.append(p1)

                def p2():
                    k.tt("pool", x1[b][:], x1[b][:], x_t[b][:], ALU.add, [t_x1[b], t_x2[b]], [t_x1[b]])
                    k.dma("pool", X1[cols, :], x1[b][:], [t_x1[b]], [])
                    k.act(jk2[b][:], x1[b][:], AF.Square, [t_x1[b]], [t_jk2[b], t_rt[b]], accum=rt[b][:, 0:1])
                pcs.append(p2)
                pcs.append(lambda: k.act(rt[b][:, 1:2], rt[b][:, 0:1], AF.Sqrt, [t_rt[b]], [t_rt[b]], scale=1.0 / D, bias=EPS))
                pcs.append(lambda: k.recip(rt[b][:, 2:3], rt[b][:, 1:2], [t_rt[b]], [t_rt[b]]))
                pcs.append(lambda: k.stt(h2[b][:], x1[b][:], rt[b][:, 2:3], gm2bc[:], ALU.mult, ALU.mult,
                                         [t_x1[b], t_rt[b], t_gm2bc], [t_h2[b]]))
                pcs.append(lambda: k.tt("pool", h2[b][:], h2[b][:], sh2bc[:], ALU.add, [t_h2[b], t_sh2bc], [t_h2[b]]))

                def p3():
                    k.cp("act", h2b[b][:], h2[b][:], [t_h2[b]], [t_h2b[b]])
                    k.dma("pool", H2[cols, :], h2b[b][:], [t_h2b[b]], [])
                    for kc in range(KC):
                        k.tr(ps_tr[:, kc * 128:(kc + 1) * 128], h2[b][:, kc * 128:(kc + 1) * 128], identf[:],
                             [t_h2[b], t_idf], [t_pstr])
                pcs.append(p3)
                pcs.append(lambda: k.cp("act", h2T[b][:].rearrange("p c t -> p (c t)"), ps_tr[:, :], [t_pstr], [t_h2T[b]]))

                def p4():
                    for kc in range(KC):
                        k.mm(ps_r[:, 0:NE], h2T[b][:, kc, :], w_rt[:, kc, :], kc == 0, kc == KC - 1,
                             [t_h2T[b], t_wrt], [t_psr])
                pcs.append(p4)
                pcs.append(lambda: k.tt("dve", lg[b][:], ps_r[:, 0:NE], brt[:], ALU.add, [t_psr, t_brt], [t_lg[b]]))
                pcs.append(lambda: P.add("dve", (lambda o_, i_: (lambda e: e.max(out=o_, in_=i_)))(rt[b][:, 8:16], lg[b][:]),
                                         [t_lg[b]], [t_rt[b]]))
                pcs.append(lambda: k.ts("dve", maskd[:, i, :], lg[b][:], rt[b][:, 11:12], None, ALU.is_ge, None,
                                        [t_lg[b], t_rt[b]], [t_maskd]))

                def p5():
                    k.cp("dve", mb[b][:], maskd[:, i, :], [t_maskd], [t_mb[b]])
                    k.ts("dve", rt[b][:, 3:4], rt[b][:, 8:9], -1.0, None, ALU.mult, None, [t_rt[b]], [t_rt[b]])
                pcs.append(p5)

                def p6():
                    k.act(ex[b][:], lg[b][:], AF.Exp, [t_lg[b], t_rt[b]], [t_ex[b]], bias=rt[b][:, 3:4])
                    k.mm(ps_r[:, 32:64], ustr[:], mb[b][:], True, True, [t_ustr, t_mb[b]], [t_psr])
                    k.mm(ps_r[:, 64:96], ones_b[:, 0:128], mb[b][:], True, True, [t_ones, t_mb[b]], [t_psr])
                pcs.append(p6)
                pcs.append(lambda: k.stt(ex[b][:], ex[b][:], 1.0, maskd[:, i, :], ALU.mult, ALU.mult,
                                         [t_ex[b], t_maskd], [t_ex[b], t_rt[b]], accum=rt[b][:, 4:5]))
                pcs.append(lambda: k.recip(rt[b][:, 5:6], rt[b][:, 4:5], [t_rt[b]], [t_rt[b]]))
                pcs.append(lambda: k.ts("dve", gated[:, i, :], ex[b][:], rt[b][:, 5:6], None, ALU.mult, None,
                                        [t_ex[b], t_rt[b]], [t_gated]))

                def p7():
                    k.tt("dve", rankd[:, i, :], ps_r[:, 32:64], basec[:], ALU.add, [t_psr, t_basec], [t_rankd])
                    k.tt("dve", basec[:], ps_r[:, 64:96], basec[:], ALU.add, [t_psr, t_basec], [t_basec])
                pcs.append(p7)
                return pcs

            for i0_ in range(0, NT, 2):
                run_pieces([tile_pieces(i0_), tile_pieces(i0_ + 1)])
            P.barrier()
            A.release()

        if stages >= 4:
            A.mark()
            jrow = A.alloc([128, JMAX], F32); t_jrow = T("jrow")
            cmp3 = A.alloc([128, NE, JMAX], F32); t_cmp3 = T("cmp3")
            nb = A.alloc([128, NE], F32); t_nb = T("nb")
            incl = A.alloc([128, NE], F32); t_incl = T("incl")
            pst = A.alloc([128, NE], F32); t_pst = T("pst")
            onesf = A.alloc([128, NE], F32); t_onesf = T("onesf")
            jb_ = A.alloc([128, NBLK], F32); t_jb = T("jb")
            cmpb = A.alloc([128, NBLK, NE], F32); t_cmpb = T("cmpb")
            be = A.alloc([128, NBLK], F32); t_be = T("be")
            widxf = A.alloc([128, NBLK, 8], F32); t_widxf = T("widxf")
            pbig = A.alloc([128, 1], F32); t_pbig = T("pbig")
            bef = A.alloc([128, NBLK], F32); t_bef = T("bef")
            key3 = A.alloc([128, NT, NE], F32); t_key3 = T("key3")
            top8 = A.alloc([128, 8], F32); t_top8 = T("top8")
            d4f = A.alloc([128, NT, 4], F32); t_d4f = T("d4f")
            jk32 = A.alloc([128, NE], F32); t_jk32 = T("jk32")
            h2l = [A.alloc([128, D], BF16) for _ in range(3)]; t_h2l = [T(f"h2l{i}") for i in range(3)]
            k.iota(jrow[:], [[BLK, JMAX]], 0, 0, [t_jrow])
            k.iota(jb_[:], [[1, NBLK]], 0, 0, [t_jb])
            k.memset("pool", onesf[:], 1.0, [t_onesf])
            k.tt("dve", cmp3[:], jrow[:].unsqueeze(1).to_broadcast([128, NE, JMAX]),
                 basec[:].unsqueeze(2).to_broadcast([128, NE, JMAX]), ALU.is_lt, [t_jrow, t_basec], [t_cmp3])
            P.add("dve", lambda e: e.tensor_reduce(out=nb[:], in_=cmp3[:], axis=AX.X, op=ALU.add), [t_cmp3], [t_nb])
            P.add("dve", lambda e: e.tensor_tensor_scan(out=incl[:], data0=onesf[:], data1=nb[:], initial=0.0,
                                                        op0=ALU.mult, op1=ALU.add), [t_onesf, t_nb], [t_incl])
            k.tt("dve", pst[:], incl[:], nb[:], ALU.subtract, [t_incl, t_nb], [t_pst])
            k.ts("dve", pst[:], pst[:], float(BLK), None, ALU.mult, None, [t_pst], [t_pst])
            k.tt("dve", cmpb[:], incl[:].unsqueeze(1).to_broadcast([128, NBLK, NE]),
                 jb_[:].unsqueeze(2).to_broadcast([128, NBLK, NE]), ALU.is_le, [t_incl, t_jb], [t_cmpb])
            P.add("dve", lambda e: e.tensor_reduce(out=be[:], in_=cmpb[:], axis=AX.X, op=ALU.add), [t_cmpb], [t_be])
            k.ts("dve", be[:], be[:], float(NE - 1), None, ALU.min, None, [t_be], [t_be])
            k.cp("dve", bidx[:], be[:], [t_be], [t_bidx])
            k.ts("dve", be[:], be[:], 1024.0, pidx[:, 0:1], ALU.mult, ALU.add, [t_be, t_pidx], [t_be])
            for kc in range(KC):
                k.ts("dve", widxf[:, :, kc], be[:], float(kc * 128), None, ALU.add, None, [t_be], [t_widxf])
            k.cp("dve", widx[:].rearrange("p b c -> p (b c)"), widxf[:].rearrange("p b c -> p (b c)"),
                 [t_widxf], [t_widx])
            k.tt("dve", key3[:], rankd[:], pst[:].unsqueeze(1).to_broadcast([128, NT, NE]), ALU.add,
                 [t_rankd, t_pst], [t_key3])
            k.ts("dve", key3[:], key3[:], -1.0, BIGC, ALU.mult, ALU.add, [t_key3], [t_key3])
            k.tt("dve", key3[:], key3[:], maskd[:], ALU.mult, [t_key3, t_maskd], [t_key3])
            for i in range(NT):
                P.add("dve", (lambda o_, i_: (lambda e: e.max(out=o_, in_=i_)))(top8[:], key3[:, i, :]),
                      [t_key3], [t_top8])
                k.ts("dve", d4f[:, i, :], top8[:, 0:4], -1.0, BIGC, ALU.mult, ALU.add, [t_top8], [t_d4f])
                for k4 in range(4):
                    k.stt(jk32[:], key3[:, i, :], top8[:, k4:k4 + 1], gated[:, i, :], ALU.is_equal, ALU.mult,
                          [t_key3, t_top8, t_gated], [t_jk32, t_gate4], accum=gate4[:, i, k4:k4 + 1])
            k.cp("dve", dest4[:].rearrange("p t f -> p (t f)"), d4f[:].rearrange("p t f -> p (t f)"),
                 [t_d4f], [t_dest4])
            if debug:
                k.dma("sp", RTD[:, 0:NT * 4], d4f[:].rearrange("p t f -> p (t f)"), [t_d4f], [])
                k.dma("sp", RTD[:, NT * 4:NT * 8], gate4[:].rearrange("p t f -> p (t f)"), [t_gate4], [])
            for i in range(NT):
                hb_ = h2l[i % 3]; thb = t_h2l[i % 3]
                k.dma("sp", hb_[:], H2[i * 128:(i + 1) * 128, :], [t_dram["H2"]], [thb])
                for k4 in range(4):
                    k.scatter(HG[:, :], hb_[:], dest4[:, i, k4:k4 + 1], [thb, t_dest4], [])
            P.barrier()
            A.release()
            A.release()

            A.mark()
            w1sb = [A.alloc([128, 9 * 2048], BF16) for _ in range(2)]; t_w1sb = [T("w1sb0"), T("w1sb1")]
            w2sb = [A.alloc([128, 9 * 1024], BF16) for _ in range(2)]; t_w2sb = [T("w2sb0"), T("w2sb1")]
            hg = [A.alloc([128, 4, D], BF16) for _ in range(2)]; t_hg = [T("hg0"), T("hg1")]
            hgT = A.alloc([128, KC, 512], BF16); t_hgT = [T(f"hgT{c}") for c in range(KC)]
            actT = A.alloc([128, KC, 512], BF16); t_actT = [T(f"actT{c}") for c in range(KC)]
            NE4 = 6
            ew = [A.alloc([128, 512], F32) for _ in range(NE4)]; t_ew = [T(f"ew{i}") for i in range(NE4)]
            ysb = [A.alloc([128, D], F32) for _ in range(2)]; t_ysb = [T("ysb0"), T("ysb1")]
            ewi = [0]

            def new_ew():
                i = ewi[0] % NE4; ewi[0] += 1
                return ew[i], t_ew[i]

            ps_t4 = [PSH[0][0].bitcast(BF16), PSH[0][1].bitcast(BF16)]; t_pst4 = [t_psh[0][0], t_psh[0][1]]
            ps_gl = [(PSH[1][0], t_psh[1][0], PSH[1][1], t_psh[1][1]), (PSH[2][0], t_psh[2][0], PSH[2][1], t_psh[2][1])]
            ps_y = [PSH[3][0], PSH[3][1]]; t_psy = [t_psh[3][0], t_psh[3][1]]
            ntr = 0; ngl = 0; ny = 0; nwf = 0
            NWS = 4
            wst = [A.alloc([128, 2048], F32) for _ in range(NWS)]; t_wst = [T(f"wst{i}") for i in range(NWS)]
            w1rows = w1_d.rearrange("e k n -> (e k) n")
            w2rows = w2_d.rearrange("e k n -> (e k) n")
            nwf_ = [0]

            def wsteps(jb):
                b = jb % 2
                for kc in range(KC):
                    s_ = nwf_[0] % NWS; nwf_[0] += 1
                    k.gather(wst[s_][:, :], w1rows[:, :], widx[:, jb, kc:kc + 1], [t_widx], [t_wst[s_]])
                    k.cp("act",
                         w1sb[b][:, kc * 2048:(kc + 1) * 2048].rearrange("p (two f) -> p two f", two=2),
                         wst[s_][:, :].rearrange("p (f two) -> p two f", two=2), [t_wst[s_]], [t_w1sb[b]])
                    yield
                s_ = nwf_[0] % NWS; nwf_[0] += 1
                k.gather(wst[s_][:, :], b1_d[:, :], bidx[:, jb:jb + 1], [t_bidx], [t_wst[s_]])
                k.cp("dve", w1sb[b][0:1, 8 * 2048:9 * 2048].rearrange("p (two f) -> p two f", two=2),
                     wst[s_][0:1, :].rearrange("p (f two) -> p two f", two=2), [t_wst[s_]], [t_w1sb[b]])
                yield
                for kc in range(KC):
                    s_ = nwf_[0] % NWS; nwf_[0] += 1
                    k.gather(wst[s_][:, 0:1024], w2rows[:, :], widx[:, jb, kc:kc + 1], [t_widx], [t_wst[s_]])
                    k.cp("dve", w2sb[b][:, kc * 1024:(kc + 1) * 1024], wst[s_][:, 0:1024], [t_wst[s_]], [t_w2sb[b]])
                    yield
                s_ = nwf_[0] % NWS; nwf_[0] += 1
                k.gather(wst[s_][:, 0:1024], b2_d[:, :], bidx[:, jb:jb + 1], [t_bidx], [t_wst[s_]])
                k.ts("dve", w2sb[b][0:1, 8 * 1024:9 * 1024], wst[s_][0:1, 0:1024], 1.702, None, ALU.mult, None,
                     [t_wst[s_]], [t_w2sb[b]])
                yield

            def adv(gen, n_=1):
                if gen is None:
                    return
                for _ in range(n_):
                    try:
                        next(gen)
                    except StopIteration:
                        return

            g0 = wsteps(0)
            adv(g0, 100)
            for jb in range(NBLK):
                b = jb % 2
                wgen = wsteps(jb + 1) if jb + 1 < NBLK else None
                k.dma("sp", hg[b][:], HG[jb * BLK:(jb + 1) * BLK, :].rearrange("(a p) d -> p a d", p=128),
                      [t_dram["HG"]], [t_hg[b]])
                for kc in range(KC):
                    pt_ = ps_t4[ntr % 2]; tpt_ = t_pst4[ntr % 2]; ntr += 1
                    for a_ in range(4):
                        k.tr(pt_[:, a_ * 128:(a_ + 1) * 128], hg[b][:, a_, kc * 128:(kc + 1) * 128], ident[:],
                             [t_hg[b], t_id], [tpt_])
                    k.cp("act" if kc % 2 == 0 else "dve", hgT[:, kc, :], pt_[:, 0:512], [tpt_], [t_hgT[kc]])
                    adv(wgen)
                for fc in range(KC):
                    pg, tpg, pl, tpl = ps_gl[ngl % 2]; ngl += 1
                    for (pz_, tz_, off) in ((pg, tpg, 0), (pl, tpl, 1024)):
                        for kc in range(KC):
                            k.mm(pz_[:, :], w1sb[b][:, kc * 2048 + off + fc * 128:kc * 2048 + off + (fc + 1) * 128],
                                 hgT[:, kc, :], kc == 0, False, [t_w1sb[b], t_hgT[kc]], [tz_])
                        k.mm(pz_[:, :], w1sb[b][0:1, 8 * 2048 + off + fc * 128:8 * 2048 + off + (fc + 1) * 128],
                             ones_b[0:1, 0:512], False, True, [t_w1sb[b], t_ones], [tz_])
                    g_, tg_ = new_ew()
                    k.ts("dve", g_[:], pg[:, :], 7.0, None, ALU.min, None, [tpg], [tg_])
                    sl, tsl = new_ew()
                    k.act(sl[:], g_[:], AF.Silu, [tg_], [tsl], scale=1.702)
                    l_, tl_ = new_ew()
                    k.ts("dve", l_[:], pl[:, :], -7.0, 7.0, ALU.max, ALU.min, [tpl], [tl_])
                    k.stt(actT[:, fc, :], l_[:], 1.0, sl[:], ALU.add, ALU.mult, [tl_, tsl], [t_actT[fc]])
                    adv(wgen)
                for a_ in range(4):
                    yb = ysb[ny % 2]; tyb = t_ysb[ny % 2]; ny += 1
                    for dh in range(2):
                        py = ps_y[dh]; tpy = t_psy[dh]
                        for fc in range(KC):
                            k.mm(py[:, :], actT[:, fc, a_ * 128:(a_ + 1) * 128],
                                 w2sb[b][:, fc * 1024 + dh * 512:fc * 1024 + (dh + 1) * 512], fc == 0, False,
                                 [t_actT[fc], t_w2sb[b]], [tpy])
                        k.mm(py[:, :], ones_b[0:1, 0:128], w2sb[b][0:1, 8 * 1024 + dh * 512:8 * 1024 + (dh + 1) * 512],
                             False, True, [t_ones, t_w2sb[b]], [tpy])
                        k.act(yb[:, dh * 512:(dh + 1) * 512], py[:, :], AF.Copy, [tpy], [tyb], scale=1.0 / 1.702)
                    r0 = jb * BLK + a_ * 128
                    k.dma("sp", YY[r0:r0 + 128, :], yb[:], [tyb], [])
                    adv(wgen)
                adv(wgen, 100)
            P.barrier()
            A.release()

            A.mark()
            g2bc = A.alloc([128, D], F32); t_g2bc = T("g2bc")
            bc_row(g2bc, 40, t_g2bc)
            NB5 = 3
            x1l = [A.alloc([128, D], F32) for _ in range(NB5)]; t_x1l = [T(f"x1l{q}") for q in range(NB5)]
            yg = [[A.alloc([128, D], F32) for _ in range(4)] for _ in range(NB5)]
            t_yg = [[T(f"yg{b}{q}") for q in range(4)] for b in range(NB5)]
            acc = [A.alloc([128, D], F32) for _ in range(NB5)]; t_acc = [T(f"acc{q}") for q in range(NB5)]
            for i in range(NT):
                b = i % NB5
                cols = slice(i * 128, (i + 1) * 128)
                k.dma("sp", x1l[b][:], X1[cols, :], [t_dram["X1"]], [t_x1l[b]])
                for k4 in range(4):
                    k.gather(yg[b][k4][:, :], YY[:, :], dest4[:, i, k4:k4 + 1], [t_dest4, t_dram["YY"]], [t_yg[b][k4]])
                k.ts("dve", acc[b][:], yg[b][0][:], gate4[:, i, 0:1], None, ALU.mult, None,
                     [t_yg[b][0], t_gate4], [t_acc[b]])
                for k4 in range(1, 4):
                    k.stt(acc[b][:], yg[b][k4][:], gate4[:, i, k4:k4 + 1], acc[b][:], ALU.mult, ALU.add,
                          [t_yg[b][k4], t_gate4, t_acc[b]], [t_acc[b]])
                k.tt("dve", acc[b][:], acc[b][:], g2bc[:], ALU.mult, [t_acc[b], t_g2bc], [t_acc[b]])
                k.tt("dve", acc[b][:], acc[b][:], x1l[b][:], ALU.add, [t_acc[b], t_x1l[b]], [t_acc[b]])
                k.dma("sp", out_d[cols, :], acc[b][:], [t_acc[b]], [])
            A.release()
        elif stages >= 1:
            A.mark()
            zt2 = A.alloc([128, D], F32); t_zt2 = T("zt2")
            k.memset("pool", zt2[:], 0.0, [t_zt2])
            k.dma("sp", out_d[0:128, :], zt2[:], [t_zt2], [])
            A.release()
        P.emit()
    return nc


def _fm(v, nchunk):
    return np.ascontiguousarray(np.asarray(v, np.float32).reshape(nchunk, 128).T)


def prep_shared(inp, small=False):
    L = 0
    f32 = np.float32
    sh = {}
    sh["w_ada"] = np.ascontiguousarray(inp["w_ada"][L], f32)
    sh["b_ada_fm"] = _fm(inp["b_ada"][L], 48)
    sh["n1g_fm"] = _fm(inp["norm1_g"][L], 8)
    sh["n2g_fm"] = _fm(inp["norm2_g"][L], 8)
    sh["w_in"] = np.ascontiguousarray(inp["w_in"][L], f32)
    cw = np.asarray(inp["conv_w"][L], f32)
    sh["convw_fm"] = np.ascontiguousarray(cw.T.reshape(4, 128, 4).transpose(1, 0, 2).reshape(128, 16))
    v4 = np.zeros((128, 28), f32)
    for j, name in enumerate(["conv_b", "b_rg_a", "b_rg_x", "lru_lambda", "rg_out_g", "attn_out_g"]):
        v4[:, j * 4:(j + 1) * 4] = _fm(inp[name][L], 4)
    v4[:, 24] = np.tile(np.asarray(inp["q_norm_g"][L], f32), 2)
    v4[:, 25] = np.tile(np.asarray(inp["k_norm_g"][L], f32), 2)
    v4[:, 26] = np.tile(np.asarray(inp["kidx_norm_g"][L], f32), 2)
    sh["vec4_fm"] = v4
    for nm, key in (("wabd", "w_rg_a"), ("wxbd", "w_rg_x")):
        w = np.asarray(inp[key][L], f32)
        bd = np.zeros((128, 4, 128), f32)
        for c in range(4):
            bd[0:64, c, 0:64] = w[2 * c]
            bd[64:128, c, 64:128] = w[2 * c + 1]
        sh[nm] = bd.reshape(128, 512)
    sh["w_out"] = np.ascontiguousarray(inp["w_out"][L], f32)
    wr = np.asarray(inp["w_router"][L], f32)
    sh["w_rt_fm"] = np.ascontiguousarray(wr.reshape(8, 128, 32).transpose(1, 0, 2).reshape(128, 256))
    sh["b_rt"] = np.asarray(inp["b_router"][L], f32).reshape(1, 32)
    if not small:
        sh["w1"] = np.ascontiguousarray(inp["w1"][L], f32)
        sh["b1"] = np.ascontiguousarray(inp["b1"][L], f32)
        sh["w2"] = np.ascontiguousarray(inp["w2"][L], f32)
        sh["b2"] = np.ascontiguousarray(inp["b2"][L], f32)
    return sh


def kernel(**inputs):
    x = np.asarray(inputs["x"], np.float32)
    c = np.asarray(inputs["c"], np.float32)
    B, S, _ = x.shape
    sh = prep_shared(inputs)
    nc = build_nc(S)
    in_maps = []
    for b in range(B):
        m = dict(sh)
        m["x"] = np.ascontiguousarray(x[b])
        m["c_fm"] = _fm(c[b], 8)
        in_maps.append(m)
    res = run_bass_kernel_spmd(nc, in_maps, core_ids=list(range(B)))
    return np.stack([np.asarray(r["out"], np.float32) for r in res.results], axis=0)
```

```python
import numpy as np
import concourse.bass as bass
import concourse.mybir as mybir
from concourse.bass_utils import run_bass_kernel_spmd

F32 = mybir.dt.float32
BF16 = mybir.dt.bfloat16
I32 = mybir.dt.int32
U32 = mybir.dt.uint32
U8 = mybir.dt.uint8
ALU = mybir.AluOpType
AF = mybir.ActivationFunctionType
AX = mybir.AxisListType
DSZ = {F32: 4, BF16: 2, I32: 4, U32: 4, U8: 1}


class Tok:
    __slots__ = ("name", "lw", "rd", "excl")

    def __init__(self, name, excl=False):
        self.name = name
        self.lw = None
        self.rd = []
        self.excl = excl


class _Op:
    __slots__ = ("eng", "fn", "deps", "idx", "dma", "sem", "val", "used", "slot_prev")

    def __init__(self, eng, fn, deps, idx, dma):
        self.eng = eng
        self.fn = fn
        self.deps = deps
        self.idx = idx
        self.dma = dma
        self.sem = None
        self.val = 0
        self.used = False
        self.slot_prev = None


class Prog:
    ENGS = ("pe", "act", "dve", "pool", "sp")
    NSLOT = 12
    SEG = 20000

    def __init__(self, nc):
        self.nc = nc
        self.ops = []
        self.by_eng = {e: [] for e in self.ENGS}
        self.pending_barrier = {e: [] for e in self.ENGS}
        self.recent_dma = {e: [] for e in self.ENGS}

    def add(self, eng, fn, rd=(), wr=(), dma=False):
        ex = [t for t in rd if t.excl]
        if ex:
            rd = [t for t in rd if not t.excl]
            wr = list(wr) + ex
        deps = set()
        for t in rd:
            if t.lw is not None:
                deps.add(t.lw)
        for t in wr:
            if t.lw is not None:
                deps.add(t.lw)
            deps.update(t.rd)
        if self.pending_barrier[eng]:
            deps.update(self.pending_barrier[eng])
            self.pending_barrier[eng] = []
        idx = len(self.ops)
        op = _Op(eng, fn, sorted(deps), idx, dma)
        self.ops.append(op)
        self.by_eng[eng].append(op)
        for t in rd:
            t.rd.append(idx)
        for t in wr:
            t.lw = idx
            t.rd = []
        if dma:
            r = self.recent_dma[eng]
            r.append(idx)
            if len(r) > self.NSLOT:
                r.pop(0)
        return idx

    def barrier(self):
        last = []
        for e in self.ENGS:
            ops = self.by_eng[e]
            for o in reversed(ops):
                if not o.dma:
                    last.append(o.idx)
                    break
            last.extend(self.recent_dma[e])
        for e in self.ENGS:
            self.pending_barrier[e] = list(set(self.pending_barrier[e]) | set(last))

    def emit(self, final_wait_eng="sp"):
        nc = self.nc
        ops = self.ops
        self.barrier()
        fin = self.pending_barrier[final_wait_eng]
        for o in ops:
            for d in o.deps:
                ops[d].used = True
        for d in fin:
            ops[d].used = True
        nsem = 0
        plan = {}
        for e in self.ENGS:
            ncomp = sum(1 for o in self.by_eng[e] if (not o.dma) and o.used)
            nseg = (ncomp + self.SEG - 1) // self.SEG
            ndma = self.NSLOT if any(o.dma for o in self.by_eng[e]) else 0
            plan[e] = (nseg, ndma)
            nsem += nseg + ndma
        import contextlib

        with contextlib.ExitStack() as st:
            sems = [st.enter_context(nc.semaphore(f"s{i}")) for i in range(nsem)]
            si = 0
            for e in self.ENGS:
                nseg, ndma = plan[e]
                seg = sems[si:si + nseg]
                si += nseg
                slots = sems[si:si + ndma]
                si += ndma
                slot_cnt = [0] * ndma
                k = 0
                c = 0
                for o in self.by_eng[e]:
                    if o.dma:
                        s = k % ndma
                        k += 1
                        o.slot_prev = (slots[s], slot_cnt[s]) if slot_cnt[s] else None
                        slot_cnt[s] += 16
                        o.sem, o.val = slots[s], slot_cnt[s]
                    elif o.used:
                        o.sem = seg[c // self.SEG]
                        o.val = (c % self.SEG) + 1
                        c += 1
            block = st.enter_context(nc.Block())

            def run(eng_name):
                def body(e):
                    waited = {}

                    def wait(sem, val):
                        key = id(sem)
                        if waited.get(key, 0) >= val:
                            return
                        waited[key] = val
                        e.wait_ge(sem, val)

                    for o in self.by_eng[eng_name]:
                        for d in o.deps:
                            p = ops[d]
                            if p.eng == "pe" and eng_name == "pe" and not p.dma:
                                continue
                            wait(p.sem, p.val)
                        if o.slot_prev is not None:
                            wait(*o.slot_prev)
                        ins = o.fn(e)
                        if o.dma:
                            ins.then_inc(o.sem, 16)
                        elif o.used:
                            ins.then_inc(o.sem, 1)
                    if eng_name == final_wait_eng:
                        for d in fin:
                            wait(ops[d].sem, ops[d].val)
                return body

            block.tensor(run("pe"))
            block.scalar(run("act"))
            block.vector(run("dve"))
            block.gpsimd(run("pool"))
            block.sync(run("sp"))


class Arena:
    def __init__(self, nc, st, nbytes, name="arena"):
        self.t = st.enter_context(nc.sbuf_tensor(name, [128, nbytes], U8))
        self.nbytes = nbytes
        self.off = 0
        self.marks = []

    def alloc(self, shape, dtype, name=None):
        assert shape[0] <= 128
        free = int(np.prod(shape[1:]))
        nb = free * DSZ[dtype]
        nb_al = (nb + 63) // 64 * 64
        assert self.off + nb_al <= self.nbytes, (self.off, nb_al, self.nbytes, name)
        ap = self.t[0:shape[0], self.off:self.off + nb]
        self.off += nb_al
        if dtype != U8:
            ap = ap.bitcast(dtype)
        if len(shape) > 2:
            names = " ".join(f"d{i}" for i in range(1, len(shape)))
            kw = {f"d{i}": shape[i] for i in range(1, len(shape))}
            ap = ap.rearrange(f"p ({names}) -> p {names}", **kw)
        return ap

    def mark(self):
        self.marks.append(self.off)

    def release(self):
        self.off = self.marks.pop()

import contextlib

D = 1024
KC = 8
NE = 32
EPS = 1e-6
BLK = 512
NIT = 14
BIGC = float(2 ** 20)


class K:
    def __init__(self, P):
        self.P = P

    def mm(self, out, lhsT, rhs, start, stop, rd, wr):
        self.P.add("pe", lambda e: e.matmul(out, lhsT=lhsT, rhs=rhs, start=start, stop=stop,
                                            skip_group_check=True), rd, wr)

    def tr(self, out, in_, ident, rd, wr):
        self.P.add("pe", lambda e: e.transpose(out=out, in_=in_, identity=ident), rd, wr)

    def act(self, out, in_, func, rd, wr, scale=1.0, bias=0.0, accum=None, eng="act"):
        if accum is None:
            self.P.add(eng, lambda e: e.activation(out=out, in_=in_, func=func, scale=scale, bias=bias), rd, wr)
        else:
            self.P.add(eng, lambda e: e.activation(out=out, in_=in_, func=func, scale=scale, bias=bias,
                                                   accum_out=accum), rd, wr)

    def ts(self, eng, out, in0, s1, s2, op0, op1, rd, wr, accum=None):
        if op1 is None:
            self.P.add(eng, lambda e: e.tensor_scalar(out=out, in0=in0, scalar1=s1, scalar2=None, op0=op0), rd, wr)
        elif accum is None:
            self.P.add(eng, lambda e: e.tensor_scalar(out=out, in0=in0, scalar1=s1, scalar2=s2, op0=op0, op1=op1), rd, wr)
        else:
            self.P.add(eng, lambda e: e.tensor_scalar(out=out, in0=in0, scalar1=s1, scalar2=s2, op0=op0, op1=op1,
                                                      accum_out=accum), rd, wr)

    def tt(self, eng, out, in0, in1, op, rd, wr):
        self.P.add(eng, lambda e: e.tensor_tensor(out=out, in0=in0, in1=in1, op=op), rd, wr)

    def stt(self, out, in0, scalar, in1, op0, op1, rd, wr, accum=None):
        if accum is None:
            self.P.add("dve", lambda e: e.scalar_tensor_tensor(out=out, in0=in0, scalar=scalar, in1=in1,
                                                              op0=op0, op1=op1), rd, wr)
        else:
            self.P.add("dve", lambda e: e.scalar_tensor_tensor(out=out, in0=in0, scalar=scalar, in1=in1,
                                                              op0=op0, op1=op1, accum_out=accum), rd, wr)

    def cp(self, eng, out, in_, rd, wr):
        if eng == "act":
            self.P.add("act", lambda e: e.activation(out=out, in_=in_, func=AF.Copy), rd, wr)
        else:
            self.P.add(eng, lambda e: e.tensor_copy(out=out, in_=in_), rd, wr)

    def memset(self, eng, ap, val, wr):
        self.P.add(eng, lambda e: e.memset(ap, val), (), wr)

    def dma(self, q, out, in_, rd, wr):
        self.P.add(q, lambda e: e.dma_start(out=out, in_=in_), rd, wr, dma=True)

    def gather(self, out, in_, idx, rd, wr, bounds=None):
        if bounds is None:
            self.P.add("pool", lambda e: e.indirect_dma_start(
                out=out, out_offset=None, in_=in_,
                in_offset=bass.IndirectOffsetOnAxis(ap=idx, axis=0)), rd, wr, dma=True)
        else:
            self.P.add("pool", lambda e: e.indirect_dma_start(
                out=out, out_offset=None, in_=in_,
                in_offset=bass.IndirectOffsetOnAxis(ap=idx, axis=0),
                bounds_check=bounds, oob_is_err=False), rd, wr, dma=True)

    def scatter(self, out, in_, idx, rd, wr):
        self.P.add("pool", lambda e: e.indirect_dma_start(
            out=out, out_offset=bass.IndirectOffsetOnAxis(ap=idx, axis=0),
            in_=in_, in_offset=None), rd, wr, dma=True)

    def recip(self, out, in_, rd, wr):
        self.P.add("dve", lambda e: e.reciprocal(out=out, in_=in_), rd, wr)

    def iota(self, out, pattern, base, cm, wr):
        self.P.add("pool", lambda e: e.iota(out, pattern=pattern, base=base, channel_multiplier=cm,
                                            allow_small_or_imprecise_dtypes=True), (), wr)


def build_nc(S, stages=5, debug=False):
    NT = S // 128
    NG = S // 512
    NSEL = min(256, S // 4)
    NSLOT = 4 * S + NE * BLK
    NBLK = NSLOT // BLK
    JMAX = S // BLK

    nc = bass.Bass("TRN2", target_bir_lowering=False)

    def din(name, shape, dt=F32):
        return nc.dram_tensor(name, list(shape), dt, kind="ExternalInput").ap()

    def dscr(name, shape, dt):
        kind = "ExternalOutput" if debug else "Internal"
        return nc.dram_tensor(name, list(shape), dt, kind=kind).ap()

    x_d = din("x", [S, D])
    c_d = din("c_fm", [128, KC])
    w_ada_d = din("w_ada", [D, 6 * D])
    b_ada_d = din("b_ada_fm", [128, 48])
    n1g_d = din("n1g_fm", [128, KC])
    n2g_d = din("n2g_fm", [128, KC])
    w_in_d = din("w_in", [D, 3144])
    convw_d = din("convw_fm", [128, 16])
    vec4_d = din("vec4_fm", [128, 28])
    wabd_d = din("wabd", [128, 4 * 128])
    wxbd_d = din("wxbd", [128, 4 * 128])
    w_out_d = din("w_out", [D, D])
    w_rt_d = din("w_rt_fm", [128, KC * NE])
    b_rt_d = din("b_rt", [1, NE])
    if stages >= 4:
        w1_d = din("w1", [NE, D, 2 * D])
        b1_d = din("b1", [NE, 2 * D])
        w2_d = din("w2", [NE, D, D])
        b2_d = din("b2", [NE, D])
    import os as _os
    CUT = int(_os.environ.get("P1CUT", "99"))
    out_d = nc.dram_tensor("out", [S, D], F32, kind="ExternalOutput").ap()

    MODROW = dscr("modrow", [64, 128], F32)
    QT = dscr("qt", [4, 128, S], BF16)
    KV = dscr("kv", [NT, 128, 1032], BF16)
    QIT = dscr("qit", [4, 128, S], BF16)
    KIT = dscr("kit", [128, S], BF16)
    WI = dscr("wi", [NT, 128, 8], F32)
    YR = dscr("yr", [4, 128, S], BF16)
    YA = dscr("ya", [4, 128, S], BF16)
    X1 = dscr("x1", [S, D], F32)
    H2 = dscr("h2", [S, D], BF16)
    HG = dscr("hg", [NSLOT, D], BF16)
    YY = dscr("yy", [NSLOT, D], F32)
    RTD = dscr("rtd", [128, NT * 8], F32)

    P = Prog(nc)
    k = K(P)
    T = Tok
    st = contextlib.ExitStack()
    with st:
        A = Arena(nc, st, 206 * 1024)
        psT = [st.enter_context(nc.psum_tensor(f"ps{i}", [128, 1024], F32)) for i in range(4)]
        PSH = [[psT[i][:, 0:512], psT[i][:, 512:1024]] for i in range(4)]
        t_psh = [[Tok(f"ps{i}a", True), Tok(f"ps{i}b", True)] for i in range(4)]
        t_dram = {n_: T(n_) for n_ in ("QT", "KV", "QIT", "KIT", "WI", "YR", "YA", "X1", "H2", "HG", "YY",
                                      "MODROW", "RTD", "OUT")}

        io = A.alloc([128, 128], F32); t_io = T("io")
        identf = A.alloc([128, 128], F32); t_idf = T("identf")
        ident = A.alloc([128, 128], BF16); t_id = T("ident")
        ones_b = A.alloc([128, 512], BF16); t_ones = T("ones")
        bo64 = A.alloc([128, 128], BF16); t_bo = T("bo64")
        ustr = A.alloc([128, 128], BF16); t_ustr = T("ustr")
        modfm = A.alloc([128, 64], F32); t_mod = T("modfm")
        vec4 = A.alloc([128, 28], F32); t_vec4 = T("vec4")
        convw = A.alloc([128, 16], F32); t_convw = T("convw")
        nsp = A.alloc([128, 4], F32); t_nsp = T("nsp")
        pidx = A.alloc([128, 1], F32); t_pidx = T("pidx")
        dest4 = A.alloc([128, NT, 4], I32); t_dest4 = T("dest4")
        gate4 = A.alloc([128, NT, 4], F32); t_gate4 = T("gate4")
        widx = A.alloc([128, NBLK, 8], I32); t_widx = T("widx")
        bidx = A.alloc([128, NBLK], I32); t_bidx = T("bidx")

        k.iota(io[:], [[1, 128]], 0, -1, [t_io])
        k.ts("dve", identf[:], io[:], 0.0, None, ALU.is_equal, None, [t_io], [t_idf])
        k.cp("dve", ident[:], identf[:], [t_idf], [t_id])
        k.ts("dve", ustr[:], io[:], 0.0, None, ALU.is_gt, None, [t_io], [t_ustr])
        k.iota(pidx[:], [[0, 1]], 0, 1, [t_pidx])
        k.memset("pool", ones_b[:], 1.0, [t_ones])
        k.memset("pool", bo64[:], 0.0, [t_bo])
        k.memset("pool", bo64[0:64, 0:64], 1.0, [t_bo])
        k.memset("pool", bo64[64:128, 64:128], 1.0, [t_bo])
        k.dma("sp", vec4[:], vec4_d[:, :], [], [t_vec4])
        k.dma("sp", convw[:], convw_d[:, :], [], [t_convw])
        CB, BA, BX, LAM, RGG, AOG = 0, 4, 8, 12, 16, 20
        QG, KG, KIG = 24, 25, 26

        A.mark()
        csb = A.alloc([128, KC], F32); t_c = T("c")
        scs = A.alloc([128, KC], F32); t_scs = T("scs")
        bada = A.alloc([128, 48], F32); t_bada = T("bada")
        n1g = A.alloc([128, KC], F32); t_n1g = T("n1g")
        n2g = A.alloc([128, KC], F32); t_n2g = T("n2g")
        wa_buf = [A.alloc([128, KC, 768], F32) for _ in range(2)]
        t_wa = [T("wa0"), T("wa1")]
        modT = A.alloc([64, 128], F32); t_modT = T("modT")
        tmp4 = A.alloc([128, 4], F32); t_tmp4 = T("tmp4")
        k.dma("sp", csb[:], c_d[:, :], [], [t_c])
        k.dma("sp", bada[:], b_ada_d[:, :], [], [t_bada])
        k.dma("sp", n1g[:], n1g_d[:, :], [], [t_n1g])
        k.dma("sp", n2g[:], n2g_d[:, :], [], [t_n2g])
        k.act(scs[:], csb[:], AF.Silu, [t_c], [t_scs])
        pmod = PSH[0][0]
        for jg in range(8):
            wb_ = wa_buf[jg % 2]
            k.dma("sp", wb_[:], w_ada_d[:, jg * 768:(jg + 1) * 768].rearrange("(kc p) n -> p kc n", p=128),
                  [], [t_wa[jg % 2]])
            for jj in range(6):
                j = jg * 6 + jj
                for kc in range(KC):
                    k.mm(pmod[:, j:j + 1], wb_[:, kc, jj * 128:(jj + 1) * 128], scs[:, kc:kc + 1],
                         kc == 0, kc == KC - 1, [t_wa[jg % 2], t_scs], [t_psh[0][0]])
        k.tt("dve", modfm[:, 0:48], pmod[:, 0:48], bada[:], ALU.add, [t_psh[0][0], t_bada], [t_mod])
        k.stt(modfm[:, 48:56], modfm[:, 8:16], 1.0, n1g[:], ALU.add, ALU.mult, [t_mod, t_n1g], [t_mod])
        k.stt(modfm[:, 56:64], modfm[:, 32:40], 1.0, n2g[:], ALU.add, ALU.mult, [t_mod, t_n2g], [t_mod])
        pTm = PSH[0][1]
        k.tr(pTm[0:64, 0:128], modfm[:, 0:64], identf[:], [t_mod, t_idf], [t_psh[0][1]])
        k.cp("dve", modT[:], pTm[0:64, 0:128], [t_psh[0][1]], [t_modT])
        k.dma("sp", MODROW[:, :], modT[:], [t_modT], [])
        mr = MODROW.rearrange("j p -> (j p)")

        def bc_row(dst, j0, tok):
            src = mr[j0 * 128:(j0 + 8) * 128].rearrange("(o n) -> o n", o=1).partition_broadcast(128)
            k.dma("sp", dst[:], src, [t_dram["MODROW"]], [tok])

        k.act(tmp4[:], vec4[:, LAM:LAM + 4], AF.Exp, [t_vec4], [t_tmp4], scale=-1.0)
        k.act(nsp[:], tmp4[:], AF.Ln, [t_tmp4], [t_nsp], bias=1.0)
        k.ts("dve", nsp[:], nsp[:], -8.0, None, ALU.mult, None, [t_nsp], [t_nsp])
        P.barrier()
        A.release()

        if stages == 0:
            A.mark()
            zt2 = A.alloc([128, D], F32); t_zt2 = T("zt2")
            k.memset("pool", zt2[:], 0.0, [t_zt2])
            k.dma("sp", out_d[0:128, :], zt2[:], [t_zt2], [])
            P.emit()
            return nc
        A.mark()
        cv_ld = [A.alloc([128, 2048], F32) for _ in range(3)]
        t_cvld = [T(f"cvld{i}") for i in range(3)]

        A.mark()
        w_in_sb = A.alloc([128, KC, 3208], BF16); t_win = T("w_in")
        wabd = A.alloc([128, 4, 128], BF16); t_wabd = T("wabd")
        wxbd = A.alloc([128, 4, 128], BF16); t_wxbd = T("wxbd")
        n = 0
        for kc in range(KC):
            sg = cv_ld[n % 3]; tg = t_cvld[n % 3]
            k.dma("sp", sg[:, 0:2048], w_in_d[kc * 128:(kc + 1) * 128, 0:2048], [], [tg])
            k.cp(["dve", "act", "pool"][n % 3], w_in_sb[:, kc, 0:2048], sg[:, 0:2048], [tg], [t_win])
            n += 1
            sg = cv_ld[n % 3]; tg = t_cvld[n % 3]
            k.dma("sp", sg[:, 0:1096], w_in_d[kc * 128:(kc + 1) * 128, 2048:3144], [], [tg])
            k.cp(["dve", "act", "pool"][n % 3], w_in_sb[:, kc, 2048:3136], sg[:, 0:1088], [tg], [t_win])
            k.cp("pool", w_in_sb[:, kc, 3136:3200], sg[:, 1024:1088], [tg], [t_win])
            k.cp("pool", w_in_sb[:, kc, 3200:3208], sg[:, 1088:1096], [tg], [t_win])
            n += 1
        for (dst, src, tk) in ((wabd, wabd_d, t_wabd), (wxbd, wxbd_d, t_wxbd)):
            sg = cv_ld[n % 3]; tg = t_cvld[n % 3]
            k.dma("sp", sg[:, 0:512], src[:, :], [], [tg])
            k.cp("dve", dst[:].rearrange("p c m -> p (c m)"), sg[:, 0:512], [tg], [tk])
            n += 1

        xt = [A.alloc([128, D], F32) for _ in range(2)]; t_xt = [T("xt0"), T("xt1")]
        xn = [A.alloc([128, D], BF16) for _ in range(2)]; t_xn = [T("xn0"), T("xn1")]
        junkb = A.alloc([128, D], BF16); t_junkb = T("junkb")
        st1 = A.alloc([128, 8], F32); t_st1 = T("st1")
        hT = [A.alloc([128, KC, 512], BF16) for _ in range(2)]; t_hT = [T("hT0"), T("hT1")]
        vsb = [A.alloc([128, 8, 65], BF16) for _ in range(2)]; t_vsb = [T("vsb0"), T("vsb1")]
        wisb = [A.alloc([128, 8], F32) for _ in range(2)]; t_wisb = [T("wisb0"), T("wisb1")]
        xr_ext = A.alloc([128, 4, 516], F32); t_xre = [T(f"xre{c}") for c in range(4)]
        carry = A.alloc([128, 4], F32); t_carry = T("carry")
        xtail = A.alloc([128, 4, 4], F32); t_xtail = T("xtail")
        NW = 8
        wf = [A.alloc([128, 512], F32) for _ in range(NW)]; t_wf = [T(f"wf{i}") for i in range(NW)]
        NWB = 6
        wb = [A.alloc([128, 512], BF16) for _ in range(NWB)]; t_wb = [T(f"wb{i}") for i in range(NWB)]
        yv = A.alloc([128, 4, 512], F32); t_yv = [T(f"yv{i}") for i in range(4)]
        xc_all = A.alloc([128, 4, 512], F32); t_xc = [T(f"xc{i}") for i in range(4)]
        xcb_all = A.alloc([128, 4, 512], BF16); t_xcb = [T(f"xcb{i}") for i in range(4)]
        ysq = A.alloc([128, 4, 512], BF16); t_ysq = [T(f"ysq{i}") for i in range(4)]
        for b in range(2):
            k.memset("pool", vsb[b][:], 1.0, [t_vsb[b]])
        k.memset("pool", xr_ext[:], 0.0, t_xre)
        k.memset("pool", carry[:], 0.0, [t_carry])
        wfi = [0]; wbi = [0]; zi = [0]

        def new_wf():
            i = wfi[0] % NW; wfi[0] += 1
            return wf[i], t_wf[i]

        def new_wb():
            i = wbi[0] % NWB; wbi[0] += 1
            return wb[i], t_wb[i]

        zbufs = [(PSH[1][0], t_psh[1][0]), (PSH[1][1], t_psh[1][1]), (PSH[2][0], t_psh[2][0])]

        def new_z():
            i = zi[0] % 3; zi[0] += 1
            return zbufs[i]

        ps_wi, t_pswi = PSH[2][1], t_psh[2][1]
        ps_ss, t_psss = PSH[3][0], t_psh[3][0]
        ps_v, t_psv = PSH[3][1], t_psh[3][1]
        gm1 = modfm[:, 48:56]
        sh1 = modfm[:, 0:8]

        for G in range(NG):
            h_T = hT[G % 2]; th = t_hT[G % 2]
            for j in range(4):
                i = 4 * G + j
                x_t = xt[i % 2]; tx = t_xt[i % 2]
                x_n = xn[i % 2]; txn = t_xn[i % 2]
                k.dma("sp", x_t[:], x_d[i * 128:(i + 1) * 128, :], [], [tx])
                k.act(junkb[:], x_t[:], AF.Square, [tx], [t_junkb, t_st1], accum=st1[:, 0:1])
                k.act(st1[:, 1:2], st1[:, 0:1], AF.Sqrt, [t_st1], [t_st1], scale=1.0 / D, bias=EPS)
                k.recip(st1[:, 2:3], st1[:, 1:2], [t_st1], [t_st1])
                k.act(x_n[:], x_t[:], AF.Copy, [tx, t_st1], [txn], scale=st1[:, 2:3])
                pt = psT[0][:, (i % 2) * 512:(i % 2 + 1) * 512].bitcast(BF16)
                tpt = t_psh[0][i % 2]
                for kc in range(KC):
                    k.tr(pt[:, kc * 128:(kc + 1) * 128], x_n[:, kc * 128:(kc + 1) * 128], ident[:],
                         [txn, t_id], [tpt])
                for kc in range(KC):
                    dst = h_T[:, kc, j * 128:(j + 1) * 128]
                    src = pt[:, kc * 128:(kc + 1) * 128]
                    if kc % 2 == 0:
                        k.act(dst, src, AF.Identity, [tpt, t_mod], [th], scale=gm1[:, kc:kc + 1],
                              bias=sh1[:, kc:kc + 1])
                    else:
                        k.ts("dve", dst, src, gm1[:, kc:kc + 1], sh1[:, kc:kc + 1], ALU.mult, ALU.add,
                             [tpt, t_mod], [th])
                if CUT < 2:
                    continue
                for kc in range(KC):
                    k.mm(ps_v[:, 0:512], h_T[:, kc, j * 128:(j + 1) * 128], w_in_sb[:, kc, 2048:2560],
                         kc == 0, kc == KC - 1, [th, t_win], [t_psv])
                for kc in range(KC):
                    k.mm(ps_wi[:, 0:8], h_T[:, kc, j * 128:(j + 1) * 128], w_in_sb[:, kc, 3200:3208],
                         kc == 0, kc == KC - 1, [th, t_win], [t_pswi])
                vb = vsb[i % 2]; tvb = t_vsb[i % 2]
                k.cp("act", vb[:, :, 0:64], ps_v[:, 0:512].rearrange("p (h d) -> p h d", d=64), [t_psv], [tvb])
                k.dma("pool", KV[i, :, 512:1032], vb[:].rearrange("p h d -> p (h d)"), [tvb], [])
                wbt = wisb[i % 2]; twb = t_wisb[i % 2]
                k.cp("dve", wbt[:], ps_wi[:, 0:8], [t_pswi], [twb])
                k.dma("pool", WI[i], wbt[:], [twb], [])

            if CUT < 3:
                continue
            cols = slice(G * 512, (G + 1) * 512)

            def zchunk(col0, M=128):
                pz, tz = new_z()
                for kc in range(KC):
                    k.mm(pz[0:M, :], w_in_sb[:, kc, col0:col0 + M], h_T[:, kc, :], kc == 0, kc == KC - 1,
                         [th, t_win], [tz])
                return pz, tz

            rnn_state = []
            for c in range(4):
                pz, tz = zchunk(c * 128)
                if G > 0:
                    k.cp("pool", xr_ext[:, c, 0:3], xtail[:, c, 0:3], [t_xtail], [t_xre[c]])
                k.cp("act", xr_ext[:, c, 3:515], pz[:, :], [tz], [t_xre[c]])
                t0, tt0 = xc_all[:, c, :], t_xc[c]
                txre = t_xre[c]
                k.ts("dve", t0[:], xr_ext[:, c, 0:512], convw[:, c * 4:c * 4 + 1], vec4[:, CB + c:CB + c + 1],
                     ALU.mult, ALU.add, [txre, t_convw, t_vec4], [tt0])
                for tap in (1, 2, 3):
                    k.stt(t0[:], xr_ext[:, c, tap:tap + 512], convw[:, c * 4 + tap:c * 4 + tap + 1], t0[:],
                          ALU.mult, ALU.add, [txre, t_convw, tt0], [tt0])
                k.cp("pool", xtail[:, c, 0:3], xr_ext[:, c, 512:515], [txre], [t_xtail])
                xcb, txcb = xcb_all[:, c, :], t_xcb[c]
                k.cp("pool", xcb, t0, [tt0], [txcb])
                rnn_state.append((t0, tt0, xcb, txcb))
            if CUT < 4:
                continue
            for c in range(4):
                pz, tz = zchunk(512 + c * 128)
                k.act(yv[:, c, :], pz[:, :], AF.Gelu_apprx_tanh, [tz], [t_yv[c]])
            if CUT < 5:
                continue
            for (base_col, gcol, dst, tkd) in ((1024, QG, QT, t_dram["QT"]), (1536, KG, None, t_dram["KV"])):
                for c in range(4):
                    pz, tz = zchunk(base_col + c * 128)
                    sq, tsq = new_wb()
                    k.act(sq[:], pz[:, :], AF.Square, [tz], [tsq])
                    qf, tqf = new_wf()
                    k.ts("dve", qf[:], pz[:, :], vec4[:, gcol:gcol + 1], None, ALU.mult, None, [tz, t_vec4], [tqf])
                    k.mm(ps_ss[:, :], bo64[:], sq[:], True, True, [t_bo, tsq], [t_psss])
                    rs, trs = new_wf()
                    k.act(rs[:], ps_ss[:, :], AF.Sqrt, [t_psss], [trs], scale=1.0 / 64, bias=EPS)
                    k.recip(rs[:], rs[:], [trs], [trs])
                    qn, tqn = new_wb()
                    k.tt("dve", qn[:], qf[:], rs[:], ALU.mult, [tqf, trs], [tqn])
                    if dst is not None:
                        k.dma("pool", dst[c, :, cols], qn[:], [tqn], [tkd])
                    else:
                        k.dma("pool", KV[4 * G:4 * G + 4, :, c * 128:(c + 1) * 128].rearrange("t p s -> p t s"),
                              qn[:].rearrange("p (t s) -> p t s", t=4), [tqn], [tkd])
            if CUT < 6:
                continue
            for c in range(4 * int(_os.environ.get("DUP", "1"))):
                c = c % 4
                pz, tz = zchunk(2560 + c * 128)
                qn, tqn = new_wb()
                k.cp("act", qn[:], pz[:, :], [tz], [tqn])
                k.dma("pool", QIT[c, :, cols], qn[:], [tqn], [])
            for _d in range(int(_os.environ.get("DVEDUP", "0"))):
                qf, tqf = new_wf()
                k.ts(_os.environ.get("DUPENG", "dve"), qf[:], xc_all[:, 0, :], 2.0, None, ALU.mult, None, [t_xc[0]], [tqf])
            if CUT < 7:
                continue
            SK = set(_os.environ.get("SKIP", "").split(","))
            pz, tz = zchunk(int(_os.environ.get("KICOL", "3072")))
            sq, tsq = new_wb()
            if "a" not in SK:
                k.act(sq[:], pz[:, :], AF.Square, [tz], [tsq])
            qf, tqf = new_wf()
            if "b" not in SK:
                k.ts("dve", qf[:], pz[:, :], vec4[:, KIG:KIG + 1], None, ALU.mult, None, [tz, t_vec4], [tqf])
            if "c" not in SK:
                k.mm(ps_ss[:, :], bo64[:], sq[:], True, True, [t_bo, tsq], [t_psss])
            rs, trs = new_wf()
            if "d" not in SK:
                k.act(rs[:], ps_ss[:, :], AF.Sqrt, [t_psss], [trs], scale=1.0 / 64, bias=EPS)
            if "e" not in SK:
                k.recip(rs[:], rs[:], [trs], [trs])
            qn, tqn = new_wb()
            if "f" not in SK:
                k.tt("dve", qn[:], qf[:], rs[:], ALU.mult, [tqf, trs], [tqn])
            if "g" not in SK:
                k.dma("sp", KIT[:, cols], qn[:], [tqn], [])
            if CUT < 8:
                continue
            for c in range(4):
                xc, txc, xcb, txcb = rnn_state[c]
                pa, tpa = new_z()
                k.mm(pa[:, :], wabd[:, c, :], xcb[:], True, True, [t_wabd, txcb], [tpa])
                r, tr_ = new_wf()
                k.act(r[:], pa[:, :], AF.Sigmoid, [tpa, t_vec4], [tr_], bias=vec4[:, BA + c:BA + c + 1])
                px, tpx = new_z()
                k.mm(px[:, :], wxbd[:, c, :], xcb[:], True, True, [t_wxbd, txcb], [tpx])
                ig, tig = new_wf()
                k.act(ig[:], px[:, :], AF.Sigmoid, [tpx, t_vec4], [tig], bias=vec4[:, BX + c:BX + c + 1])
                a, ta = new_wf()
                k.act(a[:], r[:], AF.Exp, [tr_, t_nsp], [ta], scale=nsp[:, c:c + 1])
                k.tt("pool", r[:], a[:], a[:], ALU.mult, [ta], [tr_])
                k.act(r[:], r[:], AF.Sqrt, [tr_], [tr_], scale=-1.0, bias=1.0)
                k.tt("dve", ig[:], ig[:], r[:], ALU.mult, [tig, tr_], [tig])
                k.tt("dve", ig[:], ig[:], xc[:], ALU.mult, [tig, txc], [tig])
                hsc, thsc = new_wf()
                P.add("dve", (lambda o_, a_, u_, i_: (lambda e: e.tensor_tensor_scan(
                    out=o_, data0=a_, data1=u_, initial=i_, op0=ALU.mult, op1=ALU.add)))(
                        hsc[:], a[:], ig[:], carry[:, c:c + 1]), [ta, tig, t_carry], [thsc])
                k.cp("dve", carry[:, c:c + 1], hsc[:, 511:512], [thsc], [t_carry])
                k.tt("dve", yv[:, c, :], yv[:, c, :], hsc[:], ALU.mult, [t_yv[c], thsc], [t_yv[c]])
                k.act(ysq[:, c, :], yv[:, c, :], AF.Square, [t_yv[c]], [t_ysq[c]])
            if CUT < 9:
                continue
            for c in range(4):
                k.mm(ps_ss[:, :], ones_b[:, 0:128], ysq[:, c, :], c == 0, c == 3, [t_ones, t_ysq[c]], [t_psss])
            rs, trs = new_wf()
            k.act(rs[:], ps_ss[:, :], AF.Sqrt, [t_psss], [trs], scale=1.0 / 512, bias=EPS)
            k.recip(rs[:], rs[:], [trs], [trs])
            for c in range(4):
                yn, tyn = new_wb()
                k.stt(yn[:], yv[:, c, :], vec4[:, RGG + c:RGG + c + 1], rs[:], ALU.mult, ALU.mult,
                      [t_yv[c], t_vec4, trs], [tyn])
                k.dma("pool", YR[c, :, cols], yn[:], [tyn], [])
        P.barrier()
        A.release()
        A.release()

        if stages >= 2:
            A.mark()
            kiT = A.alloc([128, S], BF16); t_kiT = T("kiT")
            score = [A.alloc([128, S], F32) for _ in range(2)]; t_score = [T("score0"), T("score1")]
            negm = [A.alloc([128, S], BF16) for _ in range(3)]; t_negm = [T("negm0"), T("negm1"), T("negm2")]
            junk8 = A.alloc([128, S], U8)
            I4 = A.alloc([128, 512], BF16); t_I4 = T("I4")
            zb = A.alloc([128, 260], BF16); t_zb = T("zb")
            pow2 = A.alloc([128, NIT + 1], F32); t_pow2 = T("pow2")
            qT_t = [A.alloc([128, 4, 128], BF16) for _ in range(2)]; t_qT = [T("qT0"), T("qT1")]
            qiT_t = [A.alloc([128, 4, 128], BF16) for _ in range(3)]; t_qiT = [T("qiT0"), T("qiT1"), T("qiT2")]
            wi_t = [A.alloc([128, 8], F32) for _ in range(3)]; t_wi = [T("wi0"), T("wi1"), T("wi2")]
            NTR = 6
            trelu = [A.alloc([128, 512], F32) for _ in range(NTR)]; t_trelu = [T(f"trelu{i}") for i in range(NTR)]
            pm = [A.alloc([128, 1024], BF16) for _ in range(2)]; t_pm = [T("pm0"), T("pm1")]
            NKV = 4
            kv = [A.alloc([128, 1032], BF16) for _ in range(NKV)]; t_kv = [T(f"kv{i}") for i in range(NKV)]
            bis = [A.alloc([128, 8], F32) for _ in range(2)]; t_bis = [T("bis0"), T("bis1")]
            dall = [A.alloc([128, NIT + 1], F32) for _ in range(2)]; t_dall = [T("dall0"), T("dall1")]
            yaf = A.alloc([128, 8, 64], F32); t_yaf = T("yaf")
            yab = A.alloc([128, 512], BF16); t_yab = T("yab")
            yaT = A.alloc([128, 4, 128], BF16); t_yaT = T("yaT")
            fin = A.alloc([128, 16], F32); t_fin = T("fin")

            k.dma("sp", kiT[:, :], KIT[:, :], [t_dram["KIT"]], [t_kiT])
            for r4 in range(4):
                k.cp("pool", I4[:, r4 * 128:(r4 + 1) * 128], ident[:], [t_id], [t_I4])
            k.memset("pool", zb[:], 0.0, [t_zb])
            for it in range(NIT + 1):
                k.memset("pool", pow2[:, it:it + 1], float(2.0 ** (-it)), [t_pow2])

            ps_i = [PSH[3][0], PSH[3][1]]; t_psi = [t_psh[3][0], t_psh[3][1]]
            psl = [psT[0], psT[1]]
            pso = [PSH[2][0], PSH[2][1]]; t_pso = [t_psh[2][0], t_psh[2][1]]
            cnt_i = [0]; cnt_v = [0]

            def A_pieces(qt):
                L = (qt + 1) * 128
                b = qt % 3
                s2 = qt % 2
                sc_ = score[s2]; tsc = t_score[s2]
                bs = bis[s2]; tbs = t_bis[s2]
                dl = dall[s2]; tdl = t_dall[s2]
                pcs = []

                def ld():
                    k.dma("sp", qiT_t[b][:], QIT.rearrange("c p s -> p c s")[:, :, qt * 128:(qt + 1) * 128],
                          [t_dram["QIT"]], [t_qiT[b]])
                    k.dma("sp", wi_t[b][:], WI[qt], [t_dram["WI"]], [t_wi[b]])
                pcs.append(ld)
                items = [(g, min(512, L - g * 512), h) for g in range((L + 511) // 512) for h in range(8)]
                LAG = 3
                used_tr = {}

                def mk(idx):
                    def pc():
                        if idx < len(items):
                            g, n_, h = items[idx]
                            c = h // 2; base = (h % 2) * 64
                            ib = cnt_i[0] % 2; itr = cnt_i[0] % NTR; cnt_i[0] += 1
                            used_tr[idx] = itr
                            k.mm(ps_i[ib][:, 0:n_], qiT_t[b][base:base + 64, c, :],
                                 kiT[base:base + 64, g * 512:g * 512 + n_], True, True,
                                 [t_qiT[b], t_kiT], [t_psi[ib]])
                            k.act(trelu[itr][:, 0:n_], ps_i[ib][:, 0:n_], AF.Relu, [t_psi[ib]], [t_trelu[itr]])
                        j = idx - LAG
                        if j >= 0:
                            g, n_, h = items[j]
                            itr = used_tr[j]
                            sc = sc_[:, g * 512:g * 512 + n_]
                            if h == 0:
                                k.ts("dve", sc, trelu[itr][:, 0:n_], wi_t[b][:, 0:1], None, ALU.mult, None,
                                     [t_trelu[itr], t_wi[b]], [tsc])
                            else:
                                k.stt(sc, trelu[itr][:, 0:n_], wi_t[b][:, h:h + 1], sc, ALU.mult, ALU.add,
                                      [t_trelu[itr], t_wi[b], tsc], [tsc])
                    return pc
                for idx in range(len(items) + LAG):
                    pcs.append(mk(idx))

                def prep():
                    if L > NSEL:
                        P.add("dve", lambda e: e.tensor_reduce(out=bs[:, 0:1], in_=sc_[:, 0:L], axis=AX.X,
                                                               op=ALU.max, apply_absolute_value=True),
                              [tsc], [tbs])
                        k.ts("dve", dl[:], pow2[:], bs[:, 0:1], None, ALU.mult, None, [t_pow2, tbs], [tdl])
                        k.memset("dve", bs[:, 1:2], 0.0, [tbs])
                        k.memset("dve", bs[:, 5:6], 0.0, [tbs])
                    else:
                        k.memset("dve", bs[:, 4:5], -1e29, [tbs])
                    k.memset("dve", sc_[0:64, L - 64:L], -1e30, [tsc])
                pcs.append(prep)
                nsplit = len(pcs)
                use_act = (qt % 2 == 1)
                if L > NSEL:
                    for it in range(NIT):
                        def pc(it=it):
                            if not use_act:
                                k.ts("dve", junk8[:, 0:L], sc_[:, 0:L], bs[:, 1:2], None, ALU.is_gt, ALU.add,
                                     [tsc, tbs], [tbs], accum=bs[:, 2:3])
                                k.ts("dve", bs[:, 3:4], bs[:, 2:3], float(NSEL), -0.5, ALU.is_ge, ALU.add,
                                     [tbs], [tbs])
                            else:
                                k.act(negm[b][:, 0:L], sc_[:, 0:L], AF.Sign, [tsc, tbs], [t_negm[b], tbs],
                                      bias=bs[:, 5:6], accum=bs[:, 2:3])
                                k.ts("dve", bs[:, 3:4], bs[:, 2:3], float(2 * NSEL - L), -0.5, ALU.is_ge, ALU.add,
                                     [tbs], [tbs])
                            k.stt(bs[:, 1:2], bs[:, 3:4], dl[:, it:it + 1], bs[:, 1:2], ALU.mult, ALU.add,
                                  [tbs, tdl], [tbs])
                            if use_act:
                                k.ts("dve", bs[:, 5:6], bs[:, 1:2], -1.0, None, ALU.mult, None, [tbs], [tbs])
                        pcs.append(pc)
                    nsplit += NIT // 4

                def fin_():
                    if L > NSEL:
                        k.tt("dve", bs[:, 4:5], bs[:, 1:2], dl[:, NIT:NIT + 1], ALU.subtract,
                             [tbs, tdl], [tbs])
                    k.ts("dve", negm[b][:, 0:L], sc_[:, 0:L], bs[:, 4:5], -30000.0, ALU.is_le, ALU.mult,
                         [tsc, tbs], [t_negm[b]])
                pcs.append(fin_)
                return pcs[:nsplit], pcs[nsplit:]

            def ld_q(qt):
                b = qt % 2
                k.dma("sp", qT_t[b][:], QT.rearrange("c p s -> p c s")[:, :, qt * 128:(qt + 1) * 128],
                      [t_dram["QT"]], [t_qT[b]])

            def B_pieces(qt):
                b = qt % 2
                nb3 = qt % 3
                pcs = []

                def init():
                    if qt + 1 < NT:
                        ld_q(qt + 1)
                    for hb in range(2):
                        k.mm(pso[hb][:, 0:260], zb[:, 0:128], zb[:, 0:260], True, False, [t_zb], [t_pso[hb]])
                pcs.append(init)
                st_ = {}

                def mkb(kt):
                    def pc():
                        if kt <= qt:
                            iv = cnt_v[0] % NKV; lb = cnt_v[0] % 2; cnt_v[0] += 1
                            st_[kt] = (iv, lb)
                            k.dma("sp", kv[iv][:], KV[kt], [t_dram["KV"]], [t_kv[iv]])
                            tl = t_psh[lb][0]
                            for half in range(2):
                                k.mm(psl[lb][:, half * 512:(half + 1) * 512], negm[nb3][:, kt * 128:(kt + 1) * 128],
                                     I4[:, :], True, False, [t_negm[nb3], t_I4], [tl])
                            for h in range(8):
                                c = h // 2; base = (h % 2) * 64
                                j = (h % 2) * 4 + h // 2
                                k.mm(psl[lb][:, j * 128:(j + 1) * 128], kv[iv][base:base + 64, c * 128:(c + 1) * 128],
                                     qT_t[b][base:base + 64, c, :], False, (h >= 6), [t_kv[iv], t_qT[b]], [tl])
                            k.act(pm[lb][:], psl[lb][:, :], AF.Exp, [tl], [t_pm[lb]], scale=0.125)
                        kp = kt - 1
                        if kp >= 0:
                            iv, lb = st_[kp]
                            for h in range(8):
                                hb = h // 4; o = (h % 4) * 65
                                j = (h % 2) * 4 + h // 2
                                k.mm(pso[hb][:, o:o + 65], pm[lb][:, j * 128:(j + 1) * 128],
                                     kv[iv][:, 512 + h * 65:512 + (h + 1) * 65], False, kp == qt,
                                     [t_pm[lb], t_kv[iv]], [t_pso[hb]])
                    return pc
                for kt in range(qt + 2):
                    pcs.append(mkb(kt))
                return pcs

            def finalize(qt):
                cols = slice(qt * 128, (qt + 1) * 128)
                yf = yaf[:].rearrange("p h d -> p (h d)")
                ptb = PSH[3][0].bitcast(BF16)
                pcs = []

                def mk_a(hb):
                    def pc():
                        v3 = pso[hb][:, 0:260].rearrange("p (h e) -> p h e", e=65)
                        k.recip(fin[:, hb * 4:(hb + 1) * 4], v3[:, :, 64], [t_pso[hb]], [t_fin])
                        k.tt("dve", yaf[:, hb * 4:(hb + 1) * 4, :], v3[:, :, 0:64],
                             fin[:, hb * 4:(hb + 1) * 4].unsqueeze(2).to_broadcast([128, 4, 64]), ALU.mult,
                             [t_pso[hb], t_fin], [t_yaf])
                    return pc
                pcs.append(mk_a(0)); pcs.append(mk_a(1))
                pcs.append(lambda: k.act(yab[:], yf, AF.Square, [t_yaf], [t_yab, t_fin], accum=fin[:, 8:9]))
                pcs.append(lambda: k.act(fin[:, 9:10], fin[:, 8:9], AF.Sqrt, [t_fin], [t_fin], scale=1.0 / 512, bias=EPS))
                pcs.append(lambda: k.recip(fin[:, 10:11], fin[:, 9:10], [t_fin], [t_fin]))
                pcs.append(lambda: k.act(yab[:], yf, AF.Copy, [t_yaf, t_fin], [t_yab], scale=fin[:, 10:11]))

                def p_tr():
                    for c in range(4):
                        k.tr(ptb[:, c * 128:(c + 1) * 128], yab[:, c * 128:(c + 1) * 128], ident[:],
                             [t_yab, t_id], [t_psh[3][0]])

                def p_ev():
                    p_tr()
                    for c in range(4):
                        k.act(yaT[:, c, :], ptb[:, c * 128:(c + 1) * 128], AF.Copy, [t_psh[3][0], t_vec4], [t_yaT],
                              scale=vec4[:, AOG + c:AOG + c + 1])
                    k.dma("pool", YA.rearrange("c p s -> p c s")[:, :, cols], yaT[:], [t_yaT], [])
                pcs.append(p_ev)
                return pcs

            def run_pieces(lists):
                tot = max(len(l_) for l_ in lists)
                idx = [0] * len(lists)
                for step in range(tot):
                    for li, l_ in enumerate(lists):
                        tgt = (step + 1) * len(l_) // tot
                        while idx[li] < tgt:
                            l_[idx[li]]()
                            idx[li] += 1

            ld_q(0)
            AP_ = {}

            def get_A(q):
                if q not in AP_:
                    AP_[q] = A_pieces(q)
                return AP_[q]

            run_pieces([get_A(0)[0]])
            lists0 = [get_A(0)[1]]
            if NT > 1:
                lists0.append(get_A(1)[0])
            run_pieces(lists0)
            for qt in range(NT):
                lists = [B_pieces(qt)]
                if qt + 1 < NT:
                    lists.append(get_A(qt + 1)[1])
                if qt + 2 < NT:
                    lists.append(get_A(qt + 2)[0])
                if qt > 0:
                    lists.append(fin_tail)
                run_pieces(lists)
                fp_ = finalize(qt)
                run_pieces([fp_[:2]])
                fin_tail = fp_[2:]
                AP_.pop(qt, None)
            run_pieces([fin_tail])
            P.barrier()
            A.release()

        if stages >= 3:
            A.mark()
            maskd = A.alloc([128, NT, NE], F32); t_maskd = T("maskd")
            gated = A.alloc([128, NT, NE], F32); t_gated = T("gated")
            rankd = A.alloc([128, NT, NE], F32); t_rankd = T("rankd")
            basec = A.alloc([128, NE], F32); t_basec = T("basec")
            A.mark()
            g1bc = A.alloc([128, D], F32); t_g1bc = T("g1bc")
            gm2bc = A.alloc([128, D], F32); t_gm2bc = T("gm2bc")
            sh2bc = A.alloc([128, D], F32); t_sh2bc = T("sh2bc")
            bc_row(g1bc, 16, t_g1bc)
            bc_row(gm2bc, 56, t_gm2bc)
            bc_row(sh2bc, 24, t_sh2bc)
            w_out_sb = A.alloc([128, KC, D], BF16); t_wout = T("w_out")
            w_rt = A.alloc([128, KC, NE], F32); t_wrt = T("w_rt")
            brt = A.alloc([128, NE], F32); t_brt = T("brt")
            stg = [A.alloc([128, D], F32) for _ in range(2)]; t_stg = [T("stg0"), T("stg1")]
            for kc in range(KC):
                k.dma("sp", stg[kc % 2][:], w_out_d[kc * 128:(kc + 1) * 128, :], [], [t_stg[kc % 2]])
                k.cp(["dve", "act"][kc % 2], w_out_sb[:, kc, :], stg[kc % 2][:], [t_stg[kc % 2]], [t_wout])
            k.dma("sp", w_rt[:].rearrange("p c e -> p (c e)"), w_rt_d[:, :], [], [t_wrt])
            k.dma("sp", brt[:], b_rt_d[0:1, :].partition_broadcast(128), [], [t_brt])
            k.memset("pool", basec[:], 0.0, [t_basec])
            zt = A.alloc([128, 4, D], BF16); t_zt = T("zt")
            k.memset("pool", zt[:], 0.0, [t_zt])
            for jb in range(NBLK):
                k.dma("pool", HG[jb * BLK:(jb + 1) * BLK, :].rearrange("(a p) d -> p a d", p=128), zt[:],
                      [t_zt], [])
            x_t = [A.alloc([128, D], F32) for _ in range(2)]; t_x2 = [T("x2a"), T("x2b")]
            cat = [A.alloc([128, 8, 128], BF16) for _ in range(2)]; t_cat = [T("cat0"), T("cat1")]
            x1 = [A.alloc([128, D], F32) for _ in range(2)]; t_x1 = [T("x1a"), T("x1b")]
            h2 = [A.alloc([128, D], F32) for _ in range(2)]; t_h2 = [T("h2a"), T("h2b")]
            h2b = [A.alloc([128, D], BF16) for _ in range(2)]; t_h2b = [T("h2ba"), T("h2bb")]
            h2T = [A.alloc([128, KC, 128], F32) for _ in range(2)]; t_h2T = [T("h2Ta"), T("h2Tb")]
            rt = [A.alloc([128, 64], F32) for _ in range(2)]; t_rt = [T("rta"), T("rtb")]
            lg = [A.alloc([128, NE], F32) for _ in range(2)]; t_lg = [T("lga"), T("lgb")]
            ex = [A.alloc([128, NE], F32) for _ in range(2)]; t_ex = [T("exa"), T("exb")]
            mb = [A.alloc([128, NE], BF16) for _ in range(2)]; t_mb = [T("mba"), T("mbb")]
            jk2 = [A.alloc([128, D], BF16) for _ in range(2)]; t_jk2 = [T("jk2a"), T("jk2b")]
            ps_mix = [PSH[0][0], PSH[0][1]]; t_psmix = [t_psh[0][0], t_psh[0][1]]
            ps_trs = [psT[1], psT[3]]; t_pstrs = [t_psh[1][0], t_psh[3][0]]
            ps_rs = [PSH[2][0], PSH[2][1]]; t_psrs = [t_psh[2][0], t_psh[2][1]]

            def tile_pieces(i):
                b = i % 2
                cols = slice(i * 128, (i + 1) * 128)
                ps_tr = ps_trs[b]; t_pstr = t_pstrs[b]
                ps_r = ps_rs[b]; t_psr = t_psrs[b]
                pcs = []

                def p0():
                    k.dma("sp", x_t[b][:], x_d[cols, :], [], [t_x2[b]])
                    k.dma("sp", cat[b][:, 0:4, :], YR.rearrange("c p s -> p c s")[:, :, cols], [t_dram["YR"]], [t_cat[b]])
                    k.dma("sp", cat[b][:, 4:8, :], YA.rearrange("c p s -> p c s")[:, :, cols], [t_dram["YA"]], [t_cat[b]])
                pcs.append(p0)

                def p1():
                    for half in range(2):
                        for c in range(8):
                            k.mm(ps_mix[half][:, :], cat[b][:, c, :], w_out_sb[:, c, half * 512:(half + 1) * 512],
                                 c == 0, c == 7, [t_cat[b], t_wout], [t_psmix[half]])
                    for half in range(2):
                        hs_ = slice(half * 512, (half + 1) * 512)
                        k.tt("dve", x1[b][:, hs_], ps_mix[half][:, :], g1bc[:, hs_], ALU.mult,
                             [t_psmix[half], t_g1bc], [t_x1[b]])
                pcs.append(p1)

                def p2():
                    k.tt("pool", x1[b][:], x1[b][:], x_t[b][:], ALU.add, [t_x1[b], t_x2[b]], [t_x1[b]])
                    k.dma("pool", X1[cols, :], x1[b][:], [t_x1[b]], [])
                    k.act(jk2[b][:], x1[b][:], AF.Square, [t_x1[b]], [t_jk2[b], t_rt[b]], accum=rt[b][:, 0:1])
                pcs.append(p2)
                pcs.append(lambda: k.act(rt[b][:, 1:2], rt[b][:, 0:1], AF.Sqrt, [t_rt[b]], [t_rt[b]], scale=1.0 / D, bias=EPS))
                pcs.append(lambda: k.recip(rt[b][:, 2:3], rt[b][:, 1:2], [t_rt[b]], [t_rt[b]]))
                pcs.append(lambda: k.stt(h2[b][:], x1[b][:], rt[b][:, 2:3], gm2bc[:], ALU.mult, ALU.mult,
                                         [t_x1[b], t_rt[b], t_gm2bc], [t_h2[b]]))
                pcs.append(lambda: k.tt("pool", h2[b][:], h2[b][:], sh2bc[:], ALU.add, [t_h2[b], t_sh2bc], [t_h2[b]]))

                def p3():
                    k.cp("act", h2b[b][:], h2[b][:], [t_h2[b]], [t_h2b[b]])
                    k.dma("pool", H2[cols, :], h2b[b][:], [t_h2b[b]], [])
                    for kc in range(KC):
                        k.tr(ps_tr[:, kc * 128:(kc + 1) * 128], h2[b][:, kc * 128:(kc + 1) * 128], identf[:],
                             [t_h2[b], t_idf], [t_pstr])
                pcs.append(p3)
                pcs.append(lambda: k.cp("act", h2T[b][:].rearrange("p c t -> p (c t)"), ps_tr[:, :], [t_pstr], [t_h2T[b]]))

                def p4():
                    for kc in range(KC):
                        k.mm(ps_r[:, 0:NE], h2T[b][:, kc, :], w_rt[:, kc, :], kc == 0, kc == KC - 1,
                             [t_h2T[b], t_wrt], [t_psr])
                pcs.append(p4)
                pcs.append(lambda: k.tt("dve", lg[b][:], ps_r[:, 0:NE], brt[:], ALU.add, [t_psr, t_brt], [t_lg[b]]))
                pcs.append(lambda: P.add("dve", (lambda o_, i_: (lambda e: e.max(out=o_, in_=i_)))(rt[b][:, 8:16], lg[b][:]),
                                         [t_lg[b]], [t_rt[b]]))
                pcs.append(lambda: k.ts("dve", maskd[:, i, :], lg[b][:], rt[b][:, 11:12], None, ALU.is_ge, None,
                                        [t_lg[b], t_rt[b]], [t_maskd]))

                def p5():
                    k.cp("dve", mb[b][:], maskd[:, i, :], [t_maskd], [t_mb[b]])
                    k.ts("dve", rt[b][:, 3:4], rt[b][:, 8:9], -1.0, None, ALU.mult, None, [t_rt[b]], [t_rt[b]])
                pcs.append(p5)

                def p6():
                    k.act(ex[b][:], lg[b][:], AF.Exp, [t_lg[b], t_rt[b]], [t_ex[b]], bias=rt[b][:, 3:4])
                    k.mm(ps_r[:, 32:64], ustr[:], mb[b][:], True, True, [t_ustr, t_mb[b]], [t_psr])
                    k.mm(ps_r[:, 64:96], ones_b[:, 0:128], mb[b][:], True, True, [t_ones, t_mb[b]], [t_psr])
                pcs.append(p6)
                pcs.append(lambda: k.stt(ex[b][:], ex[b][:], 1.0, maskd[:, i, :], ALU.mult, ALU.mult,
                                         [t_ex[b], t_maskd], [t_ex[b], t_rt[b]], accum=rt[b][:, 4:5]))
                pcs.append(lambda: k.recip(rt[b][:, 5:6], rt[b][:, 4:5], [t_rt[b]], [t_rt[b]]))
                pcs.append(lambda: k.ts("dve", gated[:, i, :], ex[b][:], rt[b][:, 5:6], None, ALU.mult, None,
                                        [t_ex[b], t_rt[b]], [t_gated]))

                def p7():
                    k.tt("dve", rankd[:, i, :], ps_r[:, 32:64], basec[:], ALU.add, [t_psr, t_basec], [t_rankd])
                    k.tt("dve", basec[:], ps_r[:, 64:96], basec[:], ALU.add, [t_psr, t_basec], [t_basec])
                pcs.append(p7)
                return pcs

            for i0_ in range(0, NT, 2):
                run_pieces([tile_pieces(i0_), tile_pieces(i0_ + 1)])
            P.barrier()
            A.release()

        if stages >= 4:
            A.mark()
            jrow = A.alloc([128, JMAX], F32); t_jrow = T("jrow")
            cmp3 = A.alloc([128, NE, JMAX], F32); t_cmp3 = T("cmp3")
            nb = A.alloc([128, NE], F32); t_nb = T("nb")
            incl = A.alloc([128, NE], F32); t_incl = T("incl")
            pst = A.alloc([128, NE], F32); t_pst = T("pst")
            onesf = A.alloc([128, NE], F32); t_onesf = T("onesf")
            jb_ = A.alloc([128, NBLK], F32); t_jb = T("jb")
            cmpb = A.alloc([128, NBLK, NE], F32); t_cmpb = T("cmpb")
            be = A.alloc([128, NBLK], F32); t_be = T("be")
            widxf = A.alloc([128, NBLK, 8], F32); t_widxf = T("widxf")
            pbig = A.alloc([128, 1], F32); t_pbig = T("pbig")
            bef = A.alloc([128, NBLK], F32); t_bef = T("bef")
            key3 = A.alloc([128, NT, NE], F32); t_key3 = T("key3")
            top8 = A.alloc([128, 8], F32); t_top8 = T("top8")
            d4f = A.alloc([128, NT, 4], F32); t_d4f = T("d4f")
            jk32 = A.alloc([128, NE], F32); t_jk32 = T("jk32")
            h2l = [A.alloc([128, D], BF16) for _ in range(3)]; t_h2l = [T(f"h2l{i}") for i in range(3)]
            k.iota(jrow[:], [[BLK, JMAX]], 0, 0, [t_jrow])
            k.iota(jb_[:], [[1, NBLK]], 0, 0, [t_jb])
            k.memset("pool", onesf[:], 1.0, [t_onesf])
            k.tt("dve", cmp3[:], jrow[:].unsqueeze(1).to_broadcast([128, NE, JMAX]),
                 basec[:].unsqueeze(2).to_broadcast([128, NE, JMAX]), ALU.is_lt, [t_jrow, t_basec], [t_cmp3])
            P.add("dve", lambda e: e.tensor_reduce(out=nb[:], in_=cmp3[:], axis=AX.X, op=ALU.add), [t_cmp3], [t_nb])
            P.add("dve", lambda e: e.tensor_tensor_scan(out=incl[:], data0=onesf[:], data1=nb[:], initial=0.0,
                                                        op0=ALU.mult, op1=ALU.add), [t_onesf, t_nb], [t_incl])
            k.tt("dve", pst[:], incl[:], nb[:], ALU.subtract, [t_incl, t_nb], [t_pst])
            k.ts("dve", pst[:], pst[:], float(BLK), None, ALU.mult, None, [t_pst], [t_pst])
            k.tt("dve", cmpb[:], incl[:].unsqueeze(1).to_broadcast([128, NBLK, NE]),
                 jb_[:].unsqueeze(2).to_broadcast([128, NBLK, NE]), ALU.is_le, [t_incl, t_jb], [t_cmpb])
            P.add("dve", lambda e: e.tensor_reduce(out=be[:], in_=cmpb[:], axis=AX.X, op=ALU.add), [t_cmpb], [t_be])
            k.ts("dve", be[:], be[:], float(NE - 1), None, ALU.min, None, [t_be], [t_be])
            k.cp("dve", bidx[:], be[:], [t_be], [t_bidx])
            k.ts("dve", be[:], be[:], 1024.0, pidx[:, 0:1], ALU.mult, ALU.add, [t_be, t_pidx], [t_be])
            for kc in range(KC):
                k.ts("dve", widxf[:, :, kc], be[:], float(kc * 128), None, ALU.add, None, [t_be], [t_widxf])
            k.cp("dve", widx[:].rearrange("p b c -> p (b c)"), widxf[:].rearrange("p b c -> p (b c)"),
                 [t_widxf], [t_widx])
            k.tt("dve", key3[:], rankd[:], pst[:].unsqueeze(1).to_broadcast([128, NT, NE]), ALU.add,
                 [t_rankd, t_pst], [t_key3])
            k.ts("dve", key3[:], key3[:], -1.0, BIGC, ALU.mult, ALU.add, [t_key3], [t_key3])
            k.tt("dve", key3[:], key3[:], maskd[:], ALU.mult, [t_key3, t_maskd], [t_key3])
            for i in range(NT):
                P.add("dve", (lambda o_, i_: (lambda e: e.max(out=o_, in_=i_)))(top8[:], key3[:, i, :]),
                      [t_key3], [t_top8])
                k.ts("dve", d4f[:, i, :], top8[:, 0:4], -1.0, BIGC, ALU.mult, ALU.add, [t_top8], [t_d4f])
                for k4 in range(4):
                    k.stt(jk32[:], key3[:, i, :], top8[:, k4:k4 + 1], gated[:, i, :], ALU.is_equal, ALU.mult,
                          [t_key3, t_top8, t_gated], [t_jk32, t_gate4], accum=gate4[:, i, k4:k4 + 1])
            k.cp("dve", dest4[:].rearrange("p t f -> p (t f)"), d4f[:].rearrange("p t f -> p (t f)"),
                 [t_d4f], [t_dest4])
            if debug:
                k.dma("sp", RTD[:, 0:NT * 4], d4f[:].rearrange("p t f -> p (t f)"), [t_d4f], [])
                k.dma("sp", RTD[:, NT * 4:NT * 8], gate4[:].rearrange("p t f -> p (t f)"), [t_gate4], [])
            for i in range(NT):
                hb_ = h2l[i % 3]; thb = t_h2l[i % 3]
                k.dma("sp", hb_[:], H2[i * 128:(i + 1) * 128, :], [t_dram["H2"]], [thb])
                for k4 in range(4):
                    k.scatter(HG[:, :], hb_[:], dest4[:, i, k4:k4 + 1], [thb, t_dest4], [])
            P.barrier()
            A.release()
            A.release()

            A.mark()
            w1sb = [A.alloc([128, 9 * 2048], BF16) for _ in range(2)]; t_w1sb = [T("w1sb0"), T("w1sb1")]
            w2sb = [A.alloc([128, 9 * 1024], BF16) for _ in range(2)]; t_w2sb = [T("w2sb0"), T("w2sb1")]
            hg = [A.alloc([128, 4, D], BF16) for _ in range(2)]; t_hg = [T("hg0"), T("hg1")]
            hgT = A.alloc([128, KC, 512], BF16); t_hgT = [T(f"hgT{c}") for c in range(KC)]
            actT = A.alloc([128, KC, 512], BF16); t_actT = [T(f"actT{c}") for c in range(KC)]
            NE4 = 6
            ew = [A.alloc([128, 512], F32) for _ in range(NE4)]; t_ew = [T(f"ew{i}") for i in range(NE4)]
            ysb = [A.alloc([128, D], F32) for _ in range(2)]; t_ysb = [T("ysb0"), T("ysb1")]
            ewi = [0]

            def new_ew():
                i = ewi[0] % NE4; ewi[0] += 1
                return ew[i], t_ew[i]

            ps_t4 = [PSH[0][0].bitcast(BF16), PSH[0][1].bitcast(BF16)]; t_pst4 = [t_psh[0][0], t_psh[0][1]]
            ps_gl = [(PSH[1][0], t_psh[1][0], PSH[1][1], t_psh[1][1]), (PSH[2][0], t_psh[2][0], PSH[2][1], t_psh[2][1])]
            ps_y = [PSH[3][0], PSH[3][1]]; t_psy = [t_psh[3][0], t_psh[3][1]]
            ntr = 0; ngl = 0; ny = 0; nwf = 0
            NWS = 4
            wst = [A.alloc([128, 2048], F32) for _ in range(NWS)]; t_wst = [T(f"wst{i}") for i in range(NWS)]
            w1rows = w1_d.rearrange("e k n -> (e k) n")
            w2rows = w2_d.rearrange("e k n -> (e k) n")
            nwf_ = [0]

            def wsteps(jb):
                b = jb % 2
                for kc in range(KC):
                    s_ = nwf_[0] % NWS; nwf_[0] += 1
                    k.gather(wst[s_][:, :], w1rows[:, :], widx[:, jb, kc:kc + 1], [t_widx], [t_wst[s_]])
                    k.cp("act",
                         w1sb[b][:, kc * 2048:(kc + 1) * 2048].rearrange("p (two f) -> p two f", two=2),
                         wst[s_][:, :].rearrange("p (f two) -> p two f", two=2), [t_wst[s_]], [t_w1sb[b]])
                    yield
                s_ = nwf_[0] % NWS; nwf_[0] += 1
                k.gather(wst[s_][:, :], b1_d[:, :], bidx[:, jb:jb + 1], [t_bidx], [t_wst[s_]])
                k.cp("dve", w1sb[b][0:1, 8 * 2048:9 * 2048].rearrange("p (two f) -> p two f", two=2),
                     wst[s_][0:1, :].rearrange("p (f two) -> p two f", two=2), [t_wst[s_]], [t_w1sb[b]])
                yield
                for kc in range(KC):
                    s_ = nwf_[0] % NWS; nwf_[0] += 1
                    k.gather(wst[s_][:, 0:1024], w2rows[:, :], widx[:, jb, kc:kc + 1], [t_widx], [t_wst[s_]])
                    k.cp("dve", w2sb[b][:, kc * 1024:(kc + 1) * 1024], wst[s_][:, 0:1024], [t_wst[s_]], [t_w2sb[b]])
                    yield
                s_ = nwf_[0] % NWS; nwf_[0] += 1
                k.gather(wst[s_][:, 0:1024], b2_d[:, :], bidx[:, jb:jb + 1], [t_bidx], [t_wst[s_]])
                k.ts("dve", w2sb[b][0:1, 8 * 1024:9 * 1024], wst[s_][0:1, 0:1024], 1.702, None, ALU.mult, None,
                     [t_wst[s_]], [t_w2sb[b]])
                yield

            def adv(gen, n_=1):
                if gen is None:
                    return
                for _ in range(n_):
                    try:
                        next(gen)
                    except StopIteration:
                        return

            g0 = wsteps(0)
            adv(g0, 100)
            for jb in range(NBLK):
                b = jb % 2
                wgen = wsteps(jb + 1) if jb + 1 < NBLK else None
                k.dma("sp", hg[b][:], HG[jb * BLK:(jb + 1) * BLK, :].rearrange("(a p) d -> p a d", p=128),
                      [t_dram["HG"]], [t_hg[b]])
                for kc in range(KC):
                    pt_ = ps_t4[ntr % 2]; tpt_ = t_pst4[ntr % 2]; ntr += 1
                    for a_ in range(4):
                        k.tr(pt_[:, a_ * 128:(a_ + 1) * 128], hg[b][:, a_, kc * 128:(kc + 1) * 128], ident[:],
                             [t_hg[b], t_id], [tpt_])
                    k.cp("act" if kc % 2 == 0 else "dve", hgT[:, kc, :], pt_[:, 0:512], [tpt_], [t_hgT[kc]])
                    adv(wgen)
                for fc in range(KC):
                    pg, tpg, pl, tpl = ps_gl[ngl % 2]; ngl += 1
                    for (pz_, tz_, off) in ((pg, tpg, 0), (pl, tpl, 1024)):
                        for kc in range(KC):
                            k.mm(pz_[:, :], w1sb[b][:, kc * 2048 + off + fc * 128:kc * 2048 + off + (fc + 1) * 128],
                                 hgT[:, kc, :], kc == 0, False, [t_w1sb[b], t_hgT[kc]], [tz_])
                        k.mm(pz_[:, :], w1sb[b][0:1, 8 * 2048 + off + fc * 128:8 * 2048 + off + (fc + 1) * 128],
                             ones_b[0:1, 0:512], False, True, [t_w1sb[b], t_ones], [tz_])
                    g_, tg_ = new_ew()
                    k.ts("dve", g_[:], pg[:, :], 7.0, None, ALU.min, None, [tpg], [tg_])
                    sl, tsl = new_ew()
                    k.act(sl[:], g_[:], AF.Silu, [tg_], [tsl], scale=1.702)
                    l_, tl_ = new_ew()
                    k.ts("dve", l_[:], pl[:, :], -7.0, 7.0, ALU.max, ALU.min, [tpl], [tl_])
                    k.stt(actT[:, fc, :], l_[:], 1.0, sl[:], ALU.add, ALU.mult, [tl_, tsl], [t_actT[fc]])
                    adv(wgen)
                for a_ in range(4):
                    yb = ysb[ny % 2]; tyb = t_ysb[ny % 2]; ny += 1
                    for dh in range(2):
                        py = ps_y[dh]; tpy = t_psy[dh]
                        for fc in range(KC):
                            k.mm(py[:, :], actT[:, fc, a_ * 128:(a_ + 1) * 128],
                                 w2sb[b][:, fc * 1024 + dh * 512:fc * 1024 + (dh + 1) * 512], fc == 0, False,
                                 [t_actT[fc], t_w2sb[b]], [tpy])
                        k.mm(py[:, :], ones_b[0:1, 0:128], w2sb[b][0:1, 8 * 1024 + dh * 512:8 * 1024 + (dh + 1) * 512],
                             False, True, [t_ones, t_w2sb[b]], [tpy])
                        k.act(yb[:, dh * 512:(dh + 1) * 512], py[:, :], AF.Copy, [tpy], [tyb], scale=1.0 / 1.702)
                    r0 = jb * BLK + a_ * 128
                    k.dma("sp", YY[r0:r0 + 128, :], yb[:], [tyb], [])
                    adv(wgen)
                adv(wgen, 100)
            P.barrier()
            A.release()

            A.mark()
            g2bc = A.alloc([128, D], F32); t_g2bc = T("g2bc")
            bc_row(g2bc, 40, t_g2bc)
            NB5 = 3
            x1l = [A.alloc([128, D], F32) for _ in range(NB5)]; t_x1l = [T(f"x1l{q}") for q in range(NB5)]
            yg = [[A.alloc([128, D], F32) for _ in range(4)] for _ in range(NB5)]
            t_yg = [[T(f"yg{b}{q}") for q in range(4)] for b in range(NB5)]
            acc = [A.alloc([128, D], F32) for _ in range(NB5)]; t_acc = [T(f"acc{q}") for q in range(NB5)]
            for i in range(NT):
                b = i % NB5
                cols = slice(i * 128, (i + 1) * 128)
                k.dma("sp", x1l[b][:], X1[cols, :], [t_dram["X1"]], [t_x1l[b]])
                for k4 in range(4):
                    k.gather(yg[b][k4][:, :], YY[:, :], dest4[:, i, k4:k4 + 1], [t_dest4, t_dram["YY"]], [t_yg[b][k4]])
                k.ts("dve", acc[b][:], yg[b][0][:], gate4[:, i, 0:1], None, ALU.mult, None,
                     [t_yg[b][0], t_gate4], [t_acc[b]])
                for k4 in range(1, 4):
                    k.stt(acc[b][:], yg[b][k4][:], gate4[:, i, k4:k4 + 1], acc[b][:], ALU.mult, ALU.add,
                          [t_yg[b][k4], t_gate4, t_acc[b]], [t_acc[b]])
                k.tt("dve", acc[b][:], acc[b][:], g2bc[:], ALU.mult, [t_acc[b], t_g2bc], [t_acc[b]])
                k.tt("dve", acc[b][:], acc[b][:], x1l[b][:], ALU.add, [t_acc[b], t_x1l[b]], [t_acc[b]])
                k.dma("sp", out_d[cols, :], acc[b][:], [t_acc[b]], [])
            A.release()
        elif stages >= 1:
            A.mark()
            zt2 = A.alloc([128, D], F32); t_zt2 = T("zt2")
            k.memset("pool", zt2[:], 0.0, [t_zt2])
            k.dma("sp", out_d[0:128, :], zt2[:], [t_zt2], [])
            A.release()
        P.emit()
    return nc


def _fm(v, nchunk):
    return np.ascontiguousarray(np.asarray(v, np.float32).reshape(nchunk, 128).T)


def prep_shared(inp, small=False):
    L = 0
    f32 = np.float32
    sh = {}
    sh["w_ada"] = np.ascontiguousarray(inp["w_ada"][L], f32)
    sh["b_ada_fm"] = _fm(inp["b_ada"][L], 48)
    sh["n1g_fm"] = _fm(inp["norm1_g"][L], 8)
    sh["n2g_fm"] = _fm(inp["norm2_g"][L], 8)
    sh["w_in"] = np.ascontiguousarray(inp["w_in"][L], f32)
    cw = np.asarray(inp["conv_w"][L], f32)
    sh["convw_fm"] = np.ascontiguousarray(cw.T.reshape(4, 128, 4).transpose(1, 0, 2).reshape(128, 16))
    v4 = np.zeros((128, 28), f32)
    for j, name in enumerate(["conv_b", "b_rg_a", "b_rg_x", "lru_lambda", "rg_out_g", "attn_out_g"]):
        v4[:, j * 4:(j + 1) * 4] = _fm(inp[name][L], 4)
    v4[:, 24] = np.tile(np.asarray(inp["q_norm_g"][L], f32), 2)
    v4[:, 25] = np.tile(np.asarray(inp["k_norm_g"][L], f32), 2)
    v4[:, 26] = np.tile(np.asarray(inp["kidx_norm_g"][L], f32), 2)
    sh["vec4_fm"] = v4
    for nm, key in (("wabd", "w_rg_a"), ("wxbd", "w_rg_x")):
        w = np.asarray(inp[key][L], f32)
        bd = np.zeros((128, 4, 128), f32)
        for c in range(4):
            bd[0:64, c, 0:64] = w[2 * c]
            bd[64:128, c, 64:128] = w[2 * c + 1]
        sh[nm] = bd.reshape(128, 512)
    sh["w_out"] = np.ascontiguousarray(inp["w_out"][L], f32)
    wr = np.asarray(inp["w_router"][L], f32)
    sh["w_rt_fm"] = np.ascontiguousarray(wr.reshape(8, 128, 32).transpose(1, 0, 2).reshape(128, 256))
    sh["b_rt"] = np.asarray(inp["b_router"][L], f32).reshape(1, 32)
    if not small:
        sh["w1"] = np.ascontiguousarray(inp["w1"][L], f32)
        sh["b1"] = np.ascontiguousarray(inp["b1"][L], f32)
        sh["w2"] = np.ascontiguousarray(inp["w2"][L], f32)
        sh["b2"] = np.ascontiguousarray(inp["b2"][L], f32)
    return sh


def kernel(**inputs):
    x = np.asarray(inputs["x"], np.float32)
    c = np.asarray(inputs["c"], np.float32)
    B, S, _ = x.shape
    sh = prep_shared(inputs)
    nc = build_nc(S)
    in_maps = []
    for b in range(B):
        m = dict(sh)
        m["x"] = np.ascontiguousarray(x[b])
        m["c_fm"] = _fm(c[b], 8)
        in_maps.append(m)
    res = run_bass_kernel_spmd(nc, in_maps, core_ids=list(range(B)))
    return np.stack([np.asarray(r["out"], np.float32) for r in res.results], axis=0)
```
